# Optimizing a Trainium2 kernel written in Bass

```python
import jax, jax.numpy as jnp
from jax import lax
import numpy as np

D_MODEL = 1024
BATCH = 16
SEQ = 2048
DEPTH = 4

CTX_LEN = 256
GRID_W = 64
N_MIXERS = 2
EPS = 1e-6
ROPE_THETA = 10000.0
N_A_LAYERS = (DEPTH + 1) // 2
N_B_LAYERS = DEPTH // 2

MLA_HEADS = 16
QK_NOPE = 64
QK_ROPE = 32
V_HEAD = 64
Q_LORA = 256
KV_LORA = 256
QK_HEAD = QK_NOPE + QK_ROPE
MLA_WIDTH = MLA_HEADS * V_HEAD
MLA_IN = Q_LORA + KV_LORA + QK_ROPE + MLA_WIDTH
Q_BLOCK = 128
ATTN_SCALE = 1.0 / math.sqrt(QK_HEAD) if False else QK_HEAD ** -0.5

ML_INNER = 2 * D_MODEL
ML_HEADS = 8
ML_HEAD_DIM = ML_INNER // ML_HEADS
QKV_BLOCK = 4
N_QKV_BLOCKS = ML_INNER // QKV_BLOCK
CONV_K = 5
CHUNK = 64
ML_IN = 3 * ML_INNER

kernel_name = "hybrid_mla_mlstm_prefix_dit"


def rmsnorm(x, g):
    xf = x.astype(jnp.float32)
    r = lax.rsqrt(jnp.mean(xf * xf, axis=-1, keepdims=True) + EPS)
    return (xf * r).astype(x.dtype) * g


def modulation(cvec, w_ada, b_ada):
    m = jax.nn.silu(cvec) @ w_ada + b_ada
    return jnp.split(m, 3, axis=-1)


def axial_rope_tables(n_tokens):
    rows = n_tokens // GRID_W
    row = jnp.repeat(jnp.arange(rows, dtype=jnp.int32), GRID_W).astype(jnp.float32)
    col = jnp.tile(jnp.arange(GRID_W, dtype=jnp.int32), rows).astype(jnp.float32)
    qd = QK_ROPE // 4
    inv = ROPE_THETA ** (-jnp.arange(qd, dtype=jnp.float32) / qd)
    ang = jnp.stack([row[:, None] * inv, col[:, None] * inv], axis=1)
    return jnp.cos(ang), jnp.sin(ang)


def apply_axial_rope(x, cos, sin):
    qd = QK_ROPE // 4
    xr = x.reshape(x.shape[:-1] + (2, 2, qd))
    x1, x2 = xr[..., 0, :], xr[..., 1, :]
    c = cos[:, None].astype(x.dtype)
    s = sin[:, None].astype(x.dtype)
    return jnp.stack([x1 * c - x2 * s, x2 * c + x1 * s], axis=-2).reshape(x.shape)


def mla_project(h, w_in, q_norm, kv_norm, w_uq, w_ukv, cos, sin):
    B, T, _ = h.shape
    u = h @ w_in
    cq, ckv, kr, gate = jnp.split(u, [Q_LORA, Q_LORA + KV_LORA, Q_LORA + KV_LORA + QK_ROPE], axis=-1)
    q = (rmsnorm(cq, q_norm) @ w_uq).reshape(B, T, MLA_HEADS, QK_HEAD)
    kv = (rmsnorm(ckv, kv_norm) @ w_ukv).reshape(B, T, MLA_HEADS, QK_NOPE + V_HEAD)
    q_nope, q_rope = q[..., :QK_NOPE], q[..., QK_NOPE:]
    k_nope, v = kv[..., :QK_NOPE], kv[..., QK_NOPE:]
    kr = kr[:, :, None, :]
    if cos is not None:
        q_rope = apply_axial_rope(q_rope, cos, sin)
        kr = apply_axial_rope(kr, cos, sin)
    q = jnp.concatenate([q_nope, q_rope], axis=-1)
    k = jnp.concatenate([k_nope, jnp.broadcast_to(kr, (B, T, MLA_HEADS, QK_ROPE))], axis=-1)
    return q, k, v, gate


def attend(q, k, v):
    s = jnp.einsum('bqhd,bkhd->bhqk', q, k).astype(jnp.float32) * ATTN_SCALE
    p = jax.nn.softmax(s, axis=-1).astype(v.dtype)
    return jnp.einsum('bhqk,bkhd->bqhd', p, v)


def mla_mixer(hl, hc, w_in, q_norm, kv_norm, w_uq, w_ukv, w_out, cos, sin, need_ctx_out):
    B, T, _ = hl.shape
    ql, kl, vl, gl = mla_project(hl, w_in, q_norm, kv_norm, w_uq, w_ukv, cos, sin)
    qc, kc, vc, gc = mla_project(hc, w_in, q_norm, kv_norm, w_uq, w_ukv, None, None)
    k_all = jnp.concatenate([kc, kl], axis=1)
    v_all = jnp.concatenate([vc, vl], axis=1)
    nb = T // Q_BLOCK
    qb = ql.reshape(B, nb, Q_BLOCK, MLA_HEADS, QK_HEAD).swapaxes(0, 1)
    ob = lax.map(lambda qq: attend(qq, k_all, v_all), qb)
    ol = ob.swapaxes(0, 1).reshape(B, T, MLA_WIDTH)
    yl = (ol * jax.nn.silu(gl)) @ w_out
    yc = None
    if need_ctx_out:
        oc = attend(qc, kc, vc).reshape(B, hc.shape[1], MLA_WIDTH)
        yc = (oc * jax.nn.silu(gc)) @ w_out
    return yl, yc


def centred_dwconv(x, w, b):
    y = lax.conv_general_dilated(x, w[:, None, :], window_strides=(1,),
                                 padding=[(CONV_K // 2, CONV_K // 2)],
                                 dimension_numbers=('NWC', 'WIO', 'NWC'),
                                 feature_group_count=x.shape[-1])
    return y + b


def blockdiag(x, w):
    B, T, _ = x.shape
    xb = x.reshape(B, T, N_QKV_BLOCKS, QKV_BLOCK)
    return jnp.einsum('btnc,ncd->btnd', xb, w).reshape(B, T, ML_INNER)


def mlstm_features(h, w_in, conv_w, conv_b, w_q, w_k, w_v):
    B, T, _ = h.shape
    xm, z, og = jnp.split(h @ w_in, 3, axis=-1)
    xc = jax.nn.silu(centred_dwconv(xm, conv_w, conv_b))
    q = blockdiag(xc, w_q)
    k = blockdiag(xc, w_k)
    v = blockdiag(xm, w_v)
    qkv = jnp.concatenate([q, k, v], axis=-1)
    heads = lambda a: a.reshape(B, T, ML_HEADS, ML_HEAD_DIM)
    return heads(q), heads(k) * (ML_HEAD_DIM ** -0.5), heads(v), qkv, xc, z, jax.nn.sigmoid(og)


def mlstm_gates(qkv, w_if, b_if):
    g = (qkv @ w_if + b_if).astype(jnp.float32)
    li, fpre = g[..., :ML_HEADS], g[..., ML_HEADS:]
    return li, jax.nn.log_sigmoid(fpre)


def mlstm_chunk_scan(q, k, v, li, lf, state, with_output):
    B, T, H, dh = q.shape
    nc = T // CHUNK
    to_c = lambda a: a.reshape(B, nc, CHUNK, H, dh).transpose(1, 0, 3, 2, 4)
    to_g = lambda a: a.reshape(B, nc, CHUNK, H).transpose(1, 0, 3, 2)
    tril = jnp.tril(jnp.ones((CHUNK, CHUNK), dtype=bool))

    def step(carry, xs):
        C, n, m = carry
        qq, kk, vv, ig, fg = xs
        b = jnp.cumsum(fg, axis=-1)
        bL = b[..., -1]
        w_end = bL[..., None] - b + ig
        m_new = jnp.maximum(bL + m, jnp.max(w_end, axis=-1))
        a_end = jnp.exp(w_end - m_new[..., None])
        decay = jnp.exp(bL + m - m_new)
        C_new = decay[..., None, None] * C + jnp.einsum('bhl,bhld,bhle->bhde', a_end, vv, kk)
        n_new = decay[..., None] * n + jnp.einsum('bhl,bhld->bhd', a_end, kk)
        if not with_output:
            return (C_new, n_new, m_new), None
        logD = jnp.where(tril, b[..., :, None] - b[..., None, :] + ig[..., None, :], -jnp.inf)
        m_t = jnp.maximum(b + m[..., None], jnp.max(logD, axis=-1))
        Dm = jnp.exp(logD - m_t[..., None])
        inter = jnp.exp(b + m[..., None] - m_t)
        sqk = jnp.einsum('bhtd,bhsd->bhts', qq, kk) * Dm
        num = jnp.einsum('bhts,bhsd->bhtd', sqk, vv) + inter[..., None] * jnp.einsum('bhde,bhte->bhtd', C, qq)
        den = jnp.sum(sqk, axis=-1) + inter * jnp.einsum('bhd,bhtd->bht', n, qq)
        hh = num / jnp.maximum(jnp.abs(den), jnp.exp(-m_t))[..., None]
        return (C_new, n_new, m_new), hh

    carry, hs = lax.scan(step, state, (to_c(q), to_c(k), to_c(v), to_g(li), to_g(lf)))
    h = None
    if with_output:
        h = hs.transpose(1, 0, 3, 2, 4).reshape(B, T, H, dh)
    return carry, h


def mlstm_direction(q, k, v, li, lf, state, reverse, with_output):
    if reverse:
        q, k, v, li, lf = (jnp.flip(a, axis=1) for a in (q, k, v, li, lf))
    st, h = mlstm_chunk_scan(q, k, v, li, lf, state, with_output)
    if reverse and h is not None:
        h = jnp.flip(h, axis=1)
    return st, h


def mlstm_output(h, o, xc, z, head_norm, skip, w_out):
    B, T = xc.shape[:2]
    hh = o.reshape(B, T, ML_HEADS, ML_HEAD_DIM).astype(jnp.float32) * h
    hn = hh * lax.rsqrt(jnp.mean(hh * hh, axis=-1, keepdims=True) + EPS)
    hn = hn.reshape(B, T, ML_INNER).astype(xc.dtype) * head_norm
    return ((hn + skip * xc) * jax.nn.silu(z)) @ w_out


def mlstm_mixer(hl, hc, w_in, conv_w, conv_b, w_q, w_k, w_v, w_if, b_if, head_norm, skip, w_out, need_ctx_out):
    B = hl.shape[0]
    ql, kl, vl, qkvl, xcl, zl, ol = mlstm_features(hl, w_in, conv_w, conv_b, w_q, w_k, w_v)
    qc, kc, vc, qkvc, xcc, zc, oc = mlstm_features(hc, w_in, conv_w, conv_b, w_q, w_k, w_v)
    zero_state = (jnp.zeros((B, ML_HEADS, ML_HEAD_DIM, ML_HEAD_DIM), jnp.float32),
                  jnp.zeros((B, ML_HEADS, ML_HEAD_DIM), jnp.float32),
                  jnp.zeros((B, ML_HEADS), jnp.float32))
    h_lat = jnp.zeros(ql.shape, jnp.float32)
    h_ctx = jnp.zeros(qc.shape, jnp.float32)
    for d, reverse in ((0, False), (1, True)):
        li_c, lf_c = mlstm_gates(qkvc, w_if[d], b_if[d])
        st_c, hc_d = mlstm_direction(qc, kc, vc, li_c, lf_c, zero_state, reverse, need_ctx_out)
        li_l, lf_l = mlstm_gates(qkvl, w_if[d], b_if[d])
        _, hl_d = mlstm_direction(ql, kl, vl, li_l, lf_l, st_c, reverse, True)
        h_lat = h_lat + hl_d
        if need_ctx_out:
            h_ctx = h_ctx + hc_d
    yl = mlstm_output(h_lat, ol, xcl, zl, head_norm, skip, w_out)
    yc = mlstm_output(h_ctx, oc, xcc, zc, head_norm, skip, w_out) if need_ctx_out else None
    return yl, yc


def setup_inputs(seed: int = 0) -> dict:
    key = jax.random.key(seed)
    ks = iter(jax.random.split(key, 40))
    nrm = lambda shape, scale: jax.random.normal(next(ks), shape, jnp.float32) * scale
    A, Bn = N_A_LAYERS, N_B_LAYERS
    b_if = jnp.concatenate([nrm((Bn, 2, ML_HEADS), 0.1),
                            jnp.linspace(3.0, 6.0, ML_HEADS, dtype=jnp.float32) + nrm((Bn, 2, ML_HEADS), 0.01)], axis=-1)
    return {
        "x": nrm((BATCH, SEQ, D_MODEL), 1.0),
        "c": nrm((BATCH, D_MODEL), 1.0),
        "ctx": nrm((BATCH, CTX_LEN, D_MODEL), 1.0),
        "c_ctx": nrm((D_MODEL,), 1.0),
        "ada_w": nrm((DEPTH, D_MODEL, 3 * D_MODEL), 0.5 * D_MODEL ** -0.5),
        "ada_b": nrm((DEPTH, 3 * D_MODEL), 0.02),
        "norm_pre": 1.0 + nrm((DEPTH, D_MODEL), 0.01),
        "norm_post": 1.0 + nrm((DEPTH, D_MODEL), 0.01),
        "mla_w_in": nrm((A, D_MODEL, MLA_IN), D_MODEL ** -0.5),
        "mla_q_norm": 1.0 + nrm((A, Q_LORA), 0.01),
        "mla_kv_norm": 1.0 + nrm((A, KV_LORA), 0.01),
        "mla_w_uq": nrm((A, Q_LORA, MLA_HEADS * QK_HEAD), Q_LORA ** -0.5),
        "mla_w_ukv": nrm((A, KV_LORA, MLA_HEADS * (QK_NOPE + V_HEAD)), KV_LORA ** -0.5),
        "mla_w_out": nrm((A, MLA_WIDTH, D_MODEL), MLA_WIDTH ** -0.5),
        "ml_w_in": nrm((Bn, D_MODEL, ML_IN), D_MODEL ** -0.5),
        "ml_conv_w": nrm((Bn, CONV_K, ML_INNER), CONV_K ** -0.5),
        "ml_conv_b": nrm((Bn, ML_INNER), 0.02),
        "ml_w_q": nrm((Bn, N_QKV_BLOCKS, QKV_BLOCK, QKV_BLOCK), QKV_BLOCK ** -0.5),
        "ml_w_k": nrm((Bn, N_QKV_BLOCKS, QKV_BLOCK, QKV_BLOCK), QKV_BLOCK ** -0.5),
        "ml_w_v": nrm((Bn, N_QKV_BLOCKS, QKV_BLOCK, QKV_BLOCK), QKV_BLOCK ** -0.5),
        "ml_w_if": nrm((Bn, 2, 3 * ML_INNER, 2 * ML_HEADS), 0.5 * (3 * ML_INNER) ** -0.5),
        "ml_b_if": b_if,
        "ml_head_norm": 1.0 + nrm((Bn, ML_INNER), 0.01),
        "ml_skip": 1.0 + nrm((Bn, ML_INNER), 0.01),
        "ml_w_out": nrm((Bn, ML_INNER, D_MODEL), ML_INNER ** -0.5),
    }


def reference(x, c, ctx, c_ctx, ada_w, ada_b, norm_pre, norm_post,
              mla_w_in, mla_q_norm, mla_kv_norm, mla_w_uq, mla_w_ukv, mla_w_out,
              ml_w_in, ml_conv_w, ml_conv_b, ml_w_q, ml_w_k, ml_w_v, ml_w_if, ml_b_if,
              ml_head_norm, ml_skip, ml_w_out):
    T = x.shape[1]
    cos, sin = axial_rope_tables(T)
    for i in range(DEPTH):
        need_ctx_out = i < DEPTH - 1
        sh_l, sc_l, g_l = modulation(c, ada_w[i], ada_b[i])
        sh_c, sc_c, g_c = modulation(c_ctx, ada_w[i], ada_b[i])
        hl = rmsnorm(x, norm_pre[i]) * (1.0 + sc_l[:, None]) + sh_l[:, None]
        hc = rmsnorm(ctx, norm_pre[i]) * (1.0 + sc_c) + sh_c
        j = i // N_MIXERS
        if i % N_MIXERS == 0:
            yl, yc = mla_mixer(hl, hc, mla_w_in[j], mla_q_norm[j], mla_kv_norm[j], mla_w_uq[j],
                               mla_w_ukv[j], mla_w_out[j], cos, sin, need_ctx_out)
        else:
            yl, yc = mlstm_mixer(hl, hc, ml_w_in[j], ml_conv_w[j], ml_conv_b[j], ml_w_q[j], ml_w_k[j],
                                 ml_w_v[j], ml_w_if[j], ml_b_if[j], ml_head_norm[j], ml_skip[j],
                                 ml_w_out[j], need_ctx_out)
        x = x + g_l[:, None] * rmsnorm(yl, norm_post[i])
        if need_ctx_out:
            ctx = ctx + g_c * rmsnorm(yc, norm_post[i])
    return x
```

```python
import math
from contextlib import ExitStack

import numpy as np
import concourse.bass as bass
import concourse.mybir as mybir
from concourse.bass_utils import run_bass_kernel_spmd

F32 = mybir.dt.float32
BF16 = mybir.dt.bfloat16
ALU = mybir.AluOpType
AF = mybir.ActivationFunctionType
AX = mybir.AxisListType

D = 1024
T_LAT = 2048
T_CTX = 256
TA = T_LAT + T_CTX
NT = TA // 128
EPS = 1e-6
NH = 16
ATT_SCALE = 96 ** -0.5
LN16 = math.log(16.0)

EPOCH = 4096
NSLOT = 8
COMPUTE = ("pe", "act", "dve", "pool")
ENGINES = ("pe", "act", "dve", "pool", "sp")


class Op:
    __slots__ = ("eng", "fn", "idx", "is_dma", "signal", "sem", "val", "waits",
                 "eidx", "slot", "prev_slot_wait")


class Sched:
    def __init__(self):
        self.ops = []
        self.by_eng = {e: [] for e in ENGINES}
        self.last_writer = {}
        self.readers = {}
        self.waited = {}
        self.dma_waited = {e: set() for e in ENGINES}
        self.dma_count = {e: 0 for e in ENGINES}
        self.slot_last = {}

    def _add(self, eng, fn, reads, writes, is_dma):
        op = Op()
        op.eng = eng
        op.fn = fn
        op.is_dma = is_dma
        op.signal = is_dma
        op.sem = None
        op.val = None
        op.waits = []
        op.slot = None
        op.prev_slot_wait = None
        op.idx = len(self.ops)
        op.eidx = len(self.by_eng[eng])
        self.ops.append(op)
        self.by_eng[eng].append(op)
        psr = [k for k in reads if k[0] == "ps"]
        if psr:
            writes = list(writes) + psr
        lw = self.last_writer
        rd = self.readers
        deps = set()
        same_raw = None
        for k in reads:
            w = lw.get(k)
            if w is not None:
                deps.add(w)
                if w.eng == eng and eng != "pe" and not w.is_dma:
                    if same_raw is None or w.eidx > same_raw.eidx:
                        same_raw = w
        for k in writes:
            w = lw.get(k)
            if w is not None:
                deps.add(w)
            r = rd.get(k)
            if r:
                deps.update(r)
        best = {}
        for d in deps:
            if d is op:
                continue
            if d.is_dma:
                if d.idx not in self.dma_waited[eng]:
                    self.dma_waited[eng].add(d.idx)
                    op.waits.append(d)
            else:
                if d.eng == eng:
                    continue
                b = best.get(d.eng)
                if b is None or d.eidx > b.eidx:
                    best[d.eng] = d
        if same_raw is not None and not is_dma:
            best[eng] = same_raw
        for pe, d in best.items():
            key = (eng, pe)
            if self.waited.get(key, -1) >= d.eidx:
                continue
            self.waited[key] = d.eidx
            d.signal = True
            op.waits.append(d)
        if is_dma:
            n = self.dma_count[eng]
            self.dma_count[eng] = n + 1
            op.slot = (eng, n % NSLOT)
            prev = self.slot_last.get(op.slot)
            if prev is not None and prev.idx not in self.dma_waited[eng]:
                self.dma_waited[eng].add(prev.idx)
                op.prev_slot_wait = prev
            self.slot_last[op.slot] = op
        for k in reads:
            r = rd.get(k)
            if r is None:
                rd[k] = [op]
            else:
                if not op.is_dma:
                    r[:] = [x for x in r if x.is_dma or x.eng != eng]
                r.append(op)
        for k in writes:
            lw[k] = op
            rd[k] = []
        return op

    def op(self, eng, fn, reads=(), writes=()):
        return self._add(eng, fn, reads, writes, False)

    def dma(self, eng, fn, reads=(), writes=()):
        return self._add(eng, fn, reads, writes, True)

    def emit(self, nc, stack):
        sems = {}

        def get_sem(name):
            if name not in sems:
                sems[name] = stack.enter_context(nc.semaphore(name))
            return sems[name]

        for e in COMPUTE:
            cnt = 0
            for op in self.by_eng[e]:
                if op.is_dma or not op.signal:
                    continue
                op.sem = get_sem("s_%s_%d" % (e, cnt // EPOCH))
                op.val = cnt % EPOCH + 1
                cnt += 1
        slot_cnt = {}
        for op in self.ops:
            if op.is_dma:
                c = slot_cnt.get(op.slot, 0) + 1
                slot_cnt[op.slot] = c
                op.sem = get_sem("d_%s_%d" % op.slot)
                op.val = 16 * c
        finals = list(self.slot_last.values())
        block = stack.enter_context(nc.Block())

        def run(eng):
            def body(h):
                for op in self.by_eng[eng]:
                    if op.prev_slot_wait is not None:
                        p = op.prev_slot_wait
                        h.wait_ge(p.sem, p.val)
                    for d in op.waits:
                        h.wait_ge(d.sem, d.val)
                    ins = op.fn(h)
                    if op.signal:
                        ins.then_inc(op.sem, 16 if op.is_dma else 1)
                if eng == "sp":
                    for o in finals:
                        h.wait_ge(o.sem, o.val)
            return body

        block.tensor(run("pe"))
        block.scalar(run("act"))
        block.vector(run("dve"))
        block.gpsimd(run("pool"))
        block.sync(run("sp"))


GRAN = 256


class X:
    __slots__ = ("ap", "k")

    def __init__(self, ap, k):
        self.ap = ap
        self.k = k


class View:
    def __init__(self, space, base_ap, off, shape, esz, gran=GRAN):
        self.space = space
        self.ap = base_ap
        self.off = off
        self.shape = list(shape)
        self.esz = esz
        self.gran = gran
        st = []
        s = 1
        for n in reversed(self.shape):
            st.append(s)
            s *= n
        self.strides = list(reversed(st))
        self.nelem = s

    def __call__(self, pr=None, *idx):
        p0, p1 = (0, 128) if pr is None else pr
        sl = [slice(p0, p1)]
        lo = 0
        hi = 0
        for d, n in enumerate(self.shape):
            it = idx[d] if d < len(idx) else None
            if it is None:
                a, b = 0, n
                sl.append(slice(None))
            elif isinstance(it, int):
                a, b = it, it + 1
                sl.append(it)
            else:
                a, b = it
                sl.append(slice(a, b))
            lo += a * self.strides[d]
            hi += (b - 1) * self.strides[d]
        hi += 1
        b0 = self.off + lo * self.esz
        b1 = self.off + hi * self.esz
        halves = []
        if p0 < 64:
            halves.append(0)
        if p1 > 64:
            halves.append(1)
        g0 = b0 // self.gran
        g1 = (b1 - 1) // self.gran
        sp = self.space
        if sp == "ps":
            keys = [(sp, g) for g in range(g0, g1 + 1)]
        else:
            keys = [(sp, g, h) for g in range(g0, g1 + 1) for h in halves]
        return X(self.ap[tuple(sl)], keys)


class Arena:
    def __init__(self, nc, st, nbytes):
        self.nbytes = nbytes
        self.t = st.enter_context(nc.sbuf_tensor("arena", [128, nbytes // 2], BF16))
        self.top = 0

    def view(self, off, shape, dtype):
        esz = 4 if dtype == F32 else 2
        n = 1
        for s in shape:
            n *= s
        assert off % 4 == 0
        assert off + n * esz <= self.nbytes, ("arena overflow", off, n * esz, self.nbytes)
        ap = self.t[:, off // 2: off // 2 + n * esz // 2]
        if dtype == F32:
            ap = ap.bitcast(F32)
        if len(shape) > 1:
            names = " ".join("d%d" % i for i in range(len(shape)))
            kw = {"d%d" % i: shape[i] for i in range(1, len(shape))}
            ap = ap.rearrange("p (%s) -> p %s" % (names, names), **kw)
        return View("sb", ap, off, shape, esz)


class Bump:
    def __init__(self, arena, start, limit):
        self.a = arena
        self.top = start
        self.limit = limit
        self.peak = start

    def alloc(self, shape, dtype, align=GRAN):
        esz = 4 if dtype == F32 else 2
        n = esz
        for s in shape:
            n *= s
        off = (self.top + align - 1) // align * align
        self.top = off + n
        assert self.top <= self.limit, ("bump overflow", self.top, self.limit)
        self.peak = max(self.peak, self.top)
        return self.a.view(off, shape, dtype)


class Prog:
    def __init__(self, NB, layers, debug=None, stop=None):
        self.stop = stop
        self.NB = NB
        self.layers = layers
        self.debug = debug
        self.nc = bass.Bass("TRN2", target_bir_lowering=False)
        self.S = Sched()
        self.psrr = 0

    def mm(self, out, lhsT, rhs, start=True, stop=True):
        rk = lhsT.k + rhs.k
        if not start:
            rk = rk + out.k
        o, l, r = out.ap, lhsT.ap, rhs.ap
        self.S.op("pe", lambda h: h.matmul(o, lhsT=l, rhs=r, start=start, stop=stop),
                  reads=rk, writes=out.k)

    def tr(self, out, in_, ident):
        o, i, d = out.ap, in_.ap, ident.ap
        self.S.op("pe", lambda h: h.transpose(o, i, d), reads=in_.k + ident.k, writes=out.k)

    def act(self, out, in_, func, scale=1.0, bias=0.0, accum=None):
        rk = list(in_.k)
        sc = scale
        bi = bias
        if isinstance(scale, X):
            rk += scale.k
            sc = scale.ap
        if isinstance(bias, X):
            rk += bias.k
            bi = bias.ap
        wk = list(out.k)
        ac = None
        if accum is not None:
            wk += accum.k
            ac = accum.ap
        o, i = out.ap, in_.ap
        if ac is None:
            fn = lambda h: h.activation(out=o, in_=i, func=func, bias=bi, scale=sc)
        else:
            fn = lambda h: h.activation(out=o, in_=i, func=func, bias=bi, scale=sc, accum_out=ac)
        self.S.op("act", fn, reads=rk, writes=wk)

    def tt(self, eng, out, in0, in1, op):
        o, a, b = out.ap, in0.ap, in1.ap
        self.S.op(eng, lambda h: h.tensor_tensor(out=o, in0=a, in1=b, op=op),
                  reads=in0.k + in1.k, writes=out.k)

    def ts(self, eng, out, in0, s1, op0, s2=None, op1=None):
        rk = list(in0.k)
        a1 = s1
        a2 = s2
        if isinstance(s1, X):
            rk += s1.k
            a1 = s1.ap
        if isinstance(s2, X):
            rk += s2.k
            a2 = s2.ap
        o, i = out.ap, in0.ap
        if op1 is None:
            fn = lambda h: h.tensor_scalar(out=o, in0=i, scalar1=a1, scalar2=None, op0=op0)
        else:
            fn = lambda h: h.tensor_scalar(out=o, in0=i, scalar1=a1, scalar2=a2, op0=op0, op1=op1)
        self.S.op(eng, fn, reads=rk, writes=out.k)

    def stt(self, out, in0, scalar, in1, op0, op1):
        rk = in0.k + in1.k
        sc = scalar
        if isinstance(scalar, X):
            rk = rk + scalar.k
            sc = scalar.ap
        o, a, b = out.ap, in0.ap, in1.ap
        self.S.op("dve", lambda h: h.scalar_tensor_tensor(out=o, in0=a, scalar=sc, in1=b, op0=op0, op1=op1),
                  reads=rk, writes=out.k)

    def copy(self, eng, out, in_):
        o, i = out.ap, in_.ap
        if eng == "act":
            self.S.op("act", lambda h: h.activation(out=o, in_=i, func=AF.Copy), reads=in_.k, writes=out.k)
        else:
            self.S.op(eng, lambda h: h.tensor_copy(out=o, in_=i), reads=in_.k, writes=out.k)

    def recip(self, out, in_, fast=False):
        o, i = out.ap, in_.ap
        if fast:
            self.S.op("dve", lambda h: h.reciprocal_approx_fast(out=o, in_=i), reads=in_.k, writes=out.k)
        else:
            self.S.op("dve", lambda h: h.reciprocal(out=o, in_=i), reads=in_.k, writes=out.k)

    def memset(self, eng, out, val):
        o = out.ap
        self.S.op(eng, lambda h: h.memset(o, val), writes=out.k)

    def dma(self, eng, out, in_, out_keys=None, in_keys=None, **kw):
        ok = out.k if isinstance(out, X) else (out_keys or [])
        ik = in_.k if isinstance(in_, X) else (in_keys or [])
        o = out.ap if isinstance(out, X) else out
        i = in_.ap if isinstance(in_, X) else in_
        self.S.dma(eng, lambda h: h.dma_start(out=o, in_=i, **kw), reads=ik, writes=ok)

    def dump(self, name, view, nrow_chunks=1):
        if not (self.debug and name in self.debug):
            return
        tmpf = self.L.a.view(self.L.a.nbytes - TA * 4, [TA], F32)
        for k in range(nrow_chunks):
            src = view(None, k) if nrow_chunks > 1 else view()
            self.copy("dve", tmpf(), src)
            self.dma("sp", self.d[name][k * 128:(k + 1) * 128, :], tmpf())

    def barrier(self):
        S = self.S
        toks = []
        for e in ("pe", "act", "dve", "pool"):
            k = ("bar", e)
            toks.append(k)
            S.op(e, lambda h: h.nop(), reads=[], writes=[k])
        outstanding = [("bar_dma", o.idx) for o in S.slot_last.values()]
        for o in list(S.slot_last.values()):
            S.last_writer[("bar_dma", o.idx)] = o
        for e in ("pe", "act", "dve", "pool"):
            S.op(e, lambda h: h.nop(), reads=toks + outstanding, writes=[])
        S.op("sp", lambda h: h.nop(), reads=toks + outstanding, writes=[("bar", "sp")])

    def ps(self, n=1):
        if n == 2:
            if self.psrr % 2:
                self.psrr += 1
            i = (self.psrr // 2) % len(self.pspair)
            self.psrr += 2
            return self.pspair[i]
        i = self.psrr % len(self.psbank)
        self.psrr += 1
        return self.psbank[i]

    def build(self):
        nc = self.nc
        NB = self.NB
        dt = nc.dram_tensor
        I = "ExternalInput"
        self.d = d = {}

        def inp(name, shape, dtype=F32):
            d[name] = dt(name, list(shape), dtype, kind=I).ap()

        inp("x", [NB, T_LAT, D])
        inp("c", [NB, D])
        inp("ctx", [NB, T_CTX, D])
        inp("c_ctx", [D])
        inp("ada_w", [4, D, 3 * D])
        inp("ada_bc", [4, 128, 24])
        inp("npre_c", [4, 128, 8])
        inp("npost", [4, D])
        inp("mla_win", [2, D, 1920])
        inp("mla_wuq_c", [2, 256, 2048])
        inp("mla_qn_c", [2, 128, 2])
        inp("mla_kvn_c", [2, 128, 2])
        inp("mla_wuq", [2, 256, 2048])
        inp("mla_wukv", [2, 256, 2048])
        inp("mla_wout", [2, D, D])
        inp("ml_win", [2, D, 6144])
        inp("ml_cw_c", [2, 128, 16, 5])
        inp("ml_cb_c", [2, 128, 16])
        inp("ml_wq_c", [2, 128, 16, 4])
        inp("ml_wk_c", [2, 128, 16, 4])
        inp("ml_wv_c", [2, 128, 16, 4])
        inp("ml_wqT_c", [2, 128, 16, 4])
        inp("ml_wkT_c", [2, 128, 16, 4])
        inp("ml_wvT_c", [2, 128, 16, 4])
        inp("ml_wif", [2, 2, 6144, 16])
        inp("ml_bif_c", [2, 32, 1])
        inp("ml_hn_c", [2, 128, 16])
        inp("ml_sk_c", [2, 128, 16])
        inp("ml_wout", [2, 2 * D, D])
        inp("k_ident", [128, 128])
        inp("k_maskF", [128, 128])
        inp("k_maskB", [128, 128])
        inp("k_bdmask", [128, 128])
        inp("k_tq", [128, TA])
        inp("k_tkc", [128, TA])
        inp("k_tks", [128, TA])
        d["out"] = dt("out", [NB, T_LAT, D], F32, kind="ExternalOutput").ap()
        d["ctxs"] = dt("ctxs", [NB, T_CTX, D], F32, kind="Internal").ap()
        d["ptd"] = dt("ptd", [NT, 128, 16, 128], BF16, kind="Internal").ap()
        if self.debug:
            for name, shape in self.debug.items():
                d[name] = dt(name, list(shape), F32, kind="ExternalOutput").ap()

        with ExitStack() as st:
            self.st = st
            ARENA_BYTES = 200 * 1024
            self.arena = Arena(nc, st, ARENA_BYTES)
            pp = [st.enter_context(nc.psum_tensor("pp%d" % i, [128, 1024], F32)) for i in range(4)]
            self.pspair = []
            self.psbank = []
            for i in range(4):
                self.pspair.append(View("ps", pp[i][:, :], i * 4096, [1024], 4, gran=2048))
                for hh in range(2):
                    self.psbank.append(View("ps", pp[i][:, hh * 512:(hh + 1) * 512],
                                            i * 4096 + hh * 2048, [512], 4, gran=2048))
            self.psbf = []
            for i in range(4):
                for hh in range(2):
                    self.psbf.append(View("ps", pp[i][:, hh * 512:(hh + 1) * 512].bitcast(BF16),
                                          i * 4096 + hh * 2048, [1024], 2, gran=2048))
            self.psrr = 0
            self.psrr2 = 0
            self.setup()
            import os
            if os.environ.get("KBAR"):
                self.barrier()
            if self.stop != "setup":
                for b in range(NB):
                    for li in self.layers:
                        self.layer(b, li)
            self.S.emit(nc, st)
        return nc

    def setup(self):
        d = self.d
        A = self.arena
        P = Bump(A, 0, A.nbytes)
        self.persist = P
        al = P.alloc
        self.ident = al([128], F32)
        self.identb = al([128], BF16)
        self.maskF = al([128], F32)
        self.maskB = al([128], F32)
        self.bdmask = al([128], F32)
        self.ones = al([128], F32)
        self.onesb = al([128], BF16)
        self.modcol = al([4, 24, 3], F32)
        self.adab = al([4, 24], F32)
        self.npre = al([4, 8], F32)
        self.silc = al([8, 3], F32)
        self.hT = al([8, TA], BF16)
        self.junk = al([D], BF16)
        self.small = al([64], F32)
        self.acol = al([2, 8], F32)
        self.bcol = al([2, 8], F32)
        self.gcol = al([2, 8], F32)
        self.ssq_ = al([NT + 2, 8], F32)
        self.rr_ = al([NT + 2, 8], F32)
        self.ssq = lambda pr, cr: self.ssq_(pr, cr[0], (0, 1))
        self.rr = lambda pr, cr: self.rr_(pr, cr[0], (0, 1))
        self.cst = al([4], F32)
        craw = al([8, 3], F32)
        th = al([8, 3], F32)
        wst = [al([8, 128], F32) for _ in range(2)]
        self.layer_base = P.top
        for i_, v_ in enumerate((EPS, 4 * EPS, LN16, 1.0)):
            self.memset("dve", self.cst(None, (i_, i_ + 1)), v_)
        self.c_eps = self.cst(None, (0, 1))
        self.c_eps4 = self.cst(None, (1, 2))
        self.c_ln16 = self.cst(None, (2, 3))
        self.c_one = self.cst(None, (3, 4))
        for v, name in ((self.ident, "k_ident"), (self.maskF, "k_maskF"), (self.maskB, "k_maskB"),
                        (self.bdmask, "k_bdmask")):
            self.dma("sp", v(), d[name])
        self.copy("dve", self.identb(), self.ident())
        self.memset("dve", self.ones(), 1.0)
        self.memset("dve", self.onesb(), 1.0)
        self.dma("sp", self.adab(), d["ada_bc"].rearrange("l p c -> p l c"))
        self.dma("sp", self.npre(), d["npre_c"].rearrange("l p c -> p l c"))
        cc = self.small
        for v in range(3):
            if v < self.NB:
                src = d["c"][v].rearrange("(k p) -> p k", p=128)
            elif v == 2:
                src = d["c_ctx"].rearrange("(k p) -> p k", p=128)
            else:
                src = d["c_ctx"].rearrange("(k p) -> p k", p=128)
            self.S.dma("sp", (lambda o, i: (lambda h: h.dma_start(out=o, in_=i, allow_slow_non_contiguous=True)))(
                craw(None, None, v).ap, src), writes=craw(None, None, v).k)
        self.act(th(), craw(), AF.Tanh, scale=0.5)
        self.stt(self.silc(), th(), 1.0, craw(), ALU.add, ALU.mult)
        self.ts("dve", self.silc(), self.silc(), 0.5, ALU.mult)
        cnt = 0
        for l in range(4):
            if l not in self.layers:
                continue
            for n in range(24):
                w = wst[cnt % 2]
                cnt += 1
                self.dma("sp", w(), d["ada_w"][l].rearrange("(k p) n -> p k n", p=128)[:, :, n * 128:(n + 1) * 128])
                pst = self.ps()
                for k in range(8):
                    self.mm(pst(None, (0, 3)), w(None, k), self.silc(None, k), start=(k == 0), stop=(k == 7))
                self.ts("dve", self.modcol(None, l, n), pst(None, (0, 3)), self.adab(None, l, (n, n + 1)), ALU.add)

    def layer(self, b, li):
        d = self.d
        A = self.arena
        L = Bump(A, self.layer_base, A.nbytes)
        self.L = L
        is_mla = (li % 2 == 0)
        j = li // 2
        need_ctx = li < 3
        first = (li == self.layers[0])
        def xsrc(t):
            if t < 2:
                src = d["ctx"][b] if first else d["ctxs"][b]
                return src[t * 128:(t + 1) * 128, :], ("dram_ctx", b, t)
            src = d["x"][b] if first else d["out"][b]
            return src[(t - 2) * 128:(t - 1) * 128, :], ("dram_x", b, t)

        def xdst(t):
            if t < 2:
                return d["ctxs"][b][t * 128:(t + 1) * 128, :], ("dram_ctx", b, t)
            return d["out"][b][(t - 2) * 128:(t - 1) * 128, :], ("dram_x", b, t)

        for kind, v in ((0, 2), (1, b)):
            sc = self.modcol(None, li, (8, 16), v)
            sh = self.modcol(None, li, (0, 8), v)
            gt = self.modcol(None, li, (16, 24), v)
            self.stt(self.acol(None, kind), sc, 1.0, self.npre(None, li), ALU.add, ALU.mult)
            self.copy("dve", self.bcol(None, kind), sh)
            self.copy("dve", self.gcol(None, kind), gt)
        import os
        if os.environ.get("KSKIP"):
            L.top += int(os.environ["KSKIP"]) * 1024
        self.xg = L.alloc([4, D], F32)
        if self.stop == "A_mod":
            return
        ntl = NT
        if self.stop in ("A_t0", "A_t0b", "A_t0c"):
            ntl = 1
        elif self.stop == "A_n2p":
            ntl = 2
        elif self.stop and self.stop.startswith("A_n"):
            ntl = int(self.stop[3:])
        tl0 = 0
        if self.stop == "A_only1":
            tl0, ntl = 1, 2
        if self.stop == "A_only2":
            tl0, ntl = 2, 3
        for t in range(tl0, ntl):
            if self.stop == "A_n2p":
                self.psrr = 0
            xs = self.xg(None, t % 4)
            src, dk = xsrc(t)
            self.dma("sp", xs, src, in_keys=[dk])
            self.act(self.junk(), xs, AF.Square, accum=self.ssq(None, (t, t + 1)))
            self.act(self.rr(None, (t, t + 1)), self.ssq(None, (t, t + 1)), AF.Ln, scale=1.0 / D, bias=self.c_eps)
            self.act(self.rr(None, (t, t + 1)), self.rr(None, (t, t + 1)), AF.Exp, scale=-0.5)
            import os
            step = int(os.environ.get("KSTEP", "9"))
            if self.stop == "A_t0" or step <= 0:
                continue
            self.ts("dve", xs, xs, self.rr(None, (t, t + 1)), ALU.mult)
            if self.stop == "A_t0b" or step <= 1:
                continue
            kind = 0 if t < 2 else 1
            for kq in range(2):
                pst = self.ps()
                for kk in range(4):
                    k = kq * 4 + kk
                    self.tr(pst(None, (kk * 128, kk * 128 + 128)), self.xg(None, t % 4, (k * 128, k * 128 + 128)),
                            self.ident())
                if step <= 2:
                    continue
                for kk in range(4):
                    k = kq * 4 + kk
                    o = self.hT(None, k, (t * 128, t * 128 + 128))
                    i = pst(None, (kk * 128, kk * 128 + 128))
                    if kq == 0:
                        self.act(o, i, AF.Identity, scale=self.acol(None, kind, (k, k + 1)),
                                 bias=self.bcol(None, kind, (k, k + 1)))
                    else:
                        self.ts("dve", o, i, self.acol(None, kind, (k, k + 1)), ALU.mult,
                                self.bcol(None, kind, (k, k + 1)), ALU.add)
        if self.stop in ("A_t0", "A_t0b", "A_t0c") or (self.stop and (self.stop.startswith("A_n") or self.stop.startswith("A_only"))):
            return
        if self.debug and "dbg_hT" in self.debug:
            tmpf = L.alloc([TA], F32)
            for k in range(8):
                self.copy("dve", tmpf(), self.hT(None, k))
                self.dma("sp", d["dbg_hT"][k * 128:(k + 1) * 128, :], tmpf())
            L.top = self.layer_base

        if self.stop in ("A", "A_nodbg"):
            return
        L.top = self.layer_base
        if is_mla:
            KC = 8
            self.mla(b, li, j, need_ctx)
        else:
            KC = 16
            self.mlstm(b, li, j, need_ctx)

        if self.stop in ("proj", "attn1"):
            return
        if is_mla:
            L.top = self.pt_end
            wout = L.alloc([8, D], BF16)
            for k4 in range(2):
                self.dma("pool", wout(None, (k4 * 4, k4 * 4 + 4)),
                         d["mla_wout"][j].rearrange("(k p) n -> p k n", p=128)[:, k4 * 4:(k4 + 1) * 4, :])
        else:
            L.top = self.layer_base
            wout = L.alloc([16, D], BF16)
            for k4 in range(4):
                self.dma("pool", wout(None, (k4 * 4, k4 * 4 + 4)),
                         d["ml_wout"][j].rearrange("(k p) n -> p k n", p=128)[:, k4 * 4:(k4 + 1) * 4, :])
            ptl = [L.alloc([16, 128], BF16) for _ in range(3)]
        xt = [L.alloc([D], F32) for _ in range(3)]
        t1 = [L.alloc([D], F32) for _ in range(3)]
        self.Gt = L.alloc([2, D], F32)
        self.npost_b = L.alloc([D], F32)
        self.dma("sp", self.npost_b(), d["npost"][li:li + 1, :].broadcast_to([128, D]))
        dg = L.alloc([128], F32)
        for kind in range(2):
            for hh in range(2):
                pst = self.ps()
                for kk in range(4):
                    k = hh * 4 + kk
                    self.ts("dve", dg(), self.ident(), self.gcol(None, kind, (k, k + 1)), ALU.mult)
                    self.mm(pst(None, (kk * 128, kk * 128 + 128)), self.ones(), dg())
                self.tt("dve", self.Gt(None, kind, (hh * 512, hh * 512 + 512)), pst(),
                        self.npost_b(None, (hh * 512, hh * 512 + 512)), ALU.mult)
        t_start = 0 if need_ctx else 2
        for t in range(t_start, NT):
            kind = 0 if t < 2 else 1
            if is_mla:
                lhs = lambda k: self.PT(None, k, (t * 128, t * 128 + 128))
            else:
                pl = ptl[t % 3]
                self.dma("sp", pl(), d["ptd"][t], in_keys=[("ptd", t)])
                lhs = lambda k: pl(None, k)
            y = self.ps(2)
            for hh in range(2):
                for k in range(KC):
                    self.mm(y(None, (hh * 512, hh * 512 + 512)), lhs(k), wout(None, k, (hh * 512, hh * 512 + 512)),
                            start=(k == 0), stop=(k == KC - 1))
            sq = self.ssq(None, (NT, NT + 1))
            r = self.rr(None, (NT, NT + 1))
            sq2 = self.ssq(None, (NT + 1, NT + 2))
            self.act(self.junk(None, (0, 512)), y(None, (0, 512)), AF.Square, accum=sq)
            self.act(self.junk(None, (512, 1024)), y(None, (512, 1024)), AF.Square, accum=sq2)
            self.tt("dve", sq, sq, sq2, ALU.add)
            self.act(r, sq, AF.Ln, scale=1.0 / D, bias=self.c_eps)
            self.act(r, r, AF.Exp, scale=-0.5)
            x_ = xt[t % 3]
            src, dk = xsrc(t)
            self.dma("sp", x_(), src, in_keys=[dk])
            tt_ = t1[t % 3]
            for hh in range(2):
                cs = (hh * 512, hh * 512 + 512)
                self.stt(tt_(None, cs), y(None, cs), r, self.Gt(None, kind, cs), ALU.mult, ALU.mult)
            self.tt("pool", x_(), x_(), tt_(), ALU.add)
            dst, dk = xdst(t)
            self.dma("pool", dst, x_(), out_keys=[dk])

    def mla(self, b, li, j, need_ctx):
        d = self.d
        L = self.L
        al = L.alloc
        self.PT = al([8, TA], BF16)
        self.pt_end = L.top
        cqn = al([2, TA], BF16)
        ckvn = al([2, TA], BF16)
        Qh = [al([TA], BF16) for _ in range(2)]
        Kh = [al([TA], BF16) for _ in range(2)]
        Vh = [al([NT, 128], BF16) for _ in range(2)]
        wuq = al([2, 2048], BF16)
        wukv = al([2, 2048], BF16)
        ws = [al([8, 256], BF16) for _ in range(3)]
        tq = al([TA], BF16)
        tks = [al([512], F32) for _ in range(4)]
        pexp = [al([1024], BF16) for _ in range(4)]
        rd = [al([512], F32) for _ in range(2)]
        thb = [al([512], F32) for _ in range(2)]
        sqb = [al([512], BF16) for _ in range(2)]
        rb = [al([512], F32) for _ in range(2)]
        qn = al([2], F32)
        kvn = al([2], F32)
        wuqc = [al([2, 128], BF16) for _ in range(2)]
        tmp_top = L.top

        win = d["mla_win"][j].rearrange("(k p) n -> p k n", p=128)
        self.dma("sp", qn(), d["mla_qn_c"][j])
        self.dma("sp", kvn(), d["mla_kvn_c"][j])
        self.dma("pool", tq(), d["k_tq"])
        self.dma("pool", wuq(), d["mla_wuq"][j].rearrange("(k p) n -> p k n", p=128))
        self.dma("pool", wukv(), d["mla_wukv"][j].rearrange("(k p) n -> p k n", p=128))
        for s in range(2):
            self.memset("dve", Vh[s](None, None, (64, 128)), 2.0)
        ntiles = [(0, 256)] + [(256 + 512 * i, 256 + 512 * (i + 1)) for i in range(4)]
        wsi = 0
        for gi, (dst, gn) in enumerate(((cqn, qn), (ckvn, kvn))):
            w = ws[wsi % 3]
            wsi += 1
            self.dma("pool", w(), win[:, :, gi * 256:(gi + 1) * 256])
            for ni, (n0, n1) in enumerate(ntiles):
                n = n1 - n0
                pc = [self.ps(), self.ps()]
                for cch in range(2):
                    for k in range(8):
                        self.mm(pc[cch](None, (0, n)), w(None, k, (cch * 128, cch * 128 + 128)),
                                self.hT(None, k, (n0, n1)), start=(k == 0), stop=(k == 7))
                pss = self.ps()
                for cch in range(2):
                    sq = sqb[cch]
                    self.act(sq(None, (0, n)), pc[cch](None, (0, n)), AF.Square)
                    self.mm(pss(None, (0, n)), self.onesb(), sq(None, (0, n)), start=(cch == 0), stop=(cch == 1))
                r = rb[ni % 2]
                self.act(r(None, (0, n)), pss(None, (0, n)), AF.Ln, scale=1.0 / 256, bias=self.c_eps)
                self.act(r(None, (0, n)), r(None, (0, n)), AF.Exp, scale=-0.5)
                for cch in range(2):
                    self.stt(dst(None, cch, (n0, n1)), pc[cch](None, (0, n)), gn(None, (cch, cch + 1)),
                             r(None, (0, n)), ALU.mult, ALU.mult)
        w = ws[wsi % 3]
        wsi += 1
        self.dma("pool", w(), win[:, :, 512:768])
        wo = ws[wsi % 3]
        wsi += 1
        self.dma("pool", wo(None, None, (0, 128)), win[:, :, 768:896])
        H1 = (64, 128)
        H0 = (0, 64)
        for ni, (n0, n1) in enumerate(ntiles):
            n = n1 - n0
            pa = self.ps()
            pb = self.ps()
            wa = wo if ni == 0 else w
            for k in range(8):
                self.mm(pa(None, (0, n)), wa(None, k, (0, 128)), self.hT(None, k, (n0, n1)), start=(k == 0), stop=(k == 7))
            for k in range(8):
                self.mm(pb(None, (0, n)), w(None, k, (128, 256)), self.hT(None, k, (n0, n1)), start=(k == 0), stop=(k == 7))
            tc_ = tks[(2 * ni) % 4]
            ts_ = tks[(2 * ni + 1) % 4]
            self.dma("sp", tc_(None, (0, n)), d["k_tkc"][:, n0:n1])
            self.dma("sp", ts_(None, (0, n)), d["k_tks"][:, n0:n1])
            u = thb[0]
            v = thb[1]
            self.tt("dve", u(H1, (0, n)), pa(H1, (0, n)), tc_(H1, (0, n)), ALU.mult)
            self.tt("dve", v(H1, (0, n)), pb(H1, (0, n)), ts_(H1, (0, n)), ALU.mult)
            self.tt("pool", Kh[0](H1, (n0, n1)), u(H1, (0, n)), v(H1, (0, n)), ALU.add)
            self.copy("pool", Kh[1](H1, (n0, n1)), Kh[0](H1, (n0, n1)))
        for gc in range(8):
            if gc % 2 == 0:
                w = ws[wsi % 3]
                wsi += 1
                self.dma("pool", w(), win[:, :, 896 + gc * 128: 896 + gc * 128 + 256])
            co = (gc % 2) * 128
            for ni, (n0, n1) in enumerate(ntiles):
                n = n1 - n0
                pg = self.ps()
                for k in range(8):
                    self.mm(pg(None, (0, n)), w(None, k, (co, co + 128)), self.hT(None, k, (n0, n1)),
                            start=(k == 0), stop=(k == 7))
                th = thb[ni % 2]
                self.act(th(None, (0, n)), pg(None, (0, n)), AF.Tanh, scale=0.5)
                self.stt(self.PT(None, gc, (n0, n1)), th(None, (0, n)), 1.0, pg(None, (0, n)), ALU.add, ALU.mult)
        self.dump("dbg_cqn", cqn, 2)
        self.dump("dbg_ckvn", ckvn, 2)
        self.dump("dbg_SG", self.PT, 8)
        if self.stop == "proj":
            return
        qchunks = ([(0, 256, 2)] if need_ctx else []) + [(256 + 512 * i, 256 + 512 * (i + 1), NT) for i in range(4)]
        acnt = [0]
        nheads = 1 if self.stop == "attn1" else NH

        def proj(h):
                s = h % 2
                Q = Qh[s]
                K = Kh[s]
                V = Vh[s]
                wc_ = wuqc[s]
                self.dma("pool", wc_(), d["mla_wuq_c"][j].rearrange("(k p) n -> p k n", p=128)[:, :, h * 128:(h + 1) * 128])
                pj = [self.psbank[i_] for i_ in range(6)]
                pjc = 0
                for ni, (n0, n1) in enumerate(ntiles):
                    n = n1 - n0
                    pq = pj[pjc % 6]
                    pjc += 1
                    for k in range(2):
                        lq = wc_(None, k) if ni == 0 else wuq(None, k, (h * 128, h * 128 + 128))
                        self.mm(pq(None, (0, n)), lq, cqn(None, k, (n0, n1)),
                                start=(k == 0), stop=(k == 1))
                    self.copy("dve", Q(H0, (n0, n1)), pq(H0, (0, n)))
                    self.tt("dve", Q(H1, (n0, n1)), pq(H1, (0, n)), tq(H1, (n0, n1)), ALU.mult)
                    pk = pj[pjc % 6]
                    pjc += 1
                    for k in range(2):
                        self.mm(pk(H0, (0, n)), wukv(None, k, (h * 128, h * 128 + 64)), ckvn(None, k, (n0, n1)),
                                start=(k == 0), stop=(k == 1))
                    self.copy("dve", K(H0, (n0, n1)), pk(H0, (0, n)))
                for g0 in range(0, NT, 8):
                    g1 = min(NT, g0 + 8)
                    pv = pj[pjc % 6]
                    pjc += 1
                    for t in range(g0, g1):
                        for k in range(2):
                            self.mm(pv(None, ((t - g0) * 64, (t - g0) * 64 + 64)), ckvn(None, k, (t * 128, t * 128 + 128)),
                                    wukv(None, k, (h * 128 + 64, h * 128 + 128)), start=(k == 0), stop=(k == 1))
                    ng = g1 - g0
                    src = X(pv.ap[:, 0:ng * 64].rearrange("p (t c) -> p t c", c=64), pv(None, (0, ng * 64)).k)
                    self.copy("dve", V(None, (g0, g1), (0, 64)), src)

        proj(0)
        for h in range(nheads):
            s = h % 2
            Q = Qh[s]
            K = Kh[s]
            V = Vh[s]
            if h == 0:
                self.dump("dbg_Q0", Q)
                self.dump("dbg_K0", K)
            hp = (h % 2) * 64
            HP = (hp, hp + 64)
            for qi, (q0, q1, nk) in enumerate(qchunks):
                nq = q1 - q0
                acc = self.psbank[6 + (acnt[0] % 2)]
                acnt[0] += 1
                def scores_exp(kt):
                    sp_ = self.pspair[(kt // 2) % 3]
                    for u2 in range(2):
                        self.mm(sp_(None, (u2 * 512, u2 * 512 + nq)), K(None, ((kt + u2) * 128, (kt + u2) * 128 + 128)),
                                Q(None, (q0, q1)))
                    pe_ = pexp[(kt // 2) % 4]
                    if nq == 512:
                        self.act(pe_(), sp_(), AF.Exp, scale=ATT_SCALE)
                    else:
                        for u2 in range(2):
                            self.act(pe_(None, (u2 * 512, u2 * 512 + nq)), sp_(None, (u2 * 512, u2 * 512 + nq)),
                                     AF.Exp, scale=ATT_SCALE)

                def pv(kt):
                    pe_ = pexp[(kt // 2) % 4]
                    for u2 in range(2):
                        self.mm(acc(None, (0, nq)), V(None, kt + u2), pe_(None, (u2 * 512, u2 * 512 + nq)),
                                start=(kt + u2 == 0), stop=(kt + u2 == nk - 1))

                if qi == 2 and h + 1 < nheads:
                    proj(h + 1)
                kts = list(range(0, nk, 2))
                scores_exp(kts[0])
                if len(kts) > 1:
                    scores_exp(kts[1])
                for i_, kt in enumerate(kts):
                    if i_ + 2 < len(kts):
                        scores_exp(kts[i_ + 2])
                    pv(kt)
                r = rd[qi % 2]
                self.recip(r(HP, (0, nq)), acc(H1, (0, nq)))
                ptv = self.PT(HP, h // 2, (q0, q1))
                self.tt("pool", r(HP, (0, nq)), r(HP, (0, nq)), ptv, ALU.mult)
                if hp == 0:
                    self.tt("dve", ptv, acc(H0, (0, nq)), r(HP, (0, nq)), ALU.mult)
                else:
                    r2 = thb[qi % 2]
                    self.copy("act", r2(HP, (0, nq)), acc(H0, (0, nq)))
                    self.tt("pool", ptv, r2(HP, (0, nq)), r(HP, (0, nq)), ALU.mult)
        self.dump("dbg_PT", self.PT, 8)

    def mlstm(self, b, li, j, need_ctx):
        d = self.d
        L = self.L
        al = L.alloc
        XW = 2312
        ntiles = [(0, 256)] + [(256 + 512 * i, 256 + 512 * (i + 1)) for i in range(4)]

        def PO(n0):
            return 2 + n0 if n0 < 256 else 260 + (n0 - 256)

        cw = al([16, 5], F32)
        cb = al([16], F32)
        hbc = al([16], F32)
        hgc = al([16], F32)
        skc = al([16], F32)
        wsm = [al([16, 4], F32) for _ in range(6)]
        BD = [al([16, 128], BF16) for _ in range(3)]
        Wc = al([16, 32], BF16)
        Wm = al([16, 32], BF16)
        bif = al([1], F32)
        gtok = al([NT, 32], F32)
        spc = al([NT * 8], F32)
        tmpg = al([NT * 8], F32)
        alpha = [al([NT, 8], F32) for _ in range(2)]
        ib = [al([NT, 8], F32) for _ in range(2)]
        decay = [al([NT, 8], F32) for _ in range(2)]
        aend = [al([NT, 8], F32) for _ in range(2)]
        C32 = [al([2, 260], F32) for _ in range(2)]
        Cbf = [al([2, 260], BF16) for _ in range(2)]
        AT = [al([128], BF16) for _ in range(4)]
        khat = [al([256], BF16) for _ in range(4)]
        mcol = [al([8], F32) for _ in range(4)]
        dgc = [al([5, 128], BF16) for _ in range(1)]
        tA = [al([512], F32) for _ in range(2)]
        tB = [al([512], F32) for _ in range(2)]
        ssqh = al([NT], F32)
        rrh = al([NT], F32)
        hnb = [L.a.view(tB[i_].off, [256], BF16) for i_ in range(2)]
        preb = [al([2, 128], BF16) for _ in range(2)]
        utmp = [L.a.view(tA[i_].off, [128], F32) for i_ in range(2)]
        wxm = [al([8, 256], BF16) for _ in range(2)]
        wzo = [al([8, 512], BF16) for _ in range(2)]
        B1 = al([2, XW], BF16)
        B2 = al([2, TA], BF16)
        B3 = al([2, TA], BF16)
        B4 = al([2, TA], BF16)
        B5 = al([NT, 256], BF16)
        B6 = al([NT, 258], BF16)
        HF = al([NT, 256], BF16)
        HBv = al([NT, 256], BF16)
        gT = L.a.view(HF.off, [TA], F32)
        setup_top = L.top

        self.dma("sp", cw(), d["ml_cw_c"][j])
        self.dma("sp", cb(), d["ml_cb_c"][j])
        self.dma("sp", hgc(), d["ml_hn_c"][j])
        self.dma("sp", skc(), d["ml_sk_c"][j])
        self.dma("sp", bif((0, 32)), d["ml_bif_c"][j])
        for i, nm in enumerate(("ml_wq_c", "ml_wk_c", "ml_wv_c", "ml_wqT_c", "ml_wkT_c", "ml_wvT_c")):
            self.dma("sp", wsm[i](), d[nm][j])
        self.ts("dve", hbc(), cb(), 0.5, ALU.mult)
        self.ts("dve", hgc(), hgc(), 0.5, ALU.mult)
        self.ts("dve", skc(), skc(), 0.5, ALU.mult)

        def bd_build(out_x, wv, jj):
            w_ap = wv(None, jj).ap.unsqueeze(1).broadcast_to([128, 32, 4])
            m_ap = self.bdmask().ap.rearrange("p (m d) -> p m d", d=4)
            o_ap = out_x.ap.rearrange("p (m d) -> p m d", d=4)
            self.tt("dve", X(o_ap, out_x.k), X(w_ap, wv(None, jj).k), X(m_ap, self.bdmask().k), ALU.mult)

        for jj in range(16):
            for i in range(3):
                bd_build(BD[i](None, jj), wsm[i], jj)
        L.top = B3.off
        wif = L.alloc([48, 32], F32)
        for dd in range(2):
            for q4 in range(4):
                self.dma("sp", wif(None, (q4 * 12, q4 * 12 + 12), (dd * 16, dd * 16 + 16)),
                         d["ml_wif"][j][dd].rearrange("(k p) g -> p k g", p=128)[:, q4 * 12:(q4 + 1) * 12, :])
        bdt = [L.alloc([128], F32) for _ in range(3)]
        for jj in range(16):
            for i in range(3):
                bd_build(bdt[i](), wsm[3 + i], jj)
            p1 = self.ps()
            self.mm(p1(None, (0, 32)), bdt[0](), wif(None, jj), start=True, stop=False)
            self.mm(p1(None, (0, 32)), bdt[1](), wif(None, 16 + jj), start=False, stop=True)
            self.copy("dve", Wc(None, jj), p1(None, (0, 32)))
            p2 = self.ps()
            self.mm(p2(None, (0, 32)), bdt[2](), wif(None, 32 + jj))
            self.copy("dve", Wm(None, jj), p2(None, (0, 32)))
        L.top = setup_top
        for cc in range(2):
            self.memset("dve", B1(None, cc, (0, 2)), 0.0)
            self.memset("dve", B1(None, cc, (258, 260)), 0.0)
            self.memset("dve", B1(None, cc, (2308, XW)), 0.0)
        self.memset("dve", B6(None, None, (256, 258)), 1.0)

        win = d["ml_win"][j].rearrange("(k p) n -> p k n", p=128)
        dcnt = [0]
        pending = []

        def features(w, cc, jj, gates_first):
            for ni, (n0, n1) in enumerate(ntiles):
                n = n1 - n0
                pm = self.ps()
                for k in range(8):
                    self.mm(pm(None, (0, n)), w(None, k, (cc * 128, cc * 128 + 128)), self.hT(None, k, (n0, n1)),
                            start=(k == 0), stop=(k == 7))
                self.copy("act", B1(None, cc, (PO(n0), PO(n0) + n)), pm(None, (0, n)))
            while pending:
                pending.pop(0)()
            dg = dgc[0]
            dcnt[0] += 1
            for tap in range(5):
                self.ts("dve", dg(None, tap), self.identb(), cw(None, jj, (tap, tap + 1)), ALU.mult)
            for ni, (n0, n1) in enumerate(ntiles):
                n = n1 - n0
                pc = self.ps()
                for tap in range(5):
                    c0 = PO(n0) + tap - 2
                    self.mm(pc(None, (0, n)), dg(None, tap), B1(None, cc, (c0, c0 + n)),
                            start=(tap == 0), stop=(tap == 4))
                ta = tA[ni % 2]
                tb = tB[ni % 2]
                self.ts("dve", ta(None, (0, n)), pc(None, (0, n)), 0.5, ALU.mult, hbc(None, (jj, jj + 1)), ALU.add)
                self.act(tb(None, (0, n)), pc(None, (0, n)), AF.Tanh, scale=0.5, bias=hbc(None, (jj, jj + 1)))
                self.stt(B2(None, cc, (n0, n1)), tb(None, (0, n)), 1.0, ta(None, (0, n)), ALU.add, ALU.mult)
            if gates_first is not None:
                def gates(cc=cc, jj=jj, gates_first=gates_first):
                    for ni, (n0, n1) in enumerate(ntiles):
                        n = n1 - n0
                        pg = self.ps()
                        R32 = (0, 32)
                        self.mm(pg(R32, (0, n)), Wc(None, jj), B2(None, cc, (n0, n1)), start=True, stop=False)
                        self.mm(pg(R32, (0, n)), Wm(None, jj), B1(None, cc, (PO(n0), PO(n0) + n)), start=False, stop=True)
                        if gates_first:
                            self.ts("dve", gT(R32, (n0, n1)), pg(R32, (0, n)), bif(R32), ALU.add)
                        else:
                            self.tt("dve", gT(R32, (n0, n1)), pg(R32, (0, n)), gT(R32, (n0, n1)), ALU.add)
                pending.append(gates)

        wcnt = 0
        for hh in range(8):
            w = wxm[wcnt % 2]
            wcnt += 1
            self.dma("pool", w(), win[:, :, hh * 256:(hh + 1) * 256])
            for cc in range(2):
                features(w, cc, hh * 2 + cc, gates_first=(hh == 0 and cc == 0))
        while pending:
            pending.pop(0)()
        R32 = (0, 32)
        pgA = self.ps()
        pgB = self.ps()
        for c in range(NT):
            dst = pgA(None, (c * 32, c * 32 + 32)) if c < 16 else pgB(None, ((c - 16) * 32, (c - 16) * 32 + 32))
            idn = X(self.ident.ap[0:32, 0:32], self.ident().k)
            self.tr(dst, gT(R32, (c * 128, c * 128 + 128)), idn)
        self.copy("dve", X(gtok.ap[:, 0:16, :], gtok(None, (0, 16)).k),
                  X(pgA.ap.rearrange("p (c g) -> p c g", g=32), pgA().k))
        self.copy("dve", X(gtok.ap[:, 16:18, :], gtok(None, (16, 18)).k),
                  X(pgB.ap[:, 0:64].rearrange("p (c g) -> p c g", g=32), pgB(None, (0, 64)).k))
        for dd in range(2):
            li_x = X(gtok.ap[:, :, dd * 16:dd * 16 + 8], gtok().k)
            fp_x = X(gtok.ap[:, :, dd * 16 + 8:dd * 16 + 16], gtok().k)
            sp3 = X(spc.ap.rearrange("p (c g) -> p c g", g=8), spc().k)
            tm3 = X(tmpg.ap.rearrange("p (c g) -> p c g", g=8), tmpg().k)
            self.act(sp3, fp_x, AF.Exp, scale=-1.0)
            self.act(spc(), spc(), AF.Ln, bias=self.c_one)
            tri = self.maskF if dd == 0 else self.maskB
            pcum = self.ps()
            ptot = self.ps()
            NG = NT * 8
            self.mm(pcum(None, (0, NG)), tri(), spc())
            self.mm(ptot(None, (0, NG)), self.ones(), spc())
            cum3 = X(pcum.ap[:, 0:NG].rearrange("p (c g) -> p c g", g=8), pcum(None, (0, NG)).k)
            self.tt("dve", tm3, cum3, li_x, ALU.add)
            self.act(alpha[dd](), tm3, AF.Exp)
            self.act(ib[dd](), cum3, AF.Exp, bias=self.c_ln16)
            tot3 = X(ptot.ap[:, 0:NG].rearrange("p (c g) -> p c g", g=8), ptot(None, (0, NG)).k)
            self.act(decay[dd](), tot3, AF.Exp, scale=-1.0)
            self.tt("pool", aend[dd](), decay[dd](), alpha[dd](), ALU.mult)

        order = [list(range(NT)), [1, 0] + list(range(NT - 1, 1, -1))]
        c_lo = 0 if need_ctx else 2
        slot = [0]
        def load_head_w(h2):
            w_ = wxm[(wcnt0 + h2) % 2]
            self.dma("pool", w_(), win[:, :, h2 * 256:(h2 + 1) * 256])
            wz_ = wzo[h2 % 2]
            self.dma("pool", wz_(None, None, (0, 256)), win[:, :, 2048 + h2 * 256: 2048 + (h2 + 1) * 256])
            self.dma("pool", wz_(None, None, (256, 512)), win[:, :, 4096 + h2 * 256: 4096 + (h2 + 1) * 256])

        wcnt0 = wcnt
        load_head_w(0)
        for hh in range(8):
            w = wxm[(wcnt0 + hh) % 2]
            wz = wzo[hh % 2]
            for cc in range(2):
                features(w, cc, hh * 2 + cc, gates_first=None)
            for cc in range(2):
                jj = hh * 2 + cc
                for (dstB, bd) in ((B3, BD[0]), (B4, BD[1])):
                    for ni, (n0, n1) in enumerate(ntiles):
                        n = n1 - n0
                        pq = self.ps()
                        self.mm(pq(None, (0, n)), bd(None, jj), B2(None, cc, (n0, n1)))
                        if ni % 2 == 0:
                            self.copy("act", dstB(None, cc, (n0, n1)), pq(None, (0, n)))
                        else:
                            self.copy("dve", dstB(None, cc, (n0, n1)), pq(None, (0, n)))
            for c in range(0, NT, 2):
                pk = self.ps()
                pv = self.ps()
                for u in range(2):
                    for cc in range(2):
                        jj = hh * 2 + cc
                        t0 = (c + u) * 128
                        self.mm(pk(None, (u * 256 + cc * 128, u * 256 + cc * 128 + 128)),
                                B2(None, cc, (t0, t0 + 128)), BD[1](None, jj))
                        self.mm(pv(None, (u * 256 + cc * 128, u * 256 + cc * 128 + 128)),
                                B1(None, cc, (PO(t0), PO(t0) + 128)), BD[2](None, jj))
                self.copy("act", B5(None, (c, c + 2)), X(pk.ap.rearrange("p (u f) -> p u f", f=256), pk().k))
                self.copy("dve", B6(None, (c, c + 2), (0, 256)), X(pv.ap.rearrange("p (u f) -> p u f", f=256), pv().k))
            for cc in range(2):
                self.ts("dve", B2(None, cc), B2(None, cc), skc(None, (hh * 2 + cc, hh * 2 + cc + 1)), ALU.mult)
            def front(step, dd):
                c = order[dd][step]
                sl = (step % 2) * 2 + dd
                t0 = c * 128
                pS = self.psbank[dd]
                for e in range(2):
                    self.mm(pS(None, (0, 128)), B4(None, e, (t0, t0 + 128)), B3(None, e, (t0, t0 + 128)),
                            start=(e == 0), stop=(e == 1))
                msk = self.maskF if dd == 0 else self.maskB
                self.stt(AT[sl](), pS(None, (0, 128)), alpha[dd](None, c, (hh, hh + 1)), msk(), ALU.mult, ALU.mult)
                self.act(khat[sl](), B5(None, c), AF.Copy, scale=aend[dd](None, c, (hh, hh + 1)))

            def back(step, dd):
                c = order[dd][step]
                first = (step == 0)
                sl = (step % 2) * 2 + dd
                t0 = c * 128
                po = self.psbank[2 + dd]
                self.mm(po(None, (0, 257)), AT[sl](), B6(None, c, (0, 257)), start=True, stop=first)
                if not first:
                    for e in range(2):
                        self.mm(po(None, (0, 257)), B3(None, e, (t0, t0 + 128)), Cbf[dd](None, e, (0, 257)),
                                start=False, stop=(e == 1))
                if step < NT - 1:
                    pU = self.pspair[2 + dd]
                    for e in range(2):
                        self.mm(pU(None, (e * 512, e * 512 + 257)), khat[sl](None, (e * 128, e * 128 + 128)),
                                B6(None, c, (0, 257)))
                    u3 = X(pU.ap.rearrange("p (e f) -> p e f", f=512)[:, :, 0:257], pU().k)
                    c3 = X(C32[dd].ap[:, :, 0:257], C32[dd]().k)
                    if first:
                        self.copy("dve", c3, u3)
                    else:
                        self.stt(c3, c3, decay[dd](None, c, (hh, hh + 1)), u3, ALU.mult, ALU.add)
                    self.copy("pool", Cbf[dd](), C32[dd]())
                if c >= c_lo:
                    m = mcol[sl]
                    self.act(m(None, (0, 1)), po(None, (256, 257)), AF.Abs)
                    self.ts("dve", m(None, (0, 1)), m(None, (0, 1)), ib[dd](None, c, (hh, hh + 1)), ALU.max)
                    self.recip(m(None, (1, 2)), m(None, (0, 1)))
                    hdst = HF if dd == 0 else HBv
                    self.act(hdst(None, c), po(None, (0, 256)), AF.Copy, scale=m(None, (1, 2)))

            if hh + 1 < 8:
                load_head_w(hh + 1)
            front(0, 0)
            front(0, 1)
            for step in range(NT):
                for dd in range(2):
                    if step + 1 < NT:
                        front(step + 1, dd)
                    back(step, dd)
            for c in range(c_lo, NT):
                t0 = c * 128
                pog = self.ps()
                for k in range(8):
                    self.mm(pog(None, (0, 256)), self.hT(None, k, (t0, t0 + 128)), wz(None, k, (256, 512)),
                            start=(k == 0), stop=(k == 7))
                ta = tA[c % 2]
                tb = tB[c % 2]
                self.act(ta(None, (0, 256)), pog(None, (0, 256)), AF.Tanh, scale=0.5)
                self.tt("pool", tb(None, (0, 256)), HF(None, c), HBv(None, c), ALU.add)
                self.stt(HF(None, c), ta(None, (0, 256)), 1.0, tb(None, (0, 256)), ALU.add, ALU.mult)
                self.act(self.junk(None, (0, 256)), HF(None, c), AF.Square, accum=ssqh(None, (c, c + 1)))
            for cc in range(2):
                for ni, (n0, n1) in enumerate(ntiles):
                    n = n1 - n0
                    if n1 <= 256 and not need_ctx:
                        continue
                    pz = self.ps()
                    for k in range(8):
                        self.mm(pz(None, (0, n)), wz(None, k, (cc * 128, cc * 128 + 128)), self.hT(None, k, (n0, n1)),
                                start=(k == 0), stop=(k == 7))
                    tb = tB[ni % 2]
                    self.act(tb(None, (0, n)), pz(None, (0, n)), AF.Tanh, scale=0.5)
                    self.stt(B4(None, cc, (n0, n1)), tb(None, (0, n)), 1.0, pz(None, (0, n)), ALU.add, ALU.mult)
            self.act(rrh(None, (c_lo, NT)), ssqh(None, (c_lo, NT)), AF.Ln, scale=1.0 / 256, bias=self.c_eps4)
            self.act(rrh(None, (c_lo, NT)), rrh(None, (c_lo, NT)), AF.Exp, scale=-0.5)
            for c in range(c_lo, NT):
                t0 = c * 128
                hn = hnb[c % 2]
                self.act(hn(), HF(None, c), AF.Copy, scale=rrh(None, (c, c + 1)))
                pT = self.psbf[(self.psrr) % 8]
                self.psrr += 1
                for cc in range(2):
                    self.tr(pT(None, (cc * 128, cc * 128 + 128)), hn(None, (cc * 128, cc * 128 + 128)), self.identb())
                pre = preb[c % 2]
                for cc in range(2):
                    jj = hh * 2 + cc
                    u = utmp[cc]
                    self.stt(u(), pT(None, (cc * 128, cc * 128 + 128)), hgc(None, (jj, jj + 1)),
                             B2(None, cc, (t0, t0 + 128)), ALU.mult, ALU.add)
                    self.tt("pool", pre(None, cc), u(), B4(None, cc, (t0, t0 + 128)), ALU.mult)
                self.dma("sp", d["ptd"][c][:, hh * 2:hh * 2 + 2, :], pre(), out_keys=[("ptd", c)])


def _rope_tables():
    rows = T_LAT // 64
    row = np.repeat(np.arange(rows), 64).astype(np.float32)
    col = np.tile(np.arange(64), rows).astype(np.float32)
    inv = (10000.0 ** (-np.arange(8, dtype=np.float32) / 8)).astype(np.float32)
    cr = np.cos(row[:, None] * inv)
    sr = np.sin(row[:, None] * inv)
    cc = np.cos(col[:, None] * inv)
    sc = np.sin(col[:, None] * inv)
    C = np.concatenate([cr, cr, cc, cc], axis=1).T
    Sg = np.concatenate([-sr, sr, -sc, sc], axis=1).T
    Cf = np.concatenate([np.ones((32, T_CTX), np.float32), C], axis=1).astype(np.float32)
    Sf = np.concatenate([np.zeros((32, T_CTX), np.float32), Sg], axis=1).astype(np.float32)
    tq = np.concatenate([Cf, Sf, Cf, Sf], axis=0)
    tkc = np.concatenate([Cf] * 4, axis=0)
    tks = np.concatenate([Sf] * 4, axis=0)
    return np.ascontiguousarray(tq), np.ascontiguousarray(tkc), np.ascontiguousarray(tks)


PERM_A = np.arange(32)
PERM_B = np.concatenate([np.arange(8, 16), np.arange(0, 8), np.arange(24, 32), np.arange(16, 24)])


def _col(v, nch):
    s = v.shape[:-1]
    return np.ascontiguousarray(np.swapaxes(v.reshape(s + (nch, 128)), -1, -2))


def prep_shared(inp):
    f = lambda a: np.ascontiguousarray(np.asarray(a, dtype=np.float32))
    o = {}
    o["c_ctx"] = f(inp["c_ctx"])
    o["ada_w"] = f(inp["ada_w"])
    o["ada_bc"] = _col(f(inp["ada_b"]), 24)
    o["npre_c"] = _col(f(inp["norm_pre"]), 8)
    o["npost"] = f(inp["norm_post"])
    w = f(inp["mla_w_in"])
    kr = w[:, :, 512:544]
    krA = np.tile(kr[:, :, PERM_A], (1, 1, 4))
    krB = np.tile(kr[:, :, PERM_B], (1, 1, 4))
    krO = np.tile(kr, (1, 1, 4))
    o["mla_win"] = np.ascontiguousarray(np.concatenate([w[:, :, 0:512], krA, krB, krO, w[:, :, 544:]], axis=2))
    o["mla_qn_c"] = _col(f(inp["mla_q_norm"]), 2)
    o["mla_kvn_c"] = _col(f(inp["mla_kv_norm"]), 2)
    wq = f(inp["mla_w_uq"]).reshape(2, 256, NH, 96)
    rope = wq[..., 64:]
    o["mla_wuq"] = np.ascontiguousarray(
        np.concatenate([wq[..., :64], rope[..., PERM_A], rope[..., PERM_B]], axis=-1).reshape(2, 256, 2048))
    o["mla_wuq_c"] = np.ascontiguousarray(
        np.concatenate([wq[..., :64], rope, rope], axis=-1).reshape(2, 256, 2048))
    o["mla_wukv"] = f(inp["mla_w_ukv"])
    o["mla_wout"] = f(inp["mla_w_out"])
    o["ml_win"] = f(inp["ml_w_in"])
    o["ml_cw_c"] = np.ascontiguousarray(np.transpose(f(inp["ml_conv_w"]).reshape(2, 5, 16, 128), (0, 3, 2, 1)))
    o["ml_cb_c"] = _col(f(inp["ml_conv_b"]), 16)
    for nm, key in (("ml_wq", "ml_w_q"), ("ml_wk", "ml_w_k"), ("ml_wv", "ml_w_v")):
        wqk = f(inp[key])
        a = wqk.reshape(2, 16, 32, 4, 4)
        o[nm + "_c"] = np.ascontiguousarray(np.transpose(a, (0, 2, 3, 1, 4)).reshape(2, 128, 16, 4))
        o[nm + "T_c"] = np.ascontiguousarray(np.transpose(a, (0, 2, 4, 1, 3)).reshape(2, 128, 16, 4))
    o["ml_wif"] = f(inp["ml_w_if"])
    o["ml_bif_c"] = np.ascontiguousarray(f(inp["ml_b_if"]).reshape(2, 32, 1))
    o["ml_hn_c"] = _col(f(inp["ml_head_norm"]), 16)
    o["ml_sk_c"] = _col(f(inp["ml_skip"]), 16)
    o["ml_wout"] = f(inp["ml_w_out"])
    o["k_ident"] = np.eye(128, dtype=np.float32)
    o["k_maskF"] = np.triu(np.ones((128, 128), np.float32))
    o["k_maskB"] = np.tril(np.ones((128, 128), np.float32))
    o["k_bdmask"] = np.kron(np.eye(32, dtype=np.float32), np.ones((4, 4), np.float32))
    o["k_tq"], o["k_tkc"], o["k_tks"] = _rope_tables()
    return o


_CACHE = {}


def kernel(**inputs):
    n_cores = 8
    NB = 2
    key = ("full",)
    if key not in _CACHE:
        _CACHE[key] = Prog(NB, [0, 1, 2, 3]).build()
    nc = _CACHE[key]
    shared = prep_shared(inputs)
    x = np.asarray(inputs["x"], dtype=np.float32)
    c = np.asarray(inputs["c"], dtype=np.float32)
    ctx = np.asarray(inputs["ctx"], dtype=np.float32)
    in_maps = []
    for i in range(n_cores):
        m = dict(shared)
        m["x"] = np.ascontiguousarray(x[i * NB:(i + 1) * NB])
        m["c"] = np.ascontiguousarray(c[i * NB:(i + 1) * NB])
        m["ctx"] = np.ascontiguousarray(ctx[i * NB:(i + 1) * NB])
        in_maps.append(m)
    res = run_bass_kernel_spmd(nc, in_maps, core_ids=list(range(n_cores)))
    return np.concatenate([np.asarray(r["out"]) for r in res.results], axis=0).astype(np.float32)
```

```python
import math
from contextlib import ExitStack

import numpy as np
import concourse.bass as bass
import concourse.mybir as mybir
from concourse.bass_utils import run_bass_kernel_spmd

F32 = mybir.dt.float32
BF16 = mybir.dt.bfloat16
ALU = mybir.AluOpType
AF = mybir.ActivationFunctionType
AX = mybir.AxisListType

D = 1024
T_LAT = 2048
T_CTX = 256
TA = T_LAT + T_CTX
NT = TA // 128
EPS = 1e-6
NH = 16
ATT_SCALE = 96 ** -0.5
LN16 = math.log(16.0)

EPOCH = 4096
NSLOT = 8
COMPUTE = ("pe", "act", "dve", "pool")
ENGINES = ("pe", "act", "dve", "pool", "sp")


class Op:
    __slots__ = ("eng", "fn", "idx", "is_dma", "signal", "sem", "val", "waits",
                 "eidx", "slot", "prev_slot_wait")


class Sched:
    def __init__(self):
        self.ops = []
        self.by_eng = {e: [] for e in ENGINES}
        self.last_writer = {}
        self.readers = {}
        self.waited = {}
        self.dma_waited = {e: set() for e in ENGINES}
        self.dma_count = {e: 0 for e in ENGINES}
        self.slot_last = {}

    def _add(self, eng, fn, reads, writes, is_dma):
        op = Op()
        op.eng = eng
        op.fn = fn
        op.is_dma = is_dma
        op.signal = is_dma
        op.sem = None
        op.val = None
        op.waits = []
        op.slot = None
        op.prev_slot_wait = None
        op.idx = len(self.ops)
        op.eidx = len(self.by_eng[eng])
        self.ops.append(op)
        self.by_eng[eng].append(op)
        psr = [k for k in reads if k[0] == "ps"]
        if psr:
            writes = list(writes) + psr
        lw = self.last_writer
        rd = self.readers
        deps = set()
        same_raw = None
        for k in reads:
            w = lw.get(k)
            if w is not None:
                deps.add(w)
                if w.eng == eng and eng != "pe" and not w.is_dma:
                    if same_raw is None or w.eidx > same_raw.eidx:
                        same_raw = w
        for k in writes:
            w = lw.get(k)
            if w is not None:
                deps.add(w)
            r = rd.get(k)
            if r:
                deps.update(r)
        best = {}
        for d in deps:
            if d is op:
                continue
            if d.is_dma:
                if d.idx not in self.dma_waited[eng]:
                    self.dma_waited[eng].add(d.idx)
                    op.waits.append(d)
            else:
                if d.eng == eng:
                    continue
                b = best.get(d.eng)
                if b is None or d.eidx > b.eidx:
                    best[d.eng] = d
        if same_raw is not None and not is_dma:
            best[eng] = same_raw
        for pe, d in best.items():
            key = (eng, pe)
            if self.waited.get(key, -1) >= d.eidx:
                continue
            self.waited[key] = d.eidx
            d.signal = True
            op.waits.append(d)
        if is_dma:
            n = self.dma_count[eng]
            self.dma_count[eng] = n + 1
            op.slot = (eng, n % NSLOT)
            prev = self.slot_last.get(op.slot)
            if prev is not None and prev.idx not in self.dma_waited[eng]:
                self.dma_waited[eng].add(prev.idx)
                op.prev_slot_wait = prev
            self.slot_last[op.slot] = op
        for k in reads:
            r = rd.get(k)
            if r is None:
                rd[k] = [op]
            else:
                if not op.is_dma:
                    r[:] = [x for x in r if x.is_dma or x.eng != eng]
                r.append(op)
        for k in writes:
            lw[k] = op
            rd[k] = []
        return op

    def op(self, eng, fn, reads=(), writes=()):
        return self._add(eng, fn, reads, writes, False)

    def dma(self, eng, fn, reads=(), writes=()):
        return self._add(eng, fn, reads, writes, True)

    def emit(self, nc, stack):
        sems = {}

        def get_sem(name):
            if name not in sems:
                sems[name] = stack.enter_context(nc.semaphore(name))
            return sems[name]

        for e in COMPUTE:
            cnt = 0
            for op in self.by_eng[e]:
                if op.is_dma or not op.signal:
                    continue
                op.sem = get_sem("s_%s_%d" % (e, cnt // EPOCH))
                op.val = cnt % EPOCH + 1
                cnt += 1
        slot_cnt = {}
        for op in self.ops:
            if op.is_dma:
                c = slot_cnt.get(op.slot, 0) + 1
                slot_cnt[op.slot] = c
                op.sem = get_sem("d_%s_%d" % op.slot)
                op.val = 16 * c
        finals = list(self.slot_last.values())
        block = stack.enter_context(nc.Block())

        def run(eng):
            def body(h):
                for op in self.by_eng[eng]:
                    if op.prev_slot_wait is not None:
                        p = op.prev_slot_wait
                        h.wait_ge(p.sem, p.val)
                    for d in op.waits:
                        h.wait_ge(d.sem, d.val)
                    ins = op.fn(h)
                    if op.signal:
                        ins.then_inc(op.sem, 16 if op.is_dma else 1)
                if eng == "sp":
                    for o in finals:
                        h.wait_ge(o.sem, o.val)
            return body

        block.tensor(run("pe"))
        block.scalar(run("act"))
        block.vector(run("dve"))
        block.gpsimd(run("pool"))
        block.sync(run("sp"))


GRAN = 256


class X:
    __slots__ = ("ap", "k")

    def __init__(self, ap, k):
        self.ap = ap
        self.k = k


class View:
    def __init__(self, space, base_ap, off, shape, esz, gran=GRAN):
        self.space = space
        self.ap = base_ap
        self.off = off
        self.shape = list(shape)
        self.esz = esz
        self.gran = gran
        st = []
        s = 1
        for n in reversed(self.shape):
            st.append(s)
            s *= n
        self.strides = list(reversed(st))
        self.nelem = s

    def __call__(self, pr=None, *idx):
        p0, p1 = (0, 128) if pr is None else pr
        sl = [slice(p0, p1)]
        lo = 0
        hi = 0
        for d, n in enumerate(self.shape):
            it = idx[d] if d < len(idx) else None
            if it is None:
                a, b = 0, n
                sl.append(slice(None))
            elif isinstance(it, int):
                a, b = it, it + 1
                sl.append(it)
            else:
                a, b = it
                sl.append(slice(a, b))
            lo += a * self.strides[d]
            hi += (b - 1) * self.strides[d]
        hi += 1
        b0 = self.off + lo * self.esz
        b1 = self.off + hi * self.esz
        halves = []
        if p0 < 64:
            halves.append(0)
        if p1 > 64:
            halves.append(1)
        g0 = b0 // self.gran
        g1 = (b1 - 1) // self.gran
        sp = self.space
        if sp == "ps":
            keys = [(sp, g) for g in range(g0, g1 + 1)]
        else:
            keys = [(sp, g, h) for g in range(g0, g1 + 1) for h in halves]
        return X(self.ap[tuple(sl)], keys)


class Arena:
    def __init__(self, nc, st, nbytes):
        self.nbytes = nbytes
        self.t = st.enter_context(nc.sbuf_tensor("arena", [128, nbytes // 2], BF16))
        self.top = 0

    def view(self, off, shape, dtype):
        esz = 4 if dtype == F32 else 2
        n = 1
        for s in shape:
            n *= s
        assert off % 4 == 0
        assert off + n * esz <= self.nbytes, ("arena overflow", off, n * esz, self.nbytes)
        ap = self.t[:, off // 2: off // 2 + n * esz // 2]
        if dtype == F32:
            ap = ap.bitcast(F32)
        if len(shape) > 1:
            names = " ".join("d%d" % i for i in range(len(shape)))
            kw = {"d%d" % i: shape[i] for i in range(1, len(shape))}
            ap = ap.rearrange("p (%s) -> p %s" % (names, names), **kw)
        return View("sb", ap, off, shape, esz)


class Bump:
    def __init__(self, arena, start, limit):
        self.a = arena
        self.top = start
        self.limit = limit
        self.peak = start

    def alloc(self, shape, dtype, align=GRAN):
        esz = 4 if dtype == F32 else 2
        n = esz
        for s in shape:
            n *= s
        off = (self.top + align - 1) // align * align
        self.top = off + n
        assert self.top <= self.limit, ("bump overflow", self.top, self.limit)
        self.peak = max(self.peak, self.top)
        return self.a.view(off, shape, dtype)


class Prog:
    def __init__(self, NB, layers, debug=None, stop=None):
        self.stop = stop
        self.NB = NB
        self.layers = layers
        self.debug = debug
        self.nc = bass.Bass("TRN2", target_bir_lowering=False)
        self.S = Sched()
        self.psrr = 0

    def mm(self, out, lhsT, rhs, start=True, stop=True):
        rk = lhsT.k + rhs.k
        if not start:
            rk = rk + out.k
        o, l, r = out.ap, lhsT.ap, rhs.ap
        self.S.op("pe", lambda h: h.matmul(o, lhsT=l, rhs=r, start=start, stop=stop),
                  reads=rk, writes=out.k)

    def tr(self, out, in_, ident):
        o, i, d = out.ap, in_.ap, ident.ap
        self.S.op("pe", lambda h: h.transpose(o, i, d), reads=in_.k + ident.k, writes=out.k)

    def act(self, out, in_, func, scale=1.0, bias=0.0, accum=None):
        rk = list(in_.k)
        sc = scale
        bi = bias
        if isinstance(scale, X):
            rk += scale.k
            sc = scale.ap
        if isinstance(bias, X):
            rk += bias.k
            bi = bias.ap
        wk = list(out.k)
        ac = None
        if accum is not None:
            wk += accum.k
            ac = accum.ap
        o, i = out.ap, in_.ap
        if ac is None:
            fn = lambda h: h.activation(out=o, in_=i, func=func, bias=bi, scale=sc)
        else:
            fn = lambda h: h.activation(out=o, in_=i, func=func, bias=bi, scale=sc, accum_out=ac)
        self.S.op("act", fn, reads=rk, writes=wk)

    def tt(self, eng, out, in0, in1, op):
        o, a, b = out.ap, in0.ap, in1.ap
        self.S.op(eng, lambda h: h.tensor_tensor(out=o, in0=a, in1=b, op=op),
                  reads=in0.k + in1.k, writes=out.k)

    def ts(self, eng, out, in0, s1, op0, s2=None, op1=None):
        rk = list(in0.k)
        a1 = s1
        a2 = s2
        if isinstance(s1, X):
            rk += s1.k
            a1 = s1.ap
        if isinstance(s2, X):
            rk += s2.k
            a2 = s2.ap
        o, i = out.ap, in0.ap
        if op1 is None:
            fn = lambda h: h.tensor_scalar(out=o, in0=i, scalar1=a1, scalar2=None, op0=op0)
        else:
            fn = lambda h: h.tensor_scalar(out=o, in0=i, scalar1=a1, scalar2=a2, op0=op0, op1=op1)
        self.S.op(eng, fn, reads=rk, writes=out.k)

    def stt(self, out, in0, scalar, in1, op0, op1):
        rk = in0.k + in1.k
        sc = scalar
        if isinstance(scalar, X):
            rk = rk + scalar.k
            sc = scalar.ap
        o, a, b = out.ap, in0.ap, in1.ap
        self.S.op("dve", lambda h: h.scalar_tensor_tensor(out=o, in0=a, scalar=sc, in1=b, op0=op0, op1=op1),
                  reads=rk, writes=out.k)

    def copy(self, eng, out, in_):
        o, i = out.ap, in_.ap
        if eng == "act":
            self.S.op("act", lambda h: h.activation(out=o, in_=i, func=AF.Copy), reads=in_.k, writes=out.k)
        else:
            self.S.op(eng, lambda h: h.tensor_copy(out=o, in_=i), reads=in_.k, writes=out.k)

    def recip(self, out, in_, fast=False):
        o, i = out.ap, in_.ap
        if fast:
            self.S.op("dve", lambda h: h.reciprocal_approx_fast(out=o, in_=i), reads=in_.k, writes=out.k)
        else:
            self.S.op("dve", lambda h: h.reciprocal(out=o, in_=i), reads=in_.k, writes=out.k)

    def memset(self, eng, out, val):
        o = out.ap
        self.S.op(eng, lambda h: h.memset(o, val), writes=out.k)

    def dma(self, eng, out, in_, out_keys=None, in_keys=None, **kw):
        ok = out.k if isinstance(out, X) else (out_keys or [])
        ik = in_.k if isinstance(in_, X) else (in_keys or [])
        o = out.ap if isinstance(out, X) else out
        i = in_.ap if isinstance(in_, X) else in_
        self.S.dma(eng, lambda h: h.dma_start(out=o, in_=i, **kw), reads=ik, writes=ok)

    def dump(self, name, view, nrow_chunks=1):
        if not (self.debug and name in self.debug):
            return
        tmpf = self.L.a.view(self.L.a.nbytes - TA * 4, [TA], F32)
        for k in range(nrow_chunks):
            src = view(None, k) if nrow_chunks > 1 else view()
            self.copy("dve", tmpf(), src)
            self.dma("sp", self.d[name][k * 128:(k + 1) * 128, :], tmpf())

    def barrier(self):
        S = self.S
        toks = []
        for e in ("pe", "act", "dve", "pool"):
            k = ("bar", e)
            toks.append(k)
            S.op(e, lambda h: h.nop(), reads=[], writes=[k])
        outstanding = [("bar_dma", o.idx) for o in S.slot_last.values()]
        for o in list(S.slot_last.values()):
            S.last_writer[("bar_dma", o.idx)] = o
        for e in ("pe", "act", "dve", "pool"):
            S.op(e, lambda h: h.nop(), reads=toks + outstanding, writes=[])
        S.op("sp", lambda h: h.nop(), reads=toks + outstanding, writes=[("bar", "sp")])

    def ps(self, n=1):
        if n == 2:
            if self.psrr % 2:
                self.psrr += 1
            i = (self.psrr // 2) % len(self.pspair)
            self.psrr += 2
            return self.pspair[i]
        i = self.psrr % len(self.psbank)
        self.psrr += 1
        return self.psbank[i]

    def build(self):
        nc = self.nc
        NB = self.NB
        dt = nc.dram_tensor
        I = "ExternalInput"
        self.d = d = {}

        def inp(name, shape, dtype=F32):
            d[name] = dt(name, list(shape), dtype, kind=I).ap()

        inp("x", [NB, T_LAT, D])
        inp("c", [NB, D])
        inp("ctx", [NB, T_CTX, D])
        inp("c_ctx", [D])
        inp("ada_w", [4, D, 3 * D])
        inp("ada_bc", [4, 128, 24])
        inp("npre_c", [4, 128, 8])
        inp("npost", [4, D])
        inp("mla_win", [2, D, 1920])

        inp("mla_qn_c", [2, 128, 2])
        inp("mla_kvn_c", [2, 128, 2])
        inp("mla_wuq", [2, 256, 2048])
        inp("mla_wukv", [2, 256, 2048])
        inp("mla_wout", [2, D, D])
        inp("ml_win", [2, D, 6144])
        inp("ml_cw_c", [2, 128, 16, 5])
        inp("ml_cb_c", [2, 128, 16])
        inp("ml_wq_c", [2, 128, 16, 4])
        inp("ml_wk_c", [2, 128, 16, 4])
        inp("ml_wv_c", [2, 128, 16, 4])
        inp("ml_wqT_c", [2, 128, 16, 4])
        inp("ml_wkT_c", [2, 128, 16, 4])
        inp("ml_wvT_c", [2, 128, 16, 4])
        inp("ml_wif", [2, 2, 6144, 16])
        inp("ml_bif_c", [2, 32, 1])
        inp("ml_hn_c", [2, 128, 16])
        inp("ml_sk_c", [2, 128, 16])
        inp("ml_wout", [2, 2 * D, D])
        inp("k_ident", [128, 128])
        inp("k_maskF", [128, 128])
        inp("k_maskB", [128, 128])
        inp("k_bdmask", [128, 128])
        inp("k_tq", [128, TA])
        inp("k_tkc", [128, TA])
        inp("k_tks", [128, TA])
        d["out"] = dt("out", [NB, T_LAT, D], F32, kind="ExternalOutput").ap()
        d["ctxs"] = dt("ctxs", [NB, T_CTX, D], F32, kind="Internal").ap()
        d["ptd"] = dt("ptd", [NT, 128, 16, 128], BF16, kind="Internal").ap()
        if self.debug:
            for name, shape in self.debug.items():
                d[name] = dt(name, list(shape), F32, kind="ExternalOutput").ap()

        with ExitStack() as st:
            self.st = st
            ARENA_BYTES = 200 * 1024
            self.arena = Arena(nc, st, ARENA_BYTES)
            pp = [st.enter_context(nc.psum_tensor("pp%d" % i, [128, 1024], F32)) for i in range(4)]
            self.pspair = []
            self.psbank = []
            for i in range(4):
                self.pspair.append(View("ps", pp[i][:, :], i * 4096, [1024], 4, gran=2048))
                for hh in range(2):
                    self.psbank.append(View("ps", pp[i][:, hh * 512:(hh + 1) * 512],
                                            i * 4096 + hh * 2048, [512], 4, gran=2048))
            self.psbf = []
            for i in range(4):
                for hh in range(2):
                    self.psbf.append(View("ps", pp[i][:, hh * 512:(hh + 1) * 512].bitcast(BF16),
                                          i * 4096 + hh * 2048, [1024], 2, gran=2048))
            self.psrr = 0
            self.psrr2 = 0
            self.setup()
            import os
            if os.environ.get("KBAR"):
                self.barrier()
            if self.stop != "setup":
                for b in range(NB):
                    for li in self.layers:
                        self.layer(b, li)
            self.S.emit(nc, st)
        return nc

    def setup(self):
        d = self.d
        A = self.arena
        P = Bump(A, 0, A.nbytes)
        self.persist = P
        al = P.alloc
        self.ident = al([128], F32)
        self.identb = al([128], BF16)
        self.maskF = al([128], F32)
        self.maskB = al([128], F32)
        self.bdmask = al([128], F32)
        self.ones = al([128], F32)
        self.onesb = al([128], BF16)
        self.modcol = al([4, 24, 3], F32)
        self.adab = al([4, 24], F32)
        self.npre = al([4, 8], F32)
        self.silc = al([8, 3], F32)
        self.hT = al([8, TA], BF16)
        self.junk = al([D], BF16)
        self.small = al([64], F32)
        self.acol = al([2, 8], F32)
        self.bcol = al([2, 8], F32)
        self.gcol = al([2, 8], F32)
        self.ssq_ = al([NT + 2, 8], F32)
        self.rr_ = al([NT + 2, 8], F32)
        self.ssq = lambda pr, cr: self.ssq_(pr, cr[0], (0, 1))
        self.rr = lambda pr, cr: self.rr_(pr, cr[0], (0, 1))
        self.cst = al([4], F32)
        craw = al([8, 3], F32)
        th = al([8, 3], F32)
        wst = [al([8, 128], F32) for _ in range(2)]
        self.layer_base = P.top
        for i_, v_ in enumerate((EPS, 4 * EPS, LN16, 1.0)):
            self.memset("dve", self.cst(None, (i_, i_ + 1)), v_)
        self.c_eps = self.cst(None, (0, 1))
        self.c_eps4 = self.cst(None, (1, 2))
        self.c_ln16 = self.cst(None, (2, 3))
        self.c_one = self.cst(None, (3, 4))
        for v, name in ((self.ident, "k_ident"), (self.maskF, "k_maskF"), (self.maskB, "k_maskB"),
                        (self.bdmask, "k_bdmask")):
            self.dma("sp", v(), d[name])
        self.copy("dve", self.identb(), self.ident())
        self.memset("dve", self.ones(), 1.0)
        self.memset("dve", self.onesb(), 1.0)
        self.dma("sp", self.adab(), d["ada_bc"].rearrange("l p c -> p l c"))
        self.dma("sp", self.npre(), d["npre_c"].rearrange("l p c -> p l c"))
        cc = self.small
        for v in range(3):
            if v < self.NB:
                src = d["c"][v].rearrange("(k p) -> p k", p=128)
            elif v == 2:
                src = d["c_ctx"].rearrange("(k p) -> p k", p=128)
            else:
                src = d["c_ctx"].rearrange("(k p) -> p k", p=128)
            self.S.dma("sp", (lambda o, i: (lambda h: h.dma_start(out=o, in_=i, allow_slow_non_contiguous=True)))(
                craw(None, None, v).ap, src), writes=craw(None, None, v).k)
        self.act(th(), craw(), AF.Tanh, scale=0.5)
        self.stt(self.silc(), th(), 1.0, craw(), ALU.add, ALU.mult)
        self.ts("dve", self.silc(), self.silc(), 0.5, ALU.mult)
        cnt = 0
        for l in range(4):
            if l not in self.layers:
                continue
            for n in range(24):
                w = wst[cnt % 2]
                cnt += 1
                self.dma("sp", w(), d["ada_w"][l].rearrange("(k p) n -> p k n", p=128)[:, :, n * 128:(n + 1) * 128])
                pst = self.ps()
                for k in range(8):
                    self.mm(pst(None, (0, 3)), w(None, k), self.silc(None, k), start=(k == 0), stop=(k == 7))
                self.ts("dve", self.modcol(None, l, n), pst(None, (0, 3)), self.adab(None, l, (n, n + 1)), ALU.add)

    def layer(self, b, li):
        d = self.d
        A = self.arena
        L = Bump(A, self.layer_base, A.nbytes)
        self.L = L
        is_mla = (li % 2 == 0)
        j = li // 2
        need_ctx = li < 3
        first = (li == self.layers[0])
        def xsrc(t):
            if t < 2:
                src = d["ctx"][b] if first else d["ctxs"][b]
                return src[t * 128:(t + 1) * 128, :], ("dram_ctx", b, t)
            src = d["x"][b] if first else d["out"][b]
            return src[(t - 2) * 128:(t - 1) * 128, :], ("dram_x", b, t)

        def xdst(t):
            if t < 2:
                return d["ctxs"][b][t * 128:(t + 1) * 128, :], ("dram_ctx", b, t)
            return d["out"][b][(t - 2) * 128:(t - 1) * 128, :], ("dram_x", b, t)

        for kind, v in ((0, 2), (1, b)):
            sc = self.modcol(None, li, (8, 16), v)
            sh = self.modcol(None, li, (0, 8), v)
            gt = self.modcol(None, li, (16, 24), v)
            self.stt(self.acol(None, kind), sc, 1.0, self.npre(None, li), ALU.add, ALU.mult)
            self.copy("dve", self.bcol(None, kind), sh)
            self.copy("dve", self.gcol(None, kind), gt)
        import os
        if os.environ.get("KSKIP"):
            L.top += int(os.environ["KSKIP"]) * 1024
        self.xg = L.alloc([4, D], F32)
        if self.stop == "A_mod":
            return
        ntl = NT
        if self.stop in ("A_t0", "A_t0b", "A_t0c"):
            ntl = 1
        elif self.stop == "A_n2p":
            ntl = 2
        elif self.stop and self.stop.startswith("A_n"):
            ntl = int(self.stop[3:])
        tl0 = 0
        if self.stop == "A_only1":
            tl0, ntl = 1, 2
        if self.stop == "A_only2":
            tl0, ntl = 2, 3
        for t in range(tl0, ntl):
            if self.stop == "A_n2p":
                self.psrr = 0
            xs = self.xg(None, t % 4)
            src, dk = xsrc(t)
            self.dma("sp", xs, src, in_keys=[dk])
            self.act(self.junk(), xs, AF.Square, accum=self.ssq(None, (t, t + 1)))
            self.act(self.rr(None, (t, t + 1)), self.ssq(None, (t, t + 1)), AF.Ln, scale=1.0 / D, bias=self.c_eps)
            self.act(self.rr(None, (t, t + 1)), self.rr(None, (t, t + 1)), AF.Exp, scale=-0.5)
            import os
            step = int(os.environ.get("KSTEP", "9"))
            if self.stop == "A_t0" or step <= 0:
                continue
            self.ts("dve", xs, xs, self.rr(None, (t, t + 1)), ALU.mult)
            if self.stop == "A_t0b" or step <= 1:
                continue
            kind = 0 if t < 2 else 1
            for kq in range(2):
                pst = self.ps()
                for kk in range(4):
                    k = kq * 4 + kk
                    self.tr(pst(None, (kk * 128, kk * 128 + 128)), self.xg(None, t % 4, (k * 128, k * 128 + 128)),
                            self.ident())
                if step <= 2:
                    continue
                for kk in range(4):
                    k = kq * 4 + kk
                    o = self.hT(None, k, (t * 128, t * 128 + 128))
                    i = pst(None, (kk * 128, kk * 128 + 128))
                    if kq == 0:
                        self.act(o, i, AF.Identity, scale=self.acol(None, kind, (k, k + 1)),
                                 bias=self.bcol(None, kind, (k, k + 1)))
                    else:
                        self.ts("dve", o, i, self.acol(None, kind, (k, k + 1)), ALU.mult,
                                self.bcol(None, kind, (k, k + 1)), ALU.add)
        if self.stop in ("A_t0", "A_t0b", "A_t0c") or (self.stop and (self.stop.startswith("A_n") or self.stop.startswith("A_only"))):
            return
        if self.debug and "dbg_hT" in self.debug:
            tmpf = L.alloc([TA], F32)
            for k in range(8):
                self.copy("dve", tmpf(), self.hT(None, k))
                self.dma("sp", d["dbg_hT"][k * 128:(k + 1) * 128, :], tmpf())
            L.top = self.layer_base

        if self.stop in ("A", "A_nodbg"):
            return
        L.top = self.layer_base
        if is_mla:
            KC = 8
            self.mla(b, li, j, need_ctx)
        else:
            KC = 16
            self.mlstm(b, li, j, need_ctx)

        if self.stop in ("proj", "attn1"):
            return
        if is_mla:
            L.top = self.pt_end
            wout = L.alloc([8, D], BF16)
            for k4 in range(2):
                self.dma("pool", wout(None, (k4 * 4, k4 * 4 + 4)),
                         d["mla_wout"][j].rearrange("(k p) n -> p k n", p=128)[:, k4 * 4:(k4 + 1) * 4, :])
        else:
            L.top = self.layer_base
            wout = L.alloc([16, D], BF16)
            for k4 in range(4):
                self.dma("pool", wout(None, (k4 * 4, k4 * 4 + 4)),
                         d["ml_wout"][j].rearrange("(k p) n -> p k n", p=128)[:, k4 * 4:(k4 + 1) * 4, :])
            ptl = [L.alloc([16, 128], BF16) for _ in range(3)]
        xt = [L.alloc([D], F32) for _ in range(3)]
        t1 = [L.alloc([D], F32) for _ in range(3)]
        self.Gt = L.alloc([2, D], F32)
        self.npost_b = L.alloc([D], F32)
        self.dma("sp", self.npost_b(), d["npost"][li:li + 1, :].broadcast_to([128, D]))
        dg = L.alloc([128], F32)
        for kind in range(2):
            for hh in range(2):
                pst = self.ps()
                for kk in range(4):
                    k = hh * 4 + kk
                    self.ts("dve", dg(), self.ident(), self.gcol(None, kind, (k, k + 1)), ALU.mult)
                    self.mm(pst(None, (kk * 128, kk * 128 + 128)), self.ones(), dg())
                self.tt("dve", self.Gt(None, kind, (hh * 512, hh * 512 + 512)), pst(),
                        self.npost_b(None, (hh * 512, hh * 512 + 512)), ALU.mult)
        t_start = 0 if need_ctx else 2
        for t in range(t_start, NT):
            kind = 0 if t < 2 else 1
            if is_mla:
                lhs = lambda k: self.PT(None, k, (t * 128, t * 128 + 128))
            else:
                pl = ptl[t % 3]
                self.dma("sp", pl(), d["ptd"][t], in_keys=[("ptd", t)])
                lhs = lambda k: pl(None, k)
            y = self.ps(2)
            for hh in range(2):
                for k in range(KC):
                    self.mm(y(None, (hh * 512, hh * 512 + 512)), lhs(k), wout(None, k, (hh * 512, hh * 512 + 512)),
                            start=(k == 0), stop=(k == KC - 1))
            sq = self.ssq(None, (NT, NT + 1))
            r = self.rr(None, (NT, NT + 1))
            sq2 = self.ssq(None, (NT + 1, NT + 2))
            self.act(self.junk(None, (0, 512)), y(None, (0, 512)), AF.Square, accum=sq)
            self.act(self.junk(None, (512, 1024)), y(None, (512, 1024)), AF.Square, accum=sq2)
            self.tt("dve", sq, sq, sq2, ALU.add)
            self.act(r, sq, AF.Ln, scale=1.0 / D, bias=self.c_eps)
            self.act(r, r, AF.Exp, scale=-0.5)
            x_ = xt[t % 3]
            src, dk = xsrc(t)
            self.dma("sp", x_(), src, in_keys=[dk])
            tt_ = t1[t % 3]
            for hh in range(2):
                cs = (hh * 512, hh * 512 + 512)
                self.stt(tt_(None, cs), y(None, cs), r, self.Gt(None, kind, cs), ALU.mult, ALU.mult)
            self.tt("pool", x_(), x_(), tt_(), ALU.add)
            dst, dk = xdst(t)
            self.dma("pool", dst, x_(), out_keys=[dk])

    def mla(self, b, li, j, need_ctx):
        d = self.d
        L = self.L
        al = L.alloc
        self.PT = al([8, TA], BF16)
        self.pt_end = L.top
        cqn = al([2, TA], BF16)
        ckvn = al([2, TA], BF16)
        Qh = [al([TA], BF16) for _ in range(2)]
        Kh = [al([TA], BF16) for _ in range(2)]
        Vh = [al([NT, 128], BF16) for _ in range(2)]
        wuq = al([2, 2048], BF16)
        wukv = al([2, 2048], BF16)
        ws = [al([8, 256], BF16) for _ in range(3)]
        tq = al([TA], BF16)
        tks = [al([512], F32) for _ in range(4)]
        pexp = [al([1024], BF16) for _ in range(4)]
        rd = [al([512], F32) for _ in range(2)]
        thb = [al([512], F32) for _ in range(2)]
        sqb = [al([512], BF16) for _ in range(2)]
        rb = [al([512], F32) for _ in range(2)]
        qn = al([2], F32)
        kvn = al([2], F32)
        tmp_top = L.top

        win = d["mla_win"][j].rearrange("(k p) n -> p k n", p=128)
        self.dma("sp", qn(), d["mla_qn_c"][j])
        self.dma("sp", kvn(), d["mla_kvn_c"][j])
        self.dma("pool", tq(), d["k_tq"])
        self.dma("pool", wuq(), d["mla_wuq"][j].rearrange("(k p) n -> p k n", p=128))
        self.dma("pool", wukv(), d["mla_wukv"][j].rearrange("(k p) n -> p k n", p=128))
        for s in range(2):
            self.memset("dve", Vh[s](None, None, (64, 128)), 2.0)
        ntiles = [(0, 256)] + [(256 + 512 * i, 256 + 512 * (i + 1)) for i in range(4)]
        wsi = 0
        for gi, (dst, gn) in enumerate(((cqn, qn), (ckvn, kvn))):
            w = ws[wsi % 3]
            wsi += 1
            self.dma("pool", w(), win[:, :, gi * 256:(gi + 1) * 256])
            for ni, (n0, n1) in enumerate(ntiles):
                n = n1 - n0
                pc = [self.ps(), self.ps()]
                for cch in range(2):
                    for k in range(8):
                        self.mm(pc[cch](None, (0, n)), w(None, k, (cch * 128, cch * 128 + 128)),
                                self.hT(None, k, (n0, n1)), start=(k == 0), stop=(k == 7))
                pss = self.ps()
                for cch in range(2):
                    sq = sqb[cch]
                    self.act(sq(None, (0, n)), pc[cch](None, (0, n)), AF.Square)
                    self.mm(pss(None, (0, n)), self.onesb(), sq(None, (0, n)), start=(cch == 0), stop=(cch == 1))
                r = rb[ni % 2]
                self.act(r(None, (0, n)), pss(None, (0, n)), AF.Ln, scale=1.0 / 256, bias=self.c_eps)
                self.act(r(None, (0, n)), r(None, (0, n)), AF.Exp, scale=-0.5)
                for cch in range(2):
                    self.stt(dst(None, cch, (n0, n1)), pc[cch](None, (0, n)), gn(None, (cch, cch + 1)),
                             r(None, (0, n)), ALU.mult, ALU.mult)
        w = ws[wsi % 3]
        wsi += 1
        self.dma("pool", w(), win[:, :, 512:768])
        H1 = (64, 128)
        H0 = (0, 64)
        for ni, (n0, n1) in enumerate(ntiles):
            n = n1 - n0
            pa = self.ps()
            pb = self.ps()
            wa = w
            for k in range(8):
                self.mm(pa(None, (0, n)), wa(None, k, (0, 128)), self.hT(None, k, (n0, n1)), start=(k == 0), stop=(k == 7))
            for k in range(8):
                self.mm(pb(None, (0, n)), w(None, k, (128, 256)), self.hT(None, k, (n0, n1)), start=(k == 0), stop=(k == 7))
            tc_ = tks[(2 * ni) % 4]
            ts_ = tks[(2 * ni + 1) % 4]
            self.dma("sp", tc_(None, (0, n)), d["k_tkc"][:, n0:n1])
            self.dma("sp", ts_(None, (0, n)), d["k_tks"][:, n0:n1])
            u = thb[0]
            v = thb[1]
            self.tt("dve", u(H1, (0, n)), pa(H1, (0, n)), tc_(H1, (0, n)), ALU.mult)
            self.tt("dve", v(H1, (0, n)), pb(H1, (0, n)), ts_(H1, (0, n)), ALU.mult)
            self.tt("pool", Kh[0](H1, (n0, n1)), u(H1, (0, n)), v(H1, (0, n)), ALU.add)
            self.copy("pool", Kh[1](H1, (n0, n1)), Kh[0](H1, (n0, n1)))
        for gc in range(8):
            if gc % 2 == 0:
                w = ws[wsi % 3]
                wsi += 1
                self.dma("pool", w(), win[:, :, 896 + gc * 128: 896 + gc * 128 + 256])
            co = (gc % 2) * 128
            for ni, (n0, n1) in enumerate(ntiles):
                n = n1 - n0
                pg = self.ps()
                for k in range(8):
                    self.mm(pg(None, (0, n)), w(None, k, (co, co + 128)), self.hT(None, k, (n0, n1)),
                            start=(k == 0), stop=(k == 7))
                th = thb[ni % 2]
                self.act(th(None, (0, n)), pg(None, (0, n)), AF.Tanh, scale=0.5)
                self.stt(self.PT(None, gc, (n0, n1)), th(None, (0, n)), 1.0, pg(None, (0, n)), ALU.add, ALU.mult)
        self.dump("dbg_cqn", cqn, 2)
        self.dump("dbg_ckvn", ckvn, 2)
        self.dump("dbg_SG", self.PT, 8)
        if self.stop == "proj":
            return
        qchunks = ([(0, 256, 2)] if need_ctx else []) + [(256 + 512 * i, 256 + 512 * (i + 1), NT) for i in range(4)]
        acnt = [0]
        nheads = 1 if self.stop == "attn1" else NH

        def proj(h):
                s = h % 2
                Q = Qh[s]
                K = Kh[s]
                V = Vh[s]
                pj = [self.psbank[i_] for i_ in range(6)]
                pjc = 0
                for ni, (n0, n1) in enumerate(ntiles):
                    n = n1 - n0
                    pq = pj[pjc % 6]
                    pjc += 1
                    for k in range(2):
                        lq = wuq(None, k, (h * 128, h * 128 + 128))
                        self.mm(pq(None, (0, n)), lq, cqn(None, k, (n0, n1)),
                                start=(k == 0), stop=(k == 1))
                    self.copy("dve", Q(H0, (n0, n1)), pq(H0, (0, n)))
                    self.tt("dve", Q(H1, (n0, n1)), pq(H1, (0, n)), tq(H1, (n0, n1)), ALU.mult)
                    pk = pj[pjc % 6]
                    pjc += 1
                    for k in range(2):
                        self.mm(pk(H0, (0, n)), wukv(None, k, (h * 128, h * 128 + 64)), ckvn(None, k, (n0, n1)),
                                start=(k == 0), stop=(k == 1))
                    self.copy("dve", K(H0, (n0, n1)), pk(H0, (0, n)))
                for g0 in range(0, NT, 8):
                    g1 = min(NT, g0 + 8)
                    pv = pj[pjc % 6]
                    pjc += 1
                    for t in range(g0, g1):
                        for k in range(2):
                            self.mm(pv(None, ((t - g0) * 64, (t - g0) * 64 + 64)), ckvn(None, k, (t * 128, t * 128 + 128)),
                                    wukv(None, k, (h * 128 + 64, h * 128 + 128)), start=(k == 0), stop=(k == 1))
                    ng = g1 - g0
                    src = X(pv.ap[:, 0:ng * 64].rearrange("p (t c) -> p t c", c=64), pv(None, (0, ng * 64)).k)
                    self.copy("dve", V(None, (g0, g1), (0, 64)), src)

        proj(0)
        for h in range(nheads):
            s = h % 2
            Q = Qh[s]
            K = Kh[s]
            V = Vh[s]
            if h == 0:
                self.dump("dbg_Q0", Q)
                self.dump("dbg_K0", K)
            hp = (h % 2) * 64
            HP = (hp, hp + 64)
            for qi, (q0, q1, nk) in enumerate(qchunks):
                nq = q1 - q0
                acc = self.psbank[6 + (acnt[0] % 2)]
                acnt[0] += 1
                def scores_exp(kt):
                    sp_ = self.pspair[(kt // 2) % 3]
                    for u2 in range(2):
                        self.mm(sp_(None, (u2 * 512, u2 * 512 + nq)), K(None, ((kt + u2) * 128, (kt + u2) * 128 + 128)),
                                Q(None, (q0, q1)))
                    pe_ = pexp[(kt // 2) % 4]
                    if nq == 512:
                        self.act(pe_(), sp_(), AF.Exp, scale=ATT_SCALE)
                    else:
                        for u2 in range(2):
                            self.act(pe_(None, (u2 * 512, u2 * 512 + nq)), sp_(None, (u2 * 512, u2 * 512 + nq)),
                                     AF.Exp, scale=ATT_SCALE)

                def pv(kt):
                    pe_ = pexp[(kt // 2) % 4]
                    for u2 in range(2):
                        self.mm(acc(None, (0, nq)), V(None, kt + u2), pe_(None, (u2 * 512, u2 * 512 + nq)),
                                start=(kt + u2 == 0), stop=(kt + u2 == nk - 1))

                if qi == 2 and h + 1 < nheads:
                    proj(h + 1)
                kts = list(range(0, nk, 2))
                scores_exp(kts[0])
                if len(kts) > 1:
                    scores_exp(kts[1])
                for i_, kt in enumerate(kts):
                    if i_ + 2 < len(kts):
                        scores_exp(kts[i_ + 2])
                    pv(kt)
                r = rd[qi % 2]
                accs = thb[qi % 2]
                ptv = self.PT(HP, h // 2, (q0, q1))
                if hp == 0:
                    self.copy("dve", accs(None, (0, nq)), acc(None, (0, nq)))
                    self.recip(r(H0, (0, nq)), accs(H1, (0, nq)))
                    self.tt("pool", r(H0, (0, nq)), r(H0, (0, nq)), ptv, ALU.mult)
                    self.tt("pool", ptv, accs(H0, (0, nq)), r(H0, (0, nq)), ALU.mult)
                else:
                    self.copy("dve", accs(H1, (0, nq)), acc(H0, (0, nq)))
                    self.copy("dve", accs(H0, (0, nq)), acc(H1, (0, nq)))
                    self.recip(r(H1, (0, nq)), accs(H0, (0, nq)))
                    self.tt("pool", r(H1, (0, nq)), r(H1, (0, nq)), ptv, ALU.mult)
                    self.tt("pool", ptv, accs(H1, (0, nq)), r(H1, (0, nq)), ALU.mult)
        self.dump("dbg_PT", self.PT, 8)

    def mlstm(self, b, li, j, need_ctx):
        d = self.d
        L = self.L
        al = L.alloc
        XW = 2312
        ntiles = [(0, 256)] + [(256 + 512 * i, 256 + 512 * (i + 1)) for i in range(4)]

        def PO(n0):
            return 2 + n0 if n0 < 256 else 260 + (n0 - 256)

        cw = al([16, 5], F32)
        cb = al([16], F32)
        hbc = al([16], F32)
        hgc = al([16], F32)
        skc = al([16], F32)
        wsm = [al([16, 4], F32) for _ in range(6)]
        BD = [al([16, 128], BF16) for _ in range(3)]
        Wc = al([16, 32], BF16)
        Wm = al([16, 32], BF16)
        bif = al([1], F32)
        gtok = al([NT, 32], F32)
        spc = al([NT * 8], F32)
        tmpg = al([NT * 8], F32)
        alpha = [al([NT, 8], F32) for _ in range(2)]
        ib = [al([NT, 8], F32) for _ in range(2)]
        decay = [al([NT, 8], F32) for _ in range(2)]
        aend = [al([NT, 8], F32) for _ in range(2)]
        C32 = [al([2, 260], F32) for _ in range(2)]
        Cbf = [al([2, 260], BF16) for _ in range(2)]
        AT = [al([128], BF16) for _ in range(4)]
        khat = [al([256], BF16) for _ in range(4)]
        mcol = [al([8], F32) for _ in range(4)]
        dgc = [al([5, 128], BF16) for _ in range(1)]
        tA = [al([512], F32) for _ in range(2)]
        tB = [al([512], F32) for _ in range(2)]
        ssqh = al([NT], F32)
        rrh = al([NT], F32)
        hnb = [L.a.view(tB[i_].off, [256], BF16) for i_ in range(2)]
        preb = [al([2, 128], BF16) for _ in range(2)]
        utmp = [L.a.view(tA[i_].off, [128], F32) for i_ in range(2)]
        wxm = [al([8, 256], BF16) for _ in range(2)]
        wzo = [al([8, 512], BF16) for _ in range(2)]
        B1 = al([2, XW], BF16)
        B2 = al([2, TA], BF16)
        B3 = al([2, TA], BF16)
        B4 = al([2, TA], BF16)
        B5 = al([NT, 256], BF16)
        B6 = al([NT, 258], BF16)
        HF = al([NT, 256], BF16)
        HBv = al([NT, 256], BF16)
        gT = L.a.view(HF.off, [TA], F32)
        setup_top = L.top

        self.dma("sp", cw(), d["ml_cw_c"][j])
        self.dma("sp", cb(), d["ml_cb_c"][j])
        self.dma("sp", hgc(), d["ml_hn_c"][j])
        self.dma("sp", skc(), d["ml_sk_c"][j])
        self.dma("sp", bif((0, 32)), d["ml_bif_c"][j])
        for i, nm in enumerate(("ml_wq_c", "ml_wk_c", "ml_wv_c", "ml_wqT_c", "ml_wkT_c", "ml_wvT_c")):
            self.dma("sp", wsm[i](), d[nm][j])
        self.ts("dve", hbc(), cb(), 0.5, ALU.mult)
        self.ts("dve", hgc(), hgc(), 0.5, ALU.mult)
        self.ts("dve", skc(), skc(), 0.5, ALU.mult)

        def bd_build(out_x, wv, jj):
            w_ap = wv(None, jj).ap.unsqueeze(1).broadcast_to([128, 32, 4])
            m_ap = self.bdmask().ap.rearrange("p (m d) -> p m d", d=4)
            o_ap = out_x.ap.rearrange("p (m d) -> p m d", d=4)
            self.tt("dve", X(o_ap, out_x.k), X(w_ap, wv(None, jj).k), X(m_ap, self.bdmask().k), ALU.mult)

        for jj in range(16):
            for i in range(3):
                bd_build(BD[i](None, jj), wsm[i], jj)
        L.top = B3.off
        wif = L.alloc([48, 32], F32)
        for dd in range(2):
            for q4 in range(4):
                self.dma("sp", wif(None, (q4 * 12, q4 * 12 + 12), (dd * 16, dd * 16 + 16)),
                         d["ml_wif"][j][dd].rearrange("(k p) g -> p k g", p=128)[:, q4 * 12:(q4 + 1) * 12, :])
        bdt = [L.alloc([128], F32) for _ in range(3)]
        for jj in range(16):
            for i in range(3):
                bd_build(bdt[i](), wsm[3 + i], jj)
            p1 = self.ps()
            self.mm(p1(None, (0, 32)), bdt[0](), wif(None, jj), start=True, stop=False)
            self.mm(p1(None, (0, 32)), bdt[1](), wif(None, 16 + jj), start=False, stop=True)
            self.copy("dve", Wc(None, jj), p1(None, (0, 32)))
            p2 = self.ps()
            self.mm(p2(None, (0, 32)), bdt[2](), wif(None, 32 + jj))
            self.copy("dve", Wm(None, jj), p2(None, (0, 32)))
        L.top = setup_top
        for cc in range(2):
            self.memset("dve", B1(None, cc, (0, 2)), 0.0)
            self.memset("dve", B1(None, cc, (258, 260)), 0.0)
            self.memset("dve", B1(None, cc, (2308, XW)), 0.0)
        self.memset("dve", B6(None, None, (256, 258)), 1.0)

        win = d["ml_win"][j].rearrange("(k p) n -> p k n", p=128)
        dcnt = [0]
        pending = []

        def features(w, cc, jj, gates_first):
            for ni, (n0, n1) in enumerate(ntiles):
                n = n1 - n0
                pm = self.ps()
                for k in range(8):
                    self.mm(pm(None, (0, n)), w(None, k, (cc * 128, cc * 128 + 128)), self.hT(None, k, (n0, n1)),
                            start=(k == 0), stop=(k == 7))
                self.copy("act", B1(None, cc, (PO(n0), PO(n0) + n)), pm(None, (0, n)))
            while pending:
                pending.pop(0)()
            dg = dgc[0]
            dcnt[0] += 1
            for tap in range(5):
                self.ts("dve", dg(None, tap), self.identb(), cw(None, jj, (tap, tap + 1)), ALU.mult)
            for ni, (n0, n1) in enumerate(ntiles):
                n = n1 - n0
                pc = self.ps()
                for tap in range(5):
                    c0 = PO(n0) + tap - 2
                    self.mm(pc(None, (0, n)), dg(None, tap), B1(None, cc, (c0, c0 + n)),
                            start=(tap == 0), stop=(tap == 4))
                ta = tA[ni % 2]
                tb = tB[ni % 2]
                self.ts("dve", ta(None, (0, n)), pc(None, (0, n)), 0.5, ALU.mult, hbc(None, (jj, jj + 1)), ALU.add)
                self.act(tb(None, (0, n)), pc(None, (0, n)), AF.Tanh, scale=0.5, bias=hbc(None, (jj, jj + 1)))
                self.stt(B2(None, cc, (n0, n1)), tb(None, (0, n)), 1.0, ta(None, (0, n)), ALU.add, ALU.mult)
            if gates_first is not None:
                def gates(cc=cc, jj=jj, gates_first=gates_first):
                    for ni, (n0, n1) in enumerate(ntiles):
                        n = n1 - n0
                        pg = self.ps()
                        R32 = (0, 32)
                        self.mm(pg(R32, (0, n)), Wc(None, jj), B2(None, cc, (n0, n1)), start=True, stop=False)
                        self.mm(pg(R32, (0, n)), Wm(None, jj), B1(None, cc, (PO(n0), PO(n0) + n)), start=False, stop=True)
                        if gates_first:
                            self.ts("dve", gT(R32, (n0, n1)), pg(R32, (0, n)), bif(R32), ALU.add)
                        else:
                            self.tt("dve", gT(R32, (n0, n1)), pg(R32, (0, n)), gT(R32, (n0, n1)), ALU.add)
                pending.append(gates)

        wcnt = 0
        for hh in range(8):
            w = wxm[wcnt % 2]
            wcnt += 1
            self.dma("pool", w(), win[:, :, hh * 256:(hh + 1) * 256])
            for cc in range(2):
                features(w, cc, hh * 2 + cc, gates_first=(hh == 0 and cc == 0))
        while pending:
            pending.pop(0)()
        R32 = (0, 32)
        pgA = self.ps()
        pgB = self.ps()
        for c in range(NT):
            dst = pgA(None, (c * 32, c * 32 + 32)) if c < 16 else pgB(None, ((c - 16) * 32, (c - 16) * 32 + 32))
            idn = X(self.ident.ap[0:32, 0:32], self.ident().k)
            self.tr(dst, gT(R32, (c * 128, c * 128 + 128)), idn)
        self.copy("dve", X(gtok.ap[:, 0:16, :], gtok(None, (0, 16)).k),
                  X(pgA.ap.rearrange("p (c g) -> p c g", g=32), pgA().k))
        self.copy("dve", X(gtok.ap[:, 16:18, :], gtok(None, (16, 18)).k),
                  X(pgB.ap[:, 0:64].rearrange("p (c g) -> p c g", g=32), pgB(None, (0, 64)).k))
        for dd in range(2):
            li_x = X(gtok.ap[:, :, dd * 16:dd * 16 + 8], gtok().k)
            fp_x = X(gtok.ap[:, :, dd * 16 + 8:dd * 16 + 16], gtok().k)
            sp3 = X(spc.ap.rearrange("p (c g) -> p c g", g=8), spc().k)
            tm3 = X(tmpg.ap.rearrange("p (c g) -> p c g", g=8), tmpg().k)
            self.act(sp3, fp_x, AF.Exp, scale=-1.0)
            self.act(spc(), spc(), AF.Ln, bias=self.c_one)
            tri = self.maskF if dd == 0 else self.maskB
            pcum = self.ps()
            ptot = self.ps()
            NG = NT * 8
            self.mm(pcum(None, (0, NG)), tri(), spc())
            self.mm(ptot(None, (0, NG)), self.ones(), spc())
            cum3 = X(pcum.ap[:, 0:NG].rearrange("p (c g) -> p c g", g=8), pcum(None, (0, NG)).k)
            self.tt("dve", tm3, cum3, li_x, ALU.add)
            self.act(alpha[dd](), tm3, AF.Exp)
            self.act(ib[dd](), cum3, AF.Exp, bias=self.c_ln16)
            tot3 = X(ptot.ap[:, 0:NG].rearrange("p (c g) -> p c g", g=8), ptot(None, (0, NG)).k)
            self.act(decay[dd](), tot3, AF.Exp, scale=-1.0)
            self.tt("pool", aend[dd](), decay[dd](), alpha[dd](), ALU.mult)

        order = [list(range(NT)), [1, 0] + list(range(NT - 1, 1, -1))]
        c_lo = 0 if need_ctx else 2
        slot = [0]
        def load_head_w(h2):
            w_ = wxm[(wcnt0 + h2) % 2]
            self.dma("pool", w_(), win[:, :, h2 * 256:(h2 + 1) * 256])
            wz_ = wzo[h2 % 2]
            self.dma("pool", wz_(None, None, (0, 256)), win[:, :, 2048 + h2 * 256: 2048 + (h2 + 1) * 256])
            self.dma("pool", wz_(None, None, (256, 512)), win[:, :, 4096 + h2 * 256: 4096 + (h2 + 1) * 256])

        wcnt0 = wcnt
        load_head_w(0)
        for hh in range(8):
            w = wxm[(wcnt0 + hh) % 2]
            wz = wzo[hh % 2]
            for cc in range(2):
                features(w, cc, hh * 2 + cc, gates_first=None)
            for cc in range(2):
                jj = hh * 2 + cc
                for (dstB, bd) in ((B3, BD[0]), (B4, BD[1])):
                    for ni, (n0, n1) in enumerate(ntiles):
                        n = n1 - n0
                        pq = self.ps()
                        self.mm(pq(None, (0, n)), bd(None, jj), B2(None, cc, (n0, n1)))
                        if ni % 2 == 0:
                            self.copy("act", dstB(None, cc, (n0, n1)), pq(None, (0, n)))
                        else:
                            self.copy("dve", dstB(None, cc, (n0, n1)), pq(None, (0, n)))
            for c in range(0, NT, 2):
                pk = self.ps()
                pv = self.ps()
                for u in range(2):
                    for cc in range(2):
                        jj = hh * 2 + cc
                        t0 = (c + u) * 128
                        self.mm(pk(None, (u * 256 + cc * 128, u * 256 + cc * 128 + 128)),
                                B2(None, cc, (t0, t0 + 128)), BD[1](None, jj))
                        self.mm(pv(None, (u * 256 + cc * 128, u * 256 + cc * 128 + 128)),
                                B1(None, cc, (PO(t0), PO(t0) + 128)), BD[2](None, jj))
                self.copy("act", B5(None, (c, c + 2)), X(pk.ap.rearrange("p (u f) -> p u f", f=256), pk().k))
                self.copy("dve", B6(None, (c, c + 2), (0, 256)), X(pv.ap.rearrange("p (u f) -> p u f", f=256), pv().k))
            for cc in range(2):
                self.ts("dve", B2(None, cc), B2(None, cc), skc(None, (hh * 2 + cc, hh * 2 + cc + 1)), ALU.mult)
            def front(step, dd):
                c = order[dd][step]
                sl = (step % 2) * 2 + dd
                t0 = c * 128
                pS = self.psbank[dd]
                for e in range(2):
                    self.mm(pS(None, (0, 128)), B4(None, e, (t0, t0 + 128)), B3(None, e, (t0, t0 + 128)),
                            start=(e == 0), stop=(e == 1))
                msk = self.maskF if dd == 0 else self.maskB
                self.stt(AT[sl](), pS(None, (0, 128)), alpha[dd](None, c, (hh, hh + 1)), msk(), ALU.mult, ALU.mult)
                self.act(khat[sl](), B5(None, c), AF.Copy, scale=aend[dd](None, c, (hh, hh + 1)))

            def back(step, dd):
                c = order[dd][step]
                first = (step == 0)
                sl = (step % 2) * 2 + dd
                t0 = c * 128
                po = self.psbank[2 + dd]
                self.mm(po(None, (0, 257)), AT[sl](), B6(None, c, (0, 257)), start=True, stop=first)
                if not first:
                    for e in range(2):
                        self.mm(po(None, (0, 257)), B3(None, e, (t0, t0 + 128)), Cbf[dd](None, e, (0, 257)),
                                start=False, stop=(e == 1))
                if step < NT - 1:
                    pU = self.pspair[2 + dd]
                    for e in range(2):
                        self.mm(pU(None, (e * 512, e * 512 + 257)), khat[sl](None, (e * 128, e * 128 + 128)),
                                B6(None, c, (0, 257)))
                    u3 = X(pU.ap.rearrange("p (e f) -> p e f", f=512)[:, :, 0:257], pU().k)
                    c3 = X(C32[dd].ap[:, :, 0:257], C32[dd]().k)
                    if first:
                        self.copy("dve", c3, u3)
                    else:
                        self.stt(c3, c3, decay[dd](None, c, (hh, hh + 1)), u3, ALU.mult, ALU.add)
                    self.copy("pool", Cbf[dd](), C32[dd]())
                if c >= c_lo:
                    m = mcol[sl]
                    self.act(m(None, (0, 1)), po(None, (256, 257)), AF.Abs)
                    self.ts("dve", m(None, (0, 1)), m(None, (0, 1)), ib[dd](None, c, (hh, hh + 1)), ALU.max)
                    self.recip(m(None, (1, 2)), m(None, (0, 1)))
                    hdst = HF if dd == 0 else HBv
                    self.act(hdst(None, c), po(None, (0, 256)), AF.Copy, scale=m(None, (1, 2)))

            if hh + 1 < 8:
                load_head_w(hh + 1)
            front(0, 0)
            front(0, 1)
            for step in range(NT):
                for dd in range(2):
                    if step + 1 < NT:
                        front(step + 1, dd)
                    back(step, dd)
            for c in range(c_lo, NT):
                t0 = c * 128
                pog = self.ps()
                for k in range(8):
                    self.mm(pog(None, (0, 256)), self.hT(None, k, (t0, t0 + 128)), wz(None, k, (256, 512)),
                            start=(k == 0), stop=(k == 7))
                ta = tA[c % 2]
                tb = tB[c % 2]
                self.act(ta(None, (0, 256)), pog(None, (0, 256)), AF.Tanh, scale=0.5)
                self.tt("pool", tb(None, (0, 256)), HF(None, c), HBv(None, c), ALU.add)
                self.stt(HF(None, c), ta(None, (0, 256)), 1.0, tb(None, (0, 256)), ALU.add, ALU.mult)
                self.act(self.junk(None, (0, 256)), HF(None, c), AF.Square, accum=ssqh(None, (c, c + 1)))
            for cc in range(2):
                for ni, (n0, n1) in enumerate(ntiles):
                    n = n1 - n0
                    if n1 <= 256 and not need_ctx:
                        continue
                    pz = self.ps()
                    for k in range(8):
                        self.mm(pz(None, (0, n)), wz(None, k, (cc * 128, cc * 128 + 128)), self.hT(None, k, (n0, n1)),
                                start=(k == 0), stop=(k == 7))
                    tb = tB[ni % 2]
                    self.act(tb(None, (0, n)), pz(None, (0, n)), AF.Tanh, scale=0.5)
                    self.stt(B4(None, cc, (n0, n1)), tb(None, (0, n)), 1.0, pz(None, (0, n)), ALU.add, ALU.mult)
            self.act(rrh(None, (c_lo, NT)), ssqh(None, (c_lo, NT)), AF.Ln, scale=1.0 / 256, bias=self.c_eps4)
            self.act(rrh(None, (c_lo, NT)), rrh(None, (c_lo, NT)), AF.Exp, scale=-0.5)
            for c in range(c_lo, NT):
                t0 = c * 128
                hn = hnb[c % 2]
                self.act(hn(), HF(None, c), AF.Copy, scale=rrh(None, (c, c + 1)))
                pT = self.psbf[(self.psrr) % 8]
                self.psrr += 1
                for cc in range(2):
                    self.tr(pT(None, (cc * 128, cc * 128 + 128)), hn(None, (cc * 128, cc * 128 + 128)), self.identb())
                pre = preb[c % 2]
                for cc in range(2):
                    jj = hh * 2 + cc
                    u = utmp[cc]
                    self.stt(u(), pT(None, (cc * 128, cc * 128 + 128)), hgc(None, (jj, jj + 1)),
                             B2(None, cc, (t0, t0 + 128)), ALU.mult, ALU.add)
                    self.tt("pool", pre(None, cc), u(), B4(None, cc, (t0, t0 + 128)), ALU.mult)
                self.dma("sp", d["ptd"][c][:, hh * 2:hh * 2 + 2, :], pre(), out_keys=[("ptd", c)])


def _rope_tables():
    rows = T_LAT // 64
    row = np.repeat(np.arange(rows), 64).astype(np.float32)
    col = np.tile(np.arange(64), rows).astype(np.float32)
    inv = (10000.0 ** (-np.arange(8, dtype=np.float32) / 8)).astype(np.float32)
    cr = np.cos(row[:, None] * inv)
    sr = np.sin(row[:, None] * inv)
    cc = np.cos(col[:, None] * inv)
    sc = np.sin(col[:, None] * inv)
    C = np.concatenate([cr, cr, cc, cc], axis=1).T
    Sg = np.concatenate([-sr, sr, -sc, sc], axis=1).T
    Cf = np.concatenate([np.ones((32, T_CTX), np.float32), C], axis=1).astype(np.float32)
    Sf = np.concatenate([np.zeros((32, T_CTX), np.float32), Sg], axis=1).astype(np.float32)
    tq = np.concatenate([Cf, Sf, Cf, Sf], axis=0)
    tkc = np.concatenate([Cf] * 4, axis=0)
    tks = np.concatenate([Sf] * 4, axis=0)
    return np.ascontiguousarray(tq), np.ascontiguousarray(tkc), np.ascontiguousarray(tks)


PERM_A = np.arange(32)
PERM_B = np.concatenate([np.arange(8, 16), np.arange(0, 8), np.arange(24, 32), np.arange(16, 24)])


def _col(v, nch):
    s = v.shape[:-1]
    return np.ascontiguousarray(np.swapaxes(v.reshape(s + (nch, 128)), -1, -2))


def prep_shared(inp):
    f = lambda a: np.ascontiguousarray(np.asarray(a, dtype=np.float32))
    o = {}
    o["c_ctx"] = f(inp["c_ctx"])
    o["ada_w"] = f(inp["ada_w"])
    o["ada_bc"] = _col(f(inp["ada_b"]), 24)
    o["npre_c"] = _col(f(inp["norm_pre"]), 8)
    o["npost"] = f(inp["norm_post"])
    w = f(inp["mla_w_in"])
    kr = w[:, :, 512:544]
    krA = np.tile(kr[:, :, PERM_A], (1, 1, 4))
    krB = np.tile(kr[:, :, PERM_B], (1, 1, 4))
    krO = np.tile(kr, (1, 1, 4))
    o["mla_win"] = np.ascontiguousarray(np.concatenate([w[:, :, 0:512], krA, krB, krO, w[:, :, 544:]], axis=2))
    o["mla_qn_c"] = _col(f(inp["mla_q_norm"]), 2)
    o["mla_kvn_c"] = _col(f(inp["mla_kv_norm"]), 2)
    wq = f(inp["mla_w_uq"]).reshape(2, 256, NH, 96)
    rope = wq[..., 64:]
    o["mla_wuq"] = np.ascontiguousarray(
        np.concatenate([wq[..., :64], rope[..., PERM_A], rope[..., PERM_B]], axis=-1).reshape(2, 256, 2048))
    o["mla_wukv"] = f(inp["mla_w_ukv"])
    o["mla_wout"] = f(inp["mla_w_out"])
    o["ml_win"] = f(inp["ml_w_in"])
    o["ml_cw_c"] = np.ascontiguousarray(np.transpose(f(inp["ml_conv_w"]).reshape(2, 5, 16, 128), (0, 3, 2, 1)))
    o["ml_cb_c"] = _col(f(inp["ml_conv_b"]), 16)
    for nm, key in (("ml_wq", "ml_w_q"), ("ml_wk", "ml_w_k"), ("ml_wv", "ml_w_v")):
        wqk = f(inp[key])
        a = wqk.reshape(2, 16, 32, 4, 4)
        o[nm + "_c"] = np.ascontiguousarray(np.transpose(a, (0, 2, 3, 1, 4)).reshape(2, 128, 16, 4))
        o[nm + "T_c"] = np.ascontiguousarray(np.transpose(a, (0, 2, 4, 1, 3)).reshape(2, 128, 16, 4))
    o["ml_wif"] = f(inp["ml_w_if"])
    o["ml_bif_c"] = np.ascontiguousarray(f(inp["ml_b_if"]).reshape(2, 32, 1))
    o["ml_hn_c"] = _col(f(inp["ml_head_norm"]), 16)
    o["ml_sk_c"] = _col(f(inp["ml_skip"]), 16)
    o["ml_wout"] = f(inp["ml_w_out"])
    o["k_ident"] = np.eye(128, dtype=np.float32)
    o["k_maskF"] = np.triu(np.ones((128, 128), np.float32))
    o["k_maskB"] = np.tril(np.ones((128, 128), np.float32))
    o["k_bdmask"] = np.kron(np.eye(32, dtype=np.float32), np.ones((4, 4), np.float32))
    o["k_tq"], o["k_tkc"], o["k_tks"] = _rope_tables()
    return o


_CACHE = {}


def kernel(**inputs):
    n_cores = 8
    NB = 2
    key = ("full",)
    if key not in _CACHE:
        _CACHE[key] = Prog(NB, [0, 1, 2, 3]).build()
    nc = _CACHE[key]
    shared = prep_shared(inputs)
    x = np.asarray(inputs["x"], dtype=np.float32)
    c = np.asarray(inputs["c"], dtype=np.float32)
    ctx = np.asarray(inputs["ctx"], dtype=np.float32)
    in_maps = []
    for i in range(n_cores):
        m = dict(shared)
        m["x"] = np.ascontiguousarray(x[i * NB:(i + 1) * NB])
        m["c"] = np.ascontiguousarray(c[i * NB:(i + 1) * NB])
        m["ctx"] = np.ascontiguousarray(ctx[i * NB:(i + 1) * NB])
        in_maps.append(m)
    res = run_bass_kernel_spmd(nc, in_maps, core_ids=list(range(n_cores)))
    return np.concatenate([np.asarray(r["out"]) for r in res.results], axis=0).astype(np.float32)
```

```python
import math
from contextlib import ExitStack

import numpy as np
import concourse.bass as bass
import concourse.mybir as mybir
from concourse.bass_utils import run_bass_kernel_spmd

F32 = mybir.dt.float32
BF16 = mybir.dt.bfloat16
ALU = mybir.AluOpType
AF = mybir.ActivationFunctionType
AX = mybir.AxisListType

D = 1024
T_LAT = 2048
T_CTX = 256
TA = T_LAT + T_CTX
NT = TA // 128
EPS = 1e-6
NH = 16
ATT_SCALE = 96 ** -0.5
LN16 = math.log(16.0)

EPOCH = 4096
NSLOT = 8
COMPUTE = ("pe", "act", "dve", "pool")
ENGINES = ("pe", "act", "dve", "pool", "sp")


class Op:
    __slots__ = ("eng", "fn", "idx", "is_dma", "signal", "sem", "val", "waits",
                 "eidx", "slot", "prev_slot_wait")


class Sched:
    def __init__(self):
        self.ops = []
        self.by_eng = {e: [] for e in ENGINES}
        self.last_writer = {}
        self.readers = {}
        self.waited = {}
        self.dma_waited = {e: set() for e in ENGINES}
        self.dma_count = {e: 0 for e in ENGINES}
        self.slot_last = {}

    def _add(self, eng, fn, reads, writes, is_dma):
        op = Op()
        op.eng = eng
        op.fn = fn
        op.is_dma = is_dma
        op.signal = is_dma
        op.sem = None
        op.val = None
        op.waits = []
        op.slot = None
        op.prev_slot_wait = None
        op.idx = len(self.ops)
        op.eidx = len(self.by_eng[eng])
        self.ops.append(op)
        self.by_eng[eng].append(op)
        psr = [k for k in reads if k[0] == "ps"]
        if psr:
            writes = list(writes) + psr
        lw = self.last_writer
        rd = self.readers
        deps = set()
        same_raw = None
        for k in reads:
            w = lw.get(k)
            if w is not None:
                deps.add(w)
                if w.eng == eng and eng != "pe" and not w.is_dma:
                    if same_raw is None or w.eidx > same_raw.eidx:
                        same_raw = w
        for k in writes:
            w = lw.get(k)
            if w is not None:
                deps.add(w)
            r = rd.get(k)
            if r:
                deps.update(r)
        best = {}
        for d in deps:
            if d is op:
                continue
            if d.is_dma:
                if d.idx not in self.dma_waited[eng]:
                    self.dma_waited[eng].add(d.idx)
                    op.waits.append(d)
            else:
                if d.eng == eng:
                    continue
                b = best.get(d.eng)
                if b is None or d.eidx > b.eidx:
                    best[d.eng] = d
        if same_raw is not None and not is_dma:
            best[eng] = same_raw
        for pe, d in best.items():
            key = (eng, pe)
            if self.waited.get(key, -1) >= d.eidx:
                continue
            self.waited[key] = d.eidx
            d.signal = True
            op.waits.append(d)
        if is_dma:
            n = self.dma_count[eng]
            self.dma_count[eng] = n + 1
            op.slot = (eng, n % NSLOT)
            prev = self.slot_last.get(op.slot)
            if prev is not None and prev.idx not in self.dma_waited[eng]:
                self.dma_waited[eng].add(prev.idx)
                op.prev_slot_wait = prev
            self.slot_last[op.slot] = op
        for k in reads:
            r = rd.get(k)
            if r is None:
                rd[k] = [op]
            else:
                if not op.is_dma:
                    r[:] = [x for x in r if x.is_dma or x.eng != eng]
                r.append(op)
        for k in writes:
            lw[k] = op
            rd[k] = []
        return op

    def op(self, eng, fn, reads=(), writes=()):
        return self._add(eng, fn, reads, writes, False)

    def dma(self, eng, fn, reads=(), writes=()):
        return self._add(eng, fn, reads, writes, True)

    def emit(self, nc, stack):
        sems = {}

        def get_sem(name):
            if name not in sems:
                sems[name] = stack.enter_context(nc.semaphore(name))
            return sems[name]

        for e in COMPUTE:
            cnt = 0
            for op in self.by_eng[e]:
                if op.is_dma or not op.signal:
                    continue
                op.sem = get_sem("s_%s_%d" % (e, cnt // EPOCH))
                op.val = cnt % EPOCH + 1
                cnt += 1
        slot_cnt = {}
        for op in self.ops:
            if op.is_dma:
                c = slot_cnt.get(op.slot, 0) + 1
                slot_cnt[op.slot] = c
                op.sem = get_sem("d_%s_%d" % op.slot)
                op.val = 16 * c
        finals = list(self.slot_last.values())
        block = stack.enter_context(nc.Block())

        def run(eng):
            def body(h):
                for op in self.by_eng[eng]:
                    if op.prev_slot_wait is not None:
                        p = op.prev_slot_wait
                        h.wait_ge(p.sem, p.val)
                    for d in op.waits:
                        h.wait_ge(d.sem, d.val)
                    ins = op.fn(h)
                    if op.signal:
                        ins.then_inc(op.sem, 16 if op.is_dma else 1)
                if eng == "sp":
                    for o in finals:
                        h.wait_ge(o.sem, o.val)
            return body

        block.tensor(run("pe"))
        block.scalar(run("act"))
        block.vector(run("dve"))
        block.gpsimd(run("pool"))
        block.sync(run("sp"))


GRAN = 256


class X:
    __slots__ = ("ap", "k")

    def __init__(self, ap, k):
        self.ap = ap
        self.k = k


class View:
    def __init__(self, space, base_ap, off, shape, esz, gran=GRAN):
        self.space = space
        self.ap = base_ap
        self.off = off
        self.shape = list(shape)
        self.esz = esz
        self.gran = gran
        st = []
        s = 1
        for n in reversed(self.shape):
            st.append(s)
            s *= n
        self.strides = list(reversed(st))
        self.nelem = s

    def __call__(self, pr=None, *idx):
        p0, p1 = (0, 128) if pr is None else pr
        sl = [slice(p0, p1)]
        lo = 0
        hi = 0
        for d, n in enumerate(self.shape):
            it = idx[d] if d < len(idx) else None
            if it is None:
                a, b = 0, n
                sl.append(slice(None))
            elif isinstance(it, int):
                a, b = it, it + 1
                sl.append(it)
            else:
                a, b = it
                sl.append(slice(a, b))
            lo += a * self.strides[d]
            hi += (b - 1) * self.strides[d]
        hi += 1
        b0 = self.off + lo * self.esz
        b1 = self.off + hi * self.esz
        halves = []
        if p0 < 64:
            halves.append(0)
        if p1 > 64:
            halves.append(1)
        g0 = b0 // self.gran
        g1 = (b1 - 1) // self.gran
        sp = self.space
        if sp == "ps":
            keys = [(sp, g) for g in range(g0, g1 + 1)]
        else:
            keys = [(sp, g, h) for g in range(g0, g1 + 1) for h in halves]
        return X(self.ap[tuple(sl)], keys)


class Arena:
    def __init__(self, nc, st, nbytes):
        self.nbytes = nbytes
        self.t = st.enter_context(nc.sbuf_tensor("arena", [128, nbytes // 2], BF16))
        self.top = 0

    def view(self, off, shape, dtype):
        esz = 4 if dtype == F32 else 2
        n = 1
        for s in shape:
            n *= s
        assert off % 4 == 0
        assert off + n * esz <= self.nbytes, ("arena overflow", off, n * esz, self.nbytes)
        ap = self.t[:, off // 2: off // 2 + n * esz // 2]
        if dtype == F32:
            ap = ap.bitcast(F32)
        if len(shape) > 1:
            names = " ".join("d%d" % i for i in range(len(shape)))
            kw = {"d%d" % i: shape[i] for i in range(1, len(shape))}
            ap = ap.rearrange("p (%s) -> p %s" % (names, names), **kw)
        return View("sb", ap, off, shape, esz)


class Bump:
    def __init__(self, arena, start, limit):
        self.a = arena
        self.top = start
        self.limit = limit
        self.peak = start

    def alloc(self, shape, dtype, align=GRAN):
        esz = 4 if dtype == F32 else 2
        n = esz
        for s in shape:
            n *= s
        off = (self.top + align - 1) // align * align
        self.top = off + n
        assert self.top <= self.limit, ("bump overflow", self.top, self.limit)
        self.peak = max(self.peak, self.top)
        return self.a.view(off, shape, dtype)


class Prog:
    def __init__(self, NB, layers, debug=None, stop=None):
        self.stop = stop
        self.NB = NB
        self.layers = layers
        self.debug = debug
        self.nc = bass.Bass("TRN2", target_bir_lowering=False)
        self.S = Sched()
        self.psrr = 0

    def mm(self, out, lhsT, rhs, start=True, stop=True):
        rk = lhsT.k + rhs.k
        if not start:
            rk = rk + out.k
        o, l, r = out.ap, lhsT.ap, rhs.ap
        self.S.op("pe", lambda h: h.matmul(o, lhsT=l, rhs=r, start=start, stop=stop),
                  reads=rk, writes=out.k)

    def tr(self, out, in_, ident):
        o, i, d = out.ap, in_.ap, ident.ap
        self.S.op("pe", lambda h: h.transpose(o, i, d), reads=in_.k + ident.k, writes=out.k)

    def act(self, out, in_, func, scale=1.0, bias=0.0, accum=None):
        rk = list(in_.k)
        sc = scale
        bi = bias
        if isinstance(scale, X):
            rk += scale.k
            sc = scale.ap
        if isinstance(bias, X):
            rk += bias.k
            bi = bias.ap
        wk = list(out.k)
        ac = None
        if accum is not None:
            wk += accum.k
            ac = accum.ap
        o, i = out.ap, in_.ap
        if ac is None:
            fn = lambda h: h.activation(out=o, in_=i, func=func, bias=bi, scale=sc)
        else:
            fn = lambda h: h.activation(out=o, in_=i, func=func, bias=bi, scale=sc, accum_out=ac)
        self.S.op("act", fn, reads=rk, writes=wk)

    def tt(self, eng, out, in0, in1, op):
        o, a, b = out.ap, in0.ap, in1.ap
        self.S.op(eng, lambda h: h.tensor_tensor(out=o, in0=a, in1=b, op=op),
                  reads=in0.k + in1.k, writes=out.k)

    def ts(self, eng, out, in0, s1, op0, s2=None, op1=None):
        rk = list(in0.k)
        a1 = s1
        a2 = s2
        if isinstance(s1, X):
            rk += s1.k
            a1 = s1.ap
        if isinstance(s2, X):
            rk += s2.k
            a2 = s2.ap
        o, i = out.ap, in0.ap
        if op1 is None:
            fn = lambda h: h.tensor_scalar(out=o, in0=i, scalar1=a1, scalar2=None, op0=op0)
        else:
            fn = lambda h: h.tensor_scalar(out=o, in0=i, scalar1=a1, scalar2=a2, op0=op0, op1=op1)
        self.S.op(eng, fn, reads=rk, writes=out.k)

    def stt(self, out, in0, scalar, in1, op0, op1):
        rk = in0.k + in1.k
        sc = scalar
        if isinstance(scalar, X):
            rk = rk + scalar.k
            sc = scalar.ap
        o, a, b = out.ap, in0.ap, in1.ap
        self.S.op("dve", lambda h: h.scalar_tensor_tensor(out=o, in0=a, scalar=sc, in1=b, op0=op0, op1=op1),
                  reads=rk, writes=out.k)

    def copy(self, eng, out, in_):
        o, i = out.ap, in_.ap
        if eng == "act":
            self.S.op("act", lambda h: h.activation(out=o, in_=i, func=AF.Copy), reads=in_.k, writes=out.k)
        else:
            self.S.op(eng, lambda h: h.tensor_copy(out=o, in_=i), reads=in_.k, writes=out.k)

    def recip(self, out, in_, fast=False):
        o, i = out.ap, in_.ap
        if fast:
            self.S.op("dve", lambda h: h.reciprocal_approx_fast(out=o, in_=i), reads=in_.k, writes=out.k)
        else:
            self.S.op("dve", lambda h: h.reciprocal(out=o, in_=i), reads=in_.k, writes=out.k)

    def memset(self, eng, out, val):
        o = out.ap
        self.S.op(eng, lambda h: h.memset(o, val), writes=out.k)

    def dma(self, eng, out, in_, out_keys=None, in_keys=None, **kw):
        ok = out.k if isinstance(out, X) else (out_keys or [])
        ik = in_.k if isinstance(in_, X) else (in_keys or [])
        o = out.ap if isinstance(out, X) else out
        i = in_.ap if isinstance(in_, X) else in_
        self.S.dma(eng, lambda h: h.dma_start(out=o, in_=i, **kw), reads=ik, writes=ok)

    def dump(self, name, view, nrow_chunks=1):
        if not (self.debug and name in self.debug):
            return
        tmpf = self.L.a.view(self.L.a.nbytes - TA * 4, [TA], F32)
        for k in range(nrow_chunks):
            src = view(None, k) if nrow_chunks > 1 else view()
            self.copy("dve", tmpf(), src)
            self.dma("sp", self.d[name][k * 128:(k + 1) * 128, :], tmpf())

    def barrier(self):
        S = self.S
        toks = []
        for e in ("pe", "act", "dve", "pool"):
            k = ("bar", e)
            toks.append(k)
            S.op(e, lambda h: h.nop(), reads=[], writes=[k])
        outstanding = [("bar_dma", o.idx) for o in S.slot_last.values()]
        for o in list(S.slot_last.values()):
            S.last_writer[("bar_dma", o.idx)] = o
        for e in ("pe", "act", "dve", "pool"):
            S.op(e, lambda h: h.nop(), reads=toks + outstanding, writes=[])
        S.op("sp", lambda h: h.nop(), reads=toks + outstanding, writes=[("bar", "sp")])

    def ps(self, n=1):
        if n == 2:
            if self.psrr % 2:
                self.psrr += 1
            i = (self.psrr // 2) % len(self.pspair)
            self.psrr += 2
            return self.pspair[i]
        i = self.psrr % len(self.psbank)
        self.psrr += 1
        return self.psbank[i]

    def build(self):
        nc = self.nc
        NB = self.NB
        dt = nc.dram_tensor
        I = "ExternalInput"
        self.d = d = {}

        def inp(name, shape, dtype=F32):
            d[name] = dt(name, list(shape), dtype, kind=I).ap()

        inp("x", [NB, T_LAT, D])
        inp("c", [NB, D])
        inp("ctx", [NB, T_CTX, D])
        inp("c_ctx", [D])
        inp("ada_w", [4, D, 3 * D])
        inp("ada_bc", [4, 128, 24])
        inp("npre_c", [4, 128, 8])
        inp("npost", [4, D])
        inp("mla_win", [2, D, 1920])

        inp("mla_qn_c", [2, 128, 2])
        inp("mla_kvn_c", [2, 128, 2])
        inp("mla_wuq", [2, 256, 2048])
        inp("mla_wukv", [2, 256, 2048])
        inp("mla_wout", [2, D, D])
        inp("ml_win", [2, D, 6144])
        inp("ml_cw_c", [2, 128, 16, 5])
        inp("ml_cb_c", [2, 128, 16])
        inp("ml_wq_c", [2, 128, 16, 4])
        inp("ml_wk_c", [2, 128, 16, 4])
        inp("ml_wv_c", [2, 128, 16, 4])
        inp("ml_wqT_c", [2, 128, 16, 4])
        inp("ml_wkT_c", [2, 128, 16, 4])
        inp("ml_wvT_c", [2, 128, 16, 4])
        inp("ml_wif", [2, 2, 6144, 16])
        inp("ml_bif_c", [2, 32, 1])
        inp("ml_hn_c", [2, 128, 16])
        inp("ml_sk_c", [2, 128, 16])
        inp("ml_wout", [2, 2 * D, D])
        inp("k_ident", [128, 128])
        inp("k_maskF", [128, 128])
        inp("k_maskB", [128, 128])
        inp("k_bdmask", [128, 128])
        inp("k_tq", [128, TA])
        inp("k_tkc", [128, TA])
        inp("k_tks", [128, TA])
        d["out"] = dt("out", [NB, T_LAT, D], F32, kind="ExternalOutput").ap()
        d["ctxs"] = dt("ctxs", [NB, T_CTX, D], F32, kind="Internal").ap()
        d["ptd"] = dt("ptd", [NT, 128, 16, 128], BF16, kind="Internal").ap()
        if self.debug:
            for name, shape in self.debug.items():
                d[name] = dt(name, list(shape), F32, kind="ExternalOutput").ap()

        with ExitStack() as st:
            self.st = st
            ARENA_BYTES = 200 * 1024
            self.arena = Arena(nc, st, ARENA_BYTES)
            pp = [st.enter_context(nc.psum_tensor("pp%d" % i, [128, 1024], F32)) for i in range(4)]
            self.pspair = []
            self.psbank = []
            for i in range(4):
                self.pspair.append(View("ps", pp[i][:, :], i * 4096, [1024], 4, gran=2048))
                for hh in range(2):
                    self.psbank.append(View("ps", pp[i][:, hh * 512:(hh + 1) * 512],
                                            i * 4096 + hh * 2048, [512], 4, gran=2048))
            self.psbf = []
            for i in range(4):
                for hh in range(2):
                    self.psbf.append(View("ps", pp[i][:, hh * 512:(hh + 1) * 512].bitcast(BF16),
                                          i * 4096 + hh * 2048, [1024], 2, gran=2048))
            self.psrr = 0
            self.psrr2 = 0
            self.setup()
            import os
            if os.environ.get("KBAR"):
                self.barrier()
            if self.stop != "setup":
                for b in range(NB):
                    for li in self.layers:
                        self.layer(b, li)
            self.S.emit(nc, st)
        return nc

    def setup(self):
        d = self.d
        A = self.arena
        P = Bump(A, 0, A.nbytes)
        self.persist = P
        al = P.alloc
        self.ident = al([128], F32)
        self.identb = al([128], BF16)
        self.maskF = al([128], F32)
        self.maskB = al([128], F32)
        self.bdmask = al([128], F32)
        self.ones = al([128], F32)
        self.onesb = al([128], BF16)
        self.modcol = al([4, 24, 3], F32)
        self.adab = al([4, 24], F32)
        self.npre = al([4, 8], F32)
        self.silc = al([8, 3], F32)
        self.hT = al([8, TA], BF16)
        self.junk = al([D], BF16)
        self.small = al([64], F32)
        self.acol = al([2, 8], F32)
        self.bcol = al([2, 8], F32)
        self.gcol = al([2, 8], F32)
        self.ssq_ = al([NT + 2, 8], F32)
        self.rr_ = al([NT + 2, 8], F32)
        self.ssq = lambda pr, cr: self.ssq_(pr, cr[0], (0, 1))
        self.rr = lambda pr, cr: self.rr_(pr, cr[0], (0, 1))
        self.cst = al([4], F32)
        craw = al([8, 3], F32)
        th = al([8, 3], F32)
        wst = [al([8, 128], F32) for _ in range(2)]
        self.layer_base = P.top
        for i_, v_ in enumerate((EPS, 4 * EPS, LN16, 1.0)):
            self.memset("dve", self.cst(None, (i_, i_ + 1)), v_)
        self.c_eps = self.cst(None, (0, 1))
        self.c_eps4 = self.cst(None, (1, 2))
        self.c_ln16 = self.cst(None, (2, 3))
        self.c_one = self.cst(None, (3, 4))
        for v, name in ((self.ident, "k_ident"), (self.maskF, "k_maskF"), (self.maskB, "k_maskB"),
                        (self.bdmask, "k_bdmask")):
            self.dma("sp", v(), d[name])
        self.copy("dve", self.identb(), self.ident())
        self.memset("dve", self.ones(), 1.0)
        self.memset("dve", self.onesb(), 1.0)
        self.dma("sp", self.adab(), d["ada_bc"].rearrange("l p c -> p l c"))
        self.dma("sp", self.npre(), d["npre_c"].rearrange("l p c -> p l c"))
        cc = self.small
        for v in range(3):
            if v < self.NB:
                src = d["c"][v].rearrange("(k p) -> p k", p=128)
            elif v == 2:
                src = d["c_ctx"].rearrange("(k p) -> p k", p=128)
            else:
                src = d["c_ctx"].rearrange("(k p) -> p k", p=128)
            self.S.dma("sp", (lambda o, i: (lambda h: h.dma_start(out=o, in_=i, allow_slow_non_contiguous=True)))(
                craw(None, None, v).ap, src), writes=craw(None, None, v).k)
        self.act(th(), craw(), AF.Tanh, scale=0.5)
        self.stt(self.silc(), th(), 1.0, craw(), ALU.add, ALU.mult)
        self.ts("dve", self.silc(), self.silc(), 0.5, ALU.mult)
        cnt = 0
        for l in range(4):
            if l not in self.layers:
                continue
            for n in range(24):
                w = wst[cnt % 2]
                cnt += 1
                self.dma("sp", w(), d["ada_w"][l].rearrange("(k p) n -> p k n", p=128)[:, :, n * 128:(n + 1) * 128])
                pst = self.ps()
                for k in range(8):
                    self.mm(pst(None, (0, 3)), w(None, k), self.silc(None, k), start=(k == 0), stop=(k == 7))
                self.ts("dve", self.modcol(None, l, n), pst(None, (0, 3)), self.adab(None, l, (n, n + 1)), ALU.add)

    def layer(self, b, li):
        d = self.d
        A = self.arena
        L = Bump(A, self.layer_base, A.nbytes)
        self.L = L
        is_mla = (li % 2 == 0)
        j = li // 2
        need_ctx = li < 3
        first = (li == self.layers[0])
        def xsrc(t):
            if t < 2:
                src = d["ctx"][b] if first else d["ctxs"][b]
                return src[t * 128:(t + 1) * 128, :], ("dram_ctx", b, t)
            src = d["x"][b] if first else d["out"][b]
            return src[(t - 2) * 128:(t - 1) * 128, :], ("dram_x", b, t)

        def xdst(t):
            if t < 2:
                return d["ctxs"][b][t * 128:(t + 1) * 128, :], ("dram_ctx", b, t)
            return d["out"][b][(t - 2) * 128:(t - 1) * 128, :], ("dram_x", b, t)

        for kind, v in ((0, 2), (1, b)):
            sc = self.modcol(None, li, (8, 16), v)
            sh = self.modcol(None, li, (0, 8), v)
            gt = self.modcol(None, li, (16, 24), v)
            self.stt(self.acol(None, kind), sc, 1.0, self.npre(None, li), ALU.add, ALU.mult)
            self.copy("dve", self.bcol(None, kind), sh)
            self.copy("dve", self.gcol(None, kind), gt)
        import os
        if os.environ.get("KSKIP"):
            L.top += int(os.environ["KSKIP"]) * 1024
        self.xg = L.alloc([4, D], F32)
        if self.stop == "A_mod":
            return
        ntl = NT
        if self.stop in ("A_t0", "A_t0b", "A_t0c"):
            ntl = 1
        elif self.stop == "A_n2p":
            ntl = 2
        elif self.stop and self.stop.startswith("A_n"):
            ntl = int(self.stop[3:])
        tl0 = 0
        if self.stop == "A_only1":
            tl0, ntl = 1, 2
        if self.stop == "A_only2":
            tl0, ntl = 2, 3
        for t in range(tl0, ntl):
            if self.stop == "A_n2p":
                self.psrr = 0
            xs = self.xg(None, t % 4)
            src, dk = xsrc(t)
            self.dma("sp", xs, src, in_keys=[dk])
            self.act(self.junk(), xs, AF.Square, accum=self.ssq(None, (t, t + 1)))
            self.act(self.rr(None, (t, t + 1)), self.ssq(None, (t, t + 1)), AF.Ln, scale=1.0 / D, bias=self.c_eps)
            self.act(self.rr(None, (t, t + 1)), self.rr(None, (t, t + 1)), AF.Exp, scale=-0.5)
            import os
            step = int(os.environ.get("KSTEP", "9"))
            if self.stop == "A_t0" or step <= 0:
                continue
            self.ts("dve", xs, xs, self.rr(None, (t, t + 1)), ALU.mult)
            if self.stop == "A_t0b" or step <= 1:
                continue
            kind = 0 if t < 2 else 1
            for kq in range(2):
                pst = self.ps()
                for kk in range(4):
                    k = kq * 4 + kk
                    self.tr(pst(None, (kk * 128, kk * 128 + 128)), self.xg(None, t % 4, (k * 128, k * 128 + 128)),
                            self.ident())
                if step <= 2:
                    continue
                for kk in range(4):
                    k = kq * 4 + kk
                    o = self.hT(None, k, (t * 128, t * 128 + 128))
                    i = pst(None, (kk * 128, kk * 128 + 128))
                    if kq == 0:
                        self.act(o, i, AF.Identity, scale=self.acol(None, kind, (k, k + 1)),
                                 bias=self.bcol(None, kind, (k, k + 1)))
                    else:
                        self.ts("dve", o, i, self.acol(None, kind, (k, k + 1)), ALU.mult,
                                self.bcol(None, kind, (k, k + 1)), ALU.add)
        if self.stop in ("A_t0", "A_t0b", "A_t0c") or (self.stop and (self.stop.startswith("A_n") or self.stop.startswith("A_only"))):
            return
        if self.debug and "dbg_hT" in self.debug:
            tmpf = L.alloc([TA], F32)
            for k in range(8):
                self.copy("dve", tmpf(), self.hT(None, k))
                self.dma("sp", d["dbg_hT"][k * 128:(k + 1) * 128, :], tmpf())
            L.top = self.layer_base

        if self.stop in ("A", "A_nodbg"):
            return
        L.top = self.layer_base
        if is_mla:
            KC = 8
            self.mla(b, li, j, need_ctx)
        else:
            KC = 16
            self.mlstm(b, li, j, need_ctx)

        if self.stop in ("proj", "attn1"):
            return
        if is_mla:
            L.top = self.pt_end
            wout = L.alloc([8, D], BF16)
            for k4 in range(2):
                self.dma("pool", wout(None, (k4 * 4, k4 * 4 + 4)),
                         d["mla_wout"][j].rearrange("(k p) n -> p k n", p=128)[:, k4 * 4:(k4 + 1) * 4, :])
        else:
            L.top = self.layer_base
            wout = L.alloc([16, D], BF16)
            for k4 in range(4):
                self.dma("pool", wout(None, (k4 * 4, k4 * 4 + 4)),
                         d["ml_wout"][j].rearrange("(k p) n -> p k n", p=128)[:, k4 * 4:(k4 + 1) * 4, :])
            ptl = [L.alloc([16, 128], BF16) for _ in range(3)]
        xt = [L.alloc([D], F32) for _ in range(3)]
        t1 = [L.alloc([D], F32) for _ in range(3)]
        self.Gt = L.alloc([2, D], F32)
        self.npost_b = L.alloc([D], F32)
        self.dma("sp", self.npost_b(), d["npost"][li:li + 1, :].broadcast_to([128, D]))
        dg = L.alloc([128], F32)
        for kind in range(2):
            for hh in range(2):
                pst = self.ps()
                for kk in range(4):
                    k = hh * 4 + kk
                    self.ts("dve", dg(), self.ident(), self.gcol(None, kind, (k, k + 1)), ALU.mult)
                    self.mm(pst(None, (kk * 128, kk * 128 + 128)), self.ones(), dg())
                self.tt("dve", self.Gt(None, kind, (hh * 512, hh * 512 + 512)), pst(),
                        self.npost_b(None, (hh * 512, hh * 512 + 512)), ALU.mult)
        t_start = 0 if need_ctx else 2
        for t in range(t_start, NT):
            kind = 0 if t < 2 else 1
            if is_mla:
                lhs = lambda k: self.PT(None, k, (t * 128, t * 128 + 128))
            else:
                pl = ptl[t % 3]
                self.dma("sp", pl(), d["ptd"][t], in_keys=[("ptd", t)])
                lhs = lambda k: pl(None, k)
            y = self.ps(2)
            for hh in range(2):
                for k in range(KC):
                    self.mm(y(None, (hh * 512, hh * 512 + 512)), lhs(k), wout(None, k, (hh * 512, hh * 512 + 512)),
                            start=(k == 0), stop=(k == KC - 1))
            sq = self.ssq(None, (NT, NT + 1))
            r = self.rr(None, (NT, NT + 1))
            sq2 = self.ssq(None, (NT + 1, NT + 2))
            self.act(self.junk(None, (0, 512)), y(None, (0, 512)), AF.Square, accum=sq)
            self.act(self.junk(None, (512, 1024)), y(None, (512, 1024)), AF.Square, accum=sq2)
            self.tt("dve", sq, sq, sq2, ALU.add)
            self.act(r, sq, AF.Ln, scale=1.0 / D, bias=self.c_eps)
            self.act(r, r, AF.Exp, scale=-0.5)
            x_ = xt[t % 3]
            src, dk = xsrc(t)
            self.dma("sp", x_(), src, in_keys=[dk])
            tt_ = t1[t % 3]
            for hh in range(2):
                cs = (hh * 512, hh * 512 + 512)
                self.stt(tt_(None, cs), y(None, cs), r, self.Gt(None, kind, cs), ALU.mult, ALU.mult)
            self.tt("pool", x_(), x_(), tt_(), ALU.add)
            dst, dk = xdst(t)
            self.dma("pool", dst, x_(), out_keys=[dk])

    def mla(self, b, li, j, need_ctx):
        d = self.d
        L = self.L
        al = L.alloc
        self.PT = al([8, TA], BF16)
        self.pt_end = L.top
        cqn = al([2, TA], BF16)
        ckvn = al([2, TA], BF16)
        Qh = [al([TA], BF16) for _ in range(2)]
        Kh = [al([TA], BF16) for _ in range(2)]
        Vh = [al([NT, 128], BF16) for _ in range(2)]
        wuq = al([2, 2048], BF16)
        wukv = al([2, 2048], BF16)
        ws = [al([8, 256], BF16) for _ in range(3)]
        tq = al([TA], BF16)
        tks = [al([512], F32) for _ in range(4)]
        pexp = [al([1024], BF16) for _ in range(4)]
        rd = [al([512], F32) for _ in range(2)]
        thb = [al([512], F32) for _ in range(2)]
        sqb = [al([512], BF16) for _ in range(2)]
        rb = [al([512], F32) for _ in range(2)]
        qn = al([2], F32)
        kvn = al([2], F32)
        tmp_top = L.top

        win = d["mla_win"][j].rearrange("(k p) n -> p k n", p=128)
        self.dma("sp", qn(), d["mla_qn_c"][j])
        self.dma("sp", kvn(), d["mla_kvn_c"][j])
        self.dma("pool", tq(), d["k_tq"])
        self.dma("pool", wuq(), d["mla_wuq"][j].rearrange("(k p) n -> p k n", p=128))
        self.dma("pool", wukv(), d["mla_wukv"][j].rearrange("(k p) n -> p k n", p=128))
        for s in range(2):
            self.memset("dve", Vh[s](None, None, (64, 128)), 2.0)
        ntiles = [(0, 256)] + [(256 + 512 * i, 256 + 512 * (i + 1)) for i in range(4)]
        wsi = 0
        for gi, (dst, gn) in enumerate(((cqn, qn), (ckvn, kvn))):
            w = ws[wsi % 3]
            wsi += 1
            self.dma("pool", w(), win[:, :, gi * 256:(gi + 1) * 256])
            for ni, (n0, n1) in enumerate(ntiles):
                n = n1 - n0
                pc = [self.ps(), self.ps()]
                for cch in range(2):
                    for k in range(8):
                        self.mm(pc[cch](None, (0, n)), w(None, k, (cch * 128, cch * 128 + 128)),
                                self.hT(None, k, (n0, n1)), start=(k == 0), stop=(k == 7))
                pss = self.ps()
                for cch in range(2):
                    sq = sqb[cch]
                    self.act(sq(None, (0, n)), pc[cch](None, (0, n)), AF.Square)
                    self.mm(pss(None, (0, n)), self.onesb(), sq(None, (0, n)), start=(cch == 0), stop=(cch == 1))
                r = rb[ni % 2]
                self.act(r(None, (0, n)), pss(None, (0, n)), AF.Ln, scale=1.0 / 256, bias=self.c_eps)
                self.act(r(None, (0, n)), r(None, (0, n)), AF.Exp, scale=-0.5)
                for cch in range(2):
                    self.stt(dst(None, cch, (n0, n1)), pc[cch](None, (0, n)), gn(None, (cch, cch + 1)),
                             r(None, (0, n)), ALU.mult, ALU.mult)
        w = ws[wsi % 3]
        wsi += 1
        self.dma("pool", w(), win[:, :, 512:768])
        H1 = (64, 128)
        H0 = (0, 64)
        for ni, (n0, n1) in enumerate(ntiles):
            n = n1 - n0
            pa = self.ps()
            pb = self.ps()
            wa = w
            for k in range(8):
                self.mm(pa(None, (0, n)), wa(None, k, (0, 128)), self.hT(None, k, (n0, n1)), start=(k == 0), stop=(k == 7))
            for k in range(8):
                self.mm(pb(None, (0, n)), w(None, k, (128, 256)), self.hT(None, k, (n0, n1)), start=(k == 0), stop=(k == 7))
            tc_ = tks[(2 * ni) % 4]
            ts_ = tks[(2 * ni + 1) % 4]
            self.dma("sp", tc_(None, (0, n)), d["k_tkc"][:, n0:n1])
            self.dma("sp", ts_(None, (0, n)), d["k_tks"][:, n0:n1])
            u = thb[0]
            v = thb[1]
            self.tt("dve", u(H1, (0, n)), pa(H1, (0, n)), tc_(H1, (0, n)), ALU.mult)
            self.tt("dve", v(H1, (0, n)), pb(H1, (0, n)), ts_(H1, (0, n)), ALU.mult)
            self.tt("pool", Kh[0](H1, (n0, n1)), u(H1, (0, n)), v(H1, (0, n)), ALU.add)
            self.copy("pool", Kh[1](H1, (n0, n1)), Kh[0](H1, (n0, n1)))
        for gc in range(8):
            if gc % 2 == 0:
                w = ws[wsi % 3]
                wsi += 1
                self.dma("pool", w(), win[:, :, 896 + gc * 128: 896 + gc * 128 + 256])
            co = (gc % 2) * 128
            for ni, (n0, n1) in enumerate(ntiles):
                n = n1 - n0
                pg = self.ps()
                for k in range(8):
                    self.mm(pg(None, (0, n)), w(None, k, (co, co + 128)), self.hT(None, k, (n0, n1)),
                            start=(k == 0), stop=(k == 7))
                th = thb[ni % 2]
                self.act(th(None, (0, n)), pg(None, (0, n)), AF.Tanh, scale=0.5)
                self.stt(self.PT(None, gc, (n0, n1)), th(None, (0, n)), 1.0, pg(None, (0, n)), ALU.add, ALU.mult)
        self.dump("dbg_cqn", cqn, 2)
        self.dump("dbg_ckvn", ckvn, 2)
        self.dump("dbg_SG", self.PT, 8)
        if self.stop == "proj":
            return
        qchunks = ([(0, 256, 2)] if need_ctx else []) + [(256 + 512 * i, 256 + 512 * (i + 1), NT) for i in range(4)]
        acnt = [0]
        gcnt = [0]
        nheads = 1 if self.stop == "attn1" else NH

        def proj(h):
                s = h % 2
                Q = Qh[s]
                K = Kh[s]
                V = Vh[s]
                pj = [self.psbank[i_] for i_ in range(6)]
                pjc = 0
                for ni, (n0, n1) in enumerate(ntiles):
                    n = n1 - n0
                    pq = pj[pjc % 6]
                    pjc += 1
                    for k in range(2):
                        lq = wuq(None, k, (h * 128, h * 128 + 128))
                        self.mm(pq(None, (0, n)), lq, cqn(None, k, (n0, n1)),
                                start=(k == 0), stop=(k == 1))
                    self.copy("dve", Q(H0, (n0, n1)), pq(H0, (0, n)))
                    self.tt("dve", Q(H1, (n0, n1)), pq(H1, (0, n)), tq(H1, (n0, n1)), ALU.mult)
                    pk = pj[pjc % 6]
                    pjc += 1
                    for k in range(2):
                        self.mm(pk(H0, (0, n)), wukv(None, k, (h * 128, h * 128 + 64)), ckvn(None, k, (n0, n1)),
                                start=(k == 0), stop=(k == 1))
                    self.copy("dve", K(H0, (n0, n1)), pk(H0, (0, n)))
                for g0 in range(0, NT, 8):
                    g1 = min(NT, g0 + 8)
                    pv = pj[pjc % 6]
                    pjc += 1
                    for t in range(g0, g1):
                        for k in range(2):
                            self.mm(pv(None, ((t - g0) * 64, (t - g0) * 64 + 64)), ckvn(None, k, (t * 128, t * 128 + 128)),
                                    wukv(None, k, (h * 128 + 64, h * 128 + 128)), start=(k == 0), stop=(k == 1))
                    ng = g1 - g0
                    src = X(pv.ap[:, 0:ng * 64].rearrange("p (t c) -> p t c", c=64), pv(None, (0, ng * 64)).k)
                    self.copy("dve", V(None, (g0, g1), (0, 64)), src)

        proj(0)
        for h in range(nheads):
            s = h % 2
            Q = Qh[s]
            K = Kh[s]
            V = Vh[s]
            if h == 0:
                self.dump("dbg_Q0", Q)
                self.dump("dbg_K0", K)
            hp = (h % 2) * 64
            HP = (hp, hp + 64)
            steps = []
            accs_of = {}
            for qi, (q0, q1, nk) in enumerate(qchunks):
                accs_of[qi] = self.psbank[6 + (acnt[0] % 2)]
                acnt[0] += 1
                for kt in range(0, nk, 2):
                    steps.append((qi, q0, q1, nk, kt))

            def scores_exp(st, g):
                qi, q0, q1, nk, kt = st
                nq = q1 - q0
                sp_ = self.pspair[g % 3]
                for u2 in range(2):
                    self.mm(sp_(None, (u2 * 512, u2 * 512 + nq)), K(None, ((kt + u2) * 128, (kt + u2) * 128 + 128)),
                            Q(None, (q0, q1)))
                pe_ = pexp[g % 4]
                if nq == 512:
                    self.act(pe_(), sp_(), AF.Exp, scale=ATT_SCALE)
                else:
                    for u2 in range(2):
                        self.act(pe_(None, (u2 * 512, u2 * 512 + nq)), sp_(None, (u2 * 512, u2 * 512 + nq)),
                                 AF.Exp, scale=ATT_SCALE)

            def pv(st, g):
                qi, q0, q1, nk, kt = st
                nq = q1 - q0
                acc = accs_of[qi]
                pe_ = pexp[g % 4]
                for u2 in range(2):
                    self.mm(acc(None, (0, nq)), V(None, kt + u2), pe_(None, (u2 * 512, u2 * 512 + nq)),
                            start=(kt + u2 == 0), stop=(kt + u2 == nk - 1))

            def evac(st):
                qi, q0, q1, nk, kt = st
                nq = q1 - q0
                acc = accs_of[qi]
                r = rd[qi % 2]
                accs = thb[qi % 2]
                ptv = self.PT(HP, h // 2, (q0, q1))
                if hp == 0:
                    self.copy("dve", accs(None, (0, nq)), acc(None, (0, nq)))
                    self.recip(r(H0, (0, nq)), accs(H1, (0, nq)))
                    self.tt("pool", r(H0, (0, nq)), r(H0, (0, nq)), ptv, ALU.mult)
                    self.tt("pool", ptv, accs(H0, (0, nq)), r(H0, (0, nq)), ALU.mult)
                else:
                    self.copy("dve", accs(H1, (0, nq)), acc(H0, (0, nq)))
                    self.copy("dve", accs(H0, (0, nq)), acc(H1, (0, nq)))
                    self.recip(r(H1, (0, nq)), accs(H0, (0, nq)))
                    self.tt("pool", r(H1, (0, nq)), r(H1, (0, nq)), ptv, ALU.mult)
                    self.tt("pool", ptv, accs(H1, (0, nq)), r(H1, (0, nq)), ALU.mult)

            g0_ = gcnt[0]
            ns = len(steps)
            scores_exp(steps[0], g0_)
            if ns > 1:
                scores_exp(steps[1], g0_ + 1)
            for i_, st in enumerate(steps):
                if i_ + 2 < ns:
                    scores_exp(steps[i_ + 2], g0_ + i_ + 2)
                pv(st, g0_ + i_)
                if st[4] + 2 >= st[3]:
                    evac(st)
                if i_ == ns // 2 and h + 1 < nheads:
                    proj(h + 1)
            gcnt[0] += ns
        self.dump("dbg_PT", self.PT, 8)

    def mlstm(self, b, li, j, need_ctx):
        d = self.d
        L = self.L
        al = L.alloc
        XW = 2312
        ntiles = [(0, 256)] + [(256 + 512 * i, 256 + 512 * (i + 1)) for i in range(4)]

        def PO(n0):
            return 2 + n0 if n0 < 256 else 260 + (n0 - 256)

        cw = al([16, 5], F32)
        cb = al([16], F32)
        hbc = al([16], F32)
        hgc = al([16], F32)
        skc = al([16], F32)
        wsm = [al([16, 4], F32) for _ in range(6)]
        BD = [al([16, 128], BF16) for _ in range(3)]
        Wc = al([16, 32], BF16)
        Wm = al([16, 32], BF16)
        bif = al([1], F32)
        gtok = al([NT, 32], F32)
        spc = al([NT * 8], F32)
        tmpg = al([NT * 8], F32)
        alpha = [al([NT, 8], F32) for _ in range(2)]
        ib = [al([NT, 8], F32) for _ in range(2)]
        decay = [al([NT, 8], F32) for _ in range(2)]
        aend = [al([NT, 8], F32) for _ in range(2)]
        C32 = [al([2, 260], F32) for _ in range(2)]
        Cbf = [al([2, 260], BF16) for _ in range(2)]
        AT = [al([128], BF16) for _ in range(4)]
        khat = [al([256], BF16) for _ in range(4)]
        mcol = [al([8], F32) for _ in range(4)]
        dgc = [al([5, 128], BF16) for _ in range(1)]
        tA = [al([512], F32) for _ in range(2)]
        tB = [al([512], F32) for _ in range(2)]
        ssqh = al([NT], F32)
        rrh = al([NT], F32)
        hnb = [L.a.view(tB[i_].off, [256], BF16) for i_ in range(2)]
        preb = [al([2, 128], BF16) for _ in range(2)]
        utmp = [L.a.view(tA[i_].off, [128], F32) for i_ in range(2)]
        wxm = [al([8, 256], BF16) for _ in range(2)]
        wzo = [al([8, 512], BF16) for _ in range(2)]
        B1 = al([2, XW], BF16)
        B2 = al([2, TA], BF16)
        B3 = al([2, TA], BF16)
        B4 = al([2, TA], BF16)
        B5 = al([NT, 256], BF16)
        B6 = al([NT, 258], BF16)
        HF = al([NT, 256], BF16)
        HBv = al([NT, 256], BF16)
        gT = L.a.view(HF.off, [TA], F32)
        setup_top = L.top

        self.dma("sp", cw(), d["ml_cw_c"][j])
        self.dma("sp", cb(), d["ml_cb_c"][j])
        self.dma("sp", hgc(), d["ml_hn_c"][j])
        self.dma("sp", skc(), d["ml_sk_c"][j])
        self.dma("sp", bif((0, 32)), d["ml_bif_c"][j])
        for i, nm in enumerate(("ml_wq_c", "ml_wk_c", "ml_wv_c", "ml_wqT_c", "ml_wkT_c", "ml_wvT_c")):
            self.dma("sp", wsm[i](), d[nm][j])
        self.ts("dve", hbc(), cb(), 0.5, ALU.mult)
        self.ts("dve", hgc(), hgc(), 0.5, ALU.mult)
        self.ts("dve", skc(), skc(), 0.5, ALU.mult)

        def bd_build(out_x, wv, jj):
            w_ap = wv(None, jj).ap.unsqueeze(1).broadcast_to([128, 32, 4])
            m_ap = self.bdmask().ap.rearrange("p (m d) -> p m d", d=4)
            o_ap = out_x.ap.rearrange("p (m d) -> p m d", d=4)
            self.tt("dve", X(o_ap, out_x.k), X(w_ap, wv(None, jj).k), X(m_ap, self.bdmask().k), ALU.mult)

        for jj in range(16):
            for i in range(3):
                bd_build(BD[i](None, jj), wsm[i], jj)
        L.top = B3.off
        wif = L.alloc([48, 32], F32)
        for dd in range(2):
            for q4 in range(4):
                self.dma("sp", wif(None, (q4 * 12, q4 * 12 + 12), (dd * 16, dd * 16 + 16)),
                         d["ml_wif"][j][dd].rearrange("(k p) g -> p k g", p=128)[:, q4 * 12:(q4 + 1) * 12, :])
        bdt = [L.alloc([128], F32) for _ in range(3)]
        for jj in range(16):
            for i in range(3):
                bd_build(bdt[i](), wsm[3 + i], jj)
            p1 = self.ps()
            self.mm(p1(None, (0, 32)), bdt[0](), wif(None, jj), start=True, stop=False)
            self.mm(p1(None, (0, 32)), bdt[1](), wif(None, 16 + jj), start=False, stop=True)
            self.copy("dve", Wc(None, jj), p1(None, (0, 32)))
            p2 = self.ps()
            self.mm(p2(None, (0, 32)), bdt[2](), wif(None, 32 + jj))
            self.copy("dve", Wm(None, jj), p2(None, (0, 32)))
        L.top = setup_top
        for cc in range(2):
            self.memset("dve", B1(None, cc, (0, 2)), 0.0)
            self.memset("dve", B1(None, cc, (258, 260)), 0.0)
            self.memset("dve", B1(None, cc, (2308, XW)), 0.0)
        self.memset("dve", B6(None, None, (256, 258)), 1.0)

        win = d["ml_win"][j].rearrange("(k p) n -> p k n", p=128)
        dcnt = [0]
        pending = []

        def features(w, cc, jj, gates_first):
            for ni, (n0, n1) in enumerate(ntiles):
                n = n1 - n0
                pm = self.ps()
                for k in range(8):
                    self.mm(pm(None, (0, n)), w(None, k, (cc * 128, cc * 128 + 128)), self.hT(None, k, (n0, n1)),
                            start=(k == 0), stop=(k == 7))
                self.copy("act", B1(None, cc, (PO(n0), PO(n0) + n)), pm(None, (0, n)))
            while pending:
                pending.pop(0)()
            dg = dgc[0]
            dcnt[0] += 1
            for tap in range(5):
                self.ts("dve", dg(None, tap), self.identb(), cw(None, jj, (tap, tap + 1)), ALU.mult)
            for ni, (n0, n1) in enumerate(ntiles):
                n = n1 - n0
                pc = self.ps()
                for tap in range(5):
                    c0 = PO(n0) + tap - 2
                    self.mm(pc(None, (0, n)), dg(None, tap), B1(None, cc, (c0, c0 + n)),
                            start=(tap == 0), stop=(tap == 4))
                ta = tA[ni % 2]
                tb = tB[ni % 2]
                self.ts("dve", ta(None, (0, n)), pc(None, (0, n)), 0.5, ALU.mult, hbc(None, (jj, jj + 1)), ALU.add)
                self.act(tb(None, (0, n)), pc(None, (0, n)), AF.Tanh, scale=0.5, bias=hbc(None, (jj, jj + 1)))
                self.stt(B2(None, cc, (n0, n1)), tb(None, (0, n)), 1.0, ta(None, (0, n)), ALU.add, ALU.mult)
            if gates_first is not None:
                def gates(cc=cc, jj=jj, gates_first=gates_first):
                    for ni, (n0, n1) in enumerate(ntiles):
                        n = n1 - n0
                        pg = self.ps()
                        R32 = (0, 32)
                        self.mm(pg(R32, (0, n)), Wc(None, jj), B2(None, cc, (n0, n1)), start=True, stop=False)
                        self.mm(pg(R32, (0, n)), Wm(None, jj), B1(None, cc, (PO(n0), PO(n0) + n)), start=False, stop=True)
                        if gates_first:
                            self.ts("dve", gT(R32, (n0, n1)), pg(R32, (0, n)), bif(R32), ALU.add)
                        else:
                            self.tt("dve", gT(R32, (n0, n1)), pg(R32, (0, n)), gT(R32, (n0, n1)), ALU.add)
                pending.append(gates)

        wcnt = 0
        for hh in range(8):
            w = wxm[wcnt % 2]
            wcnt += 1
            self.dma("pool", w(), win[:, :, hh * 256:(hh + 1) * 256])
            for cc in range(2):
                features(w, cc, hh * 2 + cc, gates_first=(hh == 0 and cc == 0))
        while pending:
            pending.pop(0)()
        R32 = (0, 32)
        pgA = self.ps()
        pgB = self.ps()
        for c in range(NT):
            dst = pgA(None, (c * 32, c * 32 + 32)) if c < 16 else pgB(None, ((c - 16) * 32, (c - 16) * 32 + 32))
            idn = X(self.ident.ap[0:32, 0:32], self.ident().k)
            self.tr(dst, gT(R32, (c * 128, c * 128 + 128)), idn)
        self.copy("dve", X(gtok.ap[:, 0:16, :], gtok(None, (0, 16)).k),
                  X(pgA.ap.rearrange("p (c g) -> p c g", g=32), pgA().k))
        self.copy("dve", X(gtok.ap[:, 16:18, :], gtok(None, (16, 18)).k),
                  X(pgB.ap[:, 0:64].rearrange("p (c g) -> p c g", g=32), pgB(None, (0, 64)).k))
        for dd in range(2):
            li_x = X(gtok.ap[:, :, dd * 16:dd * 16 + 8], gtok().k)
            fp_x = X(gtok.ap[:, :, dd * 16 + 8:dd * 16 + 16], gtok().k)
            sp3 = X(spc.ap.rearrange("p (c g) -> p c g", g=8), spc().k)
            tm3 = X(tmpg.ap.rearrange("p (c g) -> p c g", g=8), tmpg().k)
            self.act(sp3, fp_x, AF.Exp, scale=-1.0)
            self.act(spc(), spc(), AF.Ln, bias=self.c_one)
            tri = self.maskF if dd == 0 else self.maskB
            pcum = self.ps()
            ptot = self.ps()
            NG = NT * 8
            self.mm(pcum(None, (0, NG)), tri(), spc())
            self.mm(ptot(None, (0, NG)), self.ones(), spc())
            cum3 = X(pcum.ap[:, 0:NG].rearrange("p (c g) -> p c g", g=8), pcum(None, (0, NG)).k)
            self.tt("dve", tm3, cum3, li_x, ALU.add)
            self.act(alpha[dd](), tm3, AF.Exp)
            self.act(ib[dd](), cum3, AF.Exp, bias=self.c_ln16)
            tot3 = X(ptot.ap[:, 0:NG].rearrange("p (c g) -> p c g", g=8), ptot(None, (0, NG)).k)
            self.act(decay[dd](), tot3, AF.Exp, scale=-1.0)
            self.tt("pool", aend[dd](), decay[dd](), alpha[dd](), ALU.mult)

        order = [list(range(NT)), [1, 0] + list(range(NT - 1, 1, -1))]
        c_lo = 0 if need_ctx else 2
        slot = [0]
        def load_head_w(h2):
            w_ = wxm[(wcnt0 + h2) % 2]
            self.dma("pool", w_(), win[:, :, h2 * 256:(h2 + 1) * 256])
            wz_ = wzo[h2 % 2]
            self.dma("pool", wz_(None, None, (0, 256)), win[:, :, 2048 + h2 * 256: 2048 + (h2 + 1) * 256])
            self.dma("pool", wz_(None, None, (256, 512)), win[:, :, 4096 + h2 * 256: 4096 + (h2 + 1) * 256])

        wcnt0 = wcnt
        load_head_w(0)
        for hh in range(8):
            w = wxm[(wcnt0 + hh) % 2]
            wz = wzo[hh % 2]
            for cc in range(2):
                features(w, cc, hh * 2 + cc, gates_first=None)
            for cc in range(2):
                jj = hh * 2 + cc
                for (dstB, bd) in ((B3, BD[0]), (B4, BD[1])):
                    for ni, (n0, n1) in enumerate(ntiles):
                        n = n1 - n0
                        pq = self.ps()
                        self.mm(pq(None, (0, n)), bd(None, jj), B2(None, cc, (n0, n1)))
                        if ni % 2 == 0:
                            self.copy("act", dstB(None, cc, (n0, n1)), pq(None, (0, n)))
                        else:
                            self.copy("dve", dstB(None, cc, (n0, n1)), pq(None, (0, n)))
            for c in range(0, NT, 2):
                pk = self.ps()
                pv = self.ps()
                for u in range(2):
                    for cc in range(2):
                        jj = hh * 2 + cc
                        t0 = (c + u) * 128
                        self.mm(pk(None, (u * 256 + cc * 128, u * 256 + cc * 128 + 128)),
                                B2(None, cc, (t0, t0 + 128)), BD[1](None, jj))
                        self.mm(pv(None, (u * 256 + cc * 128, u * 256 + cc * 128 + 128)),
                                B1(None, cc, (PO(t0), PO(t0) + 128)), BD[2](None, jj))
                self.copy("act", B5(None, (c, c + 2)), X(pk.ap.rearrange("p (u f) -> p u f", f=256), pk().k))
                self.copy("dve", B6(None, (c, c + 2), (0, 256)), X(pv.ap.rearrange("p (u f) -> p u f", f=256), pv().k))
            for cc in range(2):
                self.ts("dve", B2(None, cc), B2(None, cc), skc(None, (hh * 2 + cc, hh * 2 + cc + 1)), ALU.mult)
            def front(step, dd):
                c = order[dd][step]
                sl = (step % 2) * 2 + dd
                t0 = c * 128
                pS = self.psbank[dd]
                for e in range(2):
                    self.mm(pS(None, (0, 128)), B4(None, e, (t0, t0 + 128)), B3(None, e, (t0, t0 + 128)),
                            start=(e == 0), stop=(e == 1))
                msk = self.maskF if dd == 0 else self.maskB
                self.stt(AT[sl](), pS(None, (0, 128)), alpha[dd](None, c, (hh, hh + 1)), msk(), ALU.mult, ALU.mult)
                self.act(khat[sl](), B5(None, c), AF.Copy, scale=aend[dd](None, c, (hh, hh + 1)))

            def back(step, dd):
                c = order[dd][step]
                first = (step == 0)
                sl = (step % 2) * 2 + dd
                t0 = c * 128
                po = self.psbank[2 + dd]
                self.mm(po(None, (0, 257)), AT[sl](), B6(None, c, (0, 257)), start=True, stop=first)
                if not first:
                    for e in range(2):
                        self.mm(po(None, (0, 257)), B3(None, e, (t0, t0 + 128)), Cbf[dd](None, e, (0, 257)),
                                start=False, stop=(e == 1))
                if step < NT - 1:
                    pU = self.pspair[2 + dd]
                    for e in range(2):
                        self.mm(pU(None, (e * 512, e * 512 + 257)), khat[sl](None, (e * 128, e * 128 + 128)),
                                B6(None, c, (0, 257)))
                    u3 = X(pU.ap.rearrange("p (e f) -> p e f", f=512)[:, :, 0:257], pU().k)
                    c3 = X(C32[dd].ap[:, :, 0:257], C32[dd]().k)
                    if first:
                        self.copy("dve", c3, u3)
                    else:
                        self.stt(c3, c3, decay[dd](None, c, (hh, hh + 1)), u3, ALU.mult, ALU.add)
                    self.copy("pool", Cbf[dd](), C32[dd]())
                if c >= c_lo:
                    m = mcol[sl]
                    self.act(m(None, (0, 1)), po(None, (256, 257)), AF.Abs)
                    self.ts("dve", m(None, (0, 1)), m(None, (0, 1)), ib[dd](None, c, (hh, hh + 1)), ALU.max)
                    self.recip(m(None, (1, 2)), m(None, (0, 1)))
                    hdst = HF if dd == 0 else HBv
                    self.act(hdst(None, c), po(None, (0, 256)), AF.Copy, scale=m(None, (1, 2)))

            if hh + 1 < 8:
                load_head_w(hh + 1)
            front(0, 0)
            front(0, 1)
            for step in range(NT):
                for dd in range(2):
                    if step + 1 < NT:
                        front(step + 1, dd)
                    back(step, dd)
            for c in range(c_lo, NT):
                t0 = c * 128
                pog = self.ps()
                for k in range(8):
                    self.mm(pog(None, (0, 256)), self.hT(None, k, (t0, t0 + 128)), wz(None, k, (256, 512)),
                            start=(k == 0), stop=(k == 7))
                ta = tA[c % 2]
                tb = tB[c % 2]
                self.act(ta(None, (0, 256)), pog(None, (0, 256)), AF.Tanh, scale=0.5)
                self.tt("pool", tb(None, (0, 256)), HF(None, c), HBv(None, c), ALU.add)
                self.stt(HF(None, c), ta(None, (0, 256)), 1.0, tb(None, (0, 256)), ALU.add, ALU.mult)
                self.act(self.junk(None, (0, 256)), HF(None, c), AF.Square, accum=ssqh(None, (c, c + 1)))
            for cc in range(2):
                for ni, (n0, n1) in enumerate(ntiles):
                    n = n1 - n0
                    if n1 <= 256 and not need_ctx:
                        continue
                    pz = self.ps()
                    for k in range(8):
                        self.mm(pz(None, (0, n)), wz(None, k, (cc * 128, cc * 128 + 128)), self.hT(None, k, (n0, n1)),
                                start=(k == 0), stop=(k == 7))
                    tb = tB[ni % 2]
                    self.act(tb(None, (0, n)), pz(None, (0, n)), AF.Tanh, scale=0.5)
                    self.stt(B4(None, cc, (n0, n1)), tb(None, (0, n)), 1.0, pz(None, (0, n)), ALU.add, ALU.mult)
            self.act(rrh(None, (c_lo, NT)), ssqh(None, (c_lo, NT)), AF.Ln, scale=1.0 / 256, bias=self.c_eps4)
            self.act(rrh(None, (c_lo, NT)), rrh(None, (c_lo, NT)), AF.Exp, scale=-0.5)
            for c in range(c_lo, NT):
                t0 = c * 128
                hn = hnb[c % 2]
                self.act(hn(), HF(None, c), AF.Copy, scale=rrh(None, (c, c + 1)))
                pT = self.psbf[(self.psrr) % 8]
                self.psrr += 1
                for cc in range(2):
                    self.tr(pT(None, (cc * 128, cc * 128 + 128)), hn(None, (cc * 128, cc * 128 + 128)), self.identb())
                pre = preb[c % 2]
                for cc in range(2):
                    jj = hh * 2 + cc
                    u = utmp[cc]
                    self.stt(u(), pT(None, (cc * 128, cc * 128 + 128)), hgc(None, (jj, jj + 1)),
                             B2(None, cc, (t0, t0 + 128)), ALU.mult, ALU.add)
                    self.tt("pool", pre(None, cc), u(), B4(None, cc, (t0, t0 + 128)), ALU.mult)
                self.dma("sp", d["ptd"][c][:, hh * 2:hh * 2 + 2, :], pre(), out_keys=[("ptd", c)])


def _rope_tables():
    rows = T_LAT // 64
    row = np.repeat(np.arange(rows), 64).astype(np.float32)
    col = np.tile(np.arange(64), rows).astype(np.float32)
    inv = (10000.0 ** (-np.arange(8, dtype=np.float32) / 8)).astype(np.float32)
    cr = np.cos(row[:, None] * inv)
    sr = np.sin(row[:, None] * inv)
    cc = np.cos(col[:, None] * inv)
    sc = np.sin(col[:, None] * inv)
    C = np.concatenate([cr, cr, cc, cc], axis=1).T
    Sg = np.concatenate([-sr, sr, -sc, sc], axis=1).T
    Cf = np.concatenate([np.ones((32, T_CTX), np.float32), C], axis=1).astype(np.float32)
    Sf = np.concatenate([np.zeros((32, T_CTX), np.float32), Sg], axis=1).astype(np.float32)
    tq = np.concatenate([Cf, Sf, Cf, Sf], axis=0)
    tkc = np.concatenate([Cf] * 4, axis=0)
    tks = np.concatenate([Sf] * 4, axis=0)
    return np.ascontiguousarray(tq), np.ascontiguousarray(tkc), np.ascontiguousarray(tks)


PERM_A = np.arange(32)
PERM_B = np.concatenate([np.arange(8, 16), np.arange(0, 8), np.arange(24, 32), np.arange(16, 24)])


def _col(v, nch):
    s = v.shape[:-1]
    return np.ascontiguousarray(np.swapaxes(v.reshape(s + (nch, 128)), -1, -2))


def prep_shared(inp):
    f = lambda a: np.ascontiguousarray(np.asarray(a, dtype=np.float32))
    o = {}
    o["c_ctx"] = f(inp["c_ctx"])
    o["ada_w"] = f(inp["ada_w"])
    o["ada_bc"] = _col(f(inp["ada_b"]), 24)
    o["npre_c"] = _col(f(inp["norm_pre"]), 8)
    o["npost"] = f(inp["norm_post"])
    w = f(inp["mla_w_in"])
    kr = w[:, :, 512:544]
    krA = np.tile(kr[:, :, PERM_A], (1, 1, 4))
    krB = np.tile(kr[:, :, PERM_B], (1, 1, 4))
    krO = np.tile(kr, (1, 1, 4))
    o["mla_win"] = np.ascontiguousarray(np.concatenate([w[:, :, 0:512], krA, krB, krO, w[:, :, 544:]], axis=2))
    o["mla_qn_c"] = _col(f(inp["mla_q_norm"]), 2)
    o["mla_kvn_c"] = _col(f(inp["mla_kv_norm"]), 2)
    wq = f(inp["mla_w_uq"]).reshape(2, 256, NH, 96)
    rope = wq[..., 64:]
    o["mla_wuq"] = np.ascontiguousarray(
        np.concatenate([wq[..., :64], rope[..., PERM_A], rope[..., PERM_B]], axis=-1).reshape(2, 256, 2048))
    o["mla_wukv"] = f(inp["mla_w_ukv"])
    o["mla_wout"] = f(inp["mla_w_out"])
    o["ml_win"] = f(inp["ml_w_in"])
    o["ml_cw_c"] = np.ascontiguousarray(np.transpose(f(inp["ml_conv_w"]).reshape(2, 5, 16, 128), (0, 3, 2, 1)))
    o["ml_cb_c"] = _col(f(inp["ml_conv_b"]), 16)
    for nm, key in (("ml_wq", "ml_w_q"), ("ml_wk", "ml_w_k"), ("ml_wv", "ml_w_v")):
        wqk = f(inp[key])
        a = wqk.reshape(2, 16, 32, 4, 4)
        o[nm + "_c"] = np.ascontiguousarray(np.transpose(a, (0, 2, 3, 1, 4)).reshape(2, 128, 16, 4))
        o[nm + "T_c"] = np.ascontiguousarray(np.transpose(a, (0, 2, 4, 1, 3)).reshape(2, 128, 16, 4))
    o["ml_wif"] = f(inp["ml_w_if"])
    o["ml_bif_c"] = np.ascontiguousarray(f(inp["ml_b_if"]).reshape(2, 32, 1))
    o["ml_hn_c"] = _col(f(inp["ml_head_norm"]), 16)
    o["ml_sk_c"] = _col(f(inp["ml_skip"]), 16)
    o["ml_wout"] = f(inp["ml_w_out"])
    o["k_ident"] = np.eye(128, dtype=np.float32)
    o["k_maskF"] = np.triu(np.ones((128, 128), np.float32))
    o["k_maskB"] = np.tril(np.ones((128, 128), np.float32))
    o["k_bdmask"] = np.kron(np.eye(32, dtype=np.float32), np.ones((4, 4), np.float32))
    o["k_tq"], o["k_tkc"], o["k_tks"] = _rope_tables()
    return o


_CACHE = {}


def kernel(**inputs):
    n_cores = 8
    NB = 2
    key = ("full",)
    if key not in _CACHE:
        _CACHE[key] = Prog(NB, [0, 1, 2, 3]).build()
    nc = _CACHE[key]
    shared = prep_shared(inputs)
    x = np.asarray(inputs["x"], dtype=np.float32)
    c = np.asarray(inputs["c"], dtype=np.float32)
    ctx = np.asarray(inputs["ctx"], dtype=np.float32)
    in_maps = []
    for i in range(n_cores):
        m = dict(shared)
        m["x"] = np.ascontiguousarray(x[i * NB:(i + 1) * NB])
        m["c"] = np.ascontiguousarray(c[i * NB:(i + 1) * NB])
        m["ctx"] = np.ascontiguousarray(ctx[i * NB:(i + 1) * NB])
        in_maps.append(m)
    res = run_bass_kernel_spmd(nc, in_maps, core_ids=list(range(n_cores)))
    return np.concatenate([np.asarray(r["out"]) for r in res.results], axis=0).astype(np.float32)
```

```python
import math
from contextlib import ExitStack

import numpy as np
import concourse.bass as bass
import concourse.mybir as mybir
from concourse.bass_utils import run_bass_kernel_spmd

F32 = mybir.dt.float32
BF16 = mybir.dt.bfloat16
ALU = mybir.AluOpType
AF = mybir.ActivationFunctionType
AX = mybir.AxisListType

D = 1024
T_LAT = 2048
T_CTX = 256
TA = T_LAT + T_CTX
NT = TA // 128
EPS = 1e-6
NH = 16
ATT_SCALE = 96 ** -0.5
LN16 = math.log(16.0)

EPOCH = 4096
NSLOT = 8
COMPUTE = ("pe", "act", "dve", "pool")
ENGINES = ("pe", "act", "dve", "pool", "sp")


class Op:
    __slots__ = ("eng", "fn", "idx", "is_dma", "signal", "sem", "val", "waits",
                 "eidx", "slot", "prev_slot_wait")


class Sched:
    def __init__(self):
        self.ops = []
        self.by_eng = {e: [] for e in ENGINES}
        self.last_writer = {}
        self.readers = {}
        self.waited = {}
        self.dma_waited = {e: set() for e in ENGINES}
        self.dma_count = {e: 0 for e in ENGINES}
        self.slot_last = {}

    def _add(self, eng, fn, reads, writes, is_dma):
        op = Op()
        op.eng = eng
        op.fn = fn
        op.is_dma = is_dma
        op.signal = is_dma
        op.sem = None
        op.val = None
        op.waits = []
        op.slot = None
        op.prev_slot_wait = None
        op.idx = len(self.ops)
        op.eidx = len(self.by_eng[eng])
        self.ops.append(op)
        self.by_eng[eng].append(op)
        psr = [k for k in reads if k[0] == "ps"]
        if psr:
            writes = list(writes) + psr
        lw = self.last_writer
        rd = self.readers
        deps = set()
        same_raw = None
        for k in reads:
            w = lw.get(k)
            if w is not None:
                deps.add(w)
                if w.eng == eng and eng != "pe" and not w.is_dma:
                    if same_raw is None or w.eidx > same_raw.eidx:
                        same_raw = w
        for k in writes:
            w = lw.get(k)
            if w is not None:
                deps.add(w)
            r = rd.get(k)
            if r:
                deps.update(r)
        best = {}
        for d in deps:
            if d is op:
                continue
            if d.is_dma:
                if d.idx not in self.dma_waited[eng]:
                    self.dma_waited[eng].add(d.idx)
                    op.waits.append(d)
            else:
                if d.eng == eng:
                    continue
                b = best.get(d.eng)
                if b is None or d.eidx > b.eidx:
                    best[d.eng] = d
        if same_raw is not None and not is_dma:
            best[eng] = same_raw
        for pe, d in best.items():
            key = (eng, pe)
            if self.waited.get(key, -1) >= d.eidx:
                continue
            self.waited[key] = d.eidx
            d.signal = True
            op.waits.append(d)
        if is_dma:
            n = self.dma_count[eng]
            self.dma_count[eng] = n + 1
            op.slot = (eng, n % NSLOT)
            prev = self.slot_last.get(op.slot)
            if prev is not None and prev.idx not in self.dma_waited[eng]:
                self.dma_waited[eng].add(prev.idx)
                op.prev_slot_wait = prev
            self.slot_last[op.slot] = op
        for k in reads:
            r = rd.get(k)
            if r is None:
                rd[k] = [op]
            else:
                if not op.is_dma:
                    r[:] = [x for x in r if x.is_dma or x.eng != eng]
                r.append(op)
        for k in writes:
            lw[k] = op
            rd[k] = []
        return op

    def op(self, eng, fn, reads=(), writes=()):
        return self._add(eng, fn, reads, writes, False)

    def dma(self, eng, fn, reads=(), writes=()):
        return self._add(eng, fn, reads, writes, True)

    def emit(self, nc, stack):
        sems = {}

        def get_sem(name):
            if name not in sems:
                sems[name] = stack.enter_context(nc.semaphore(name))
            return sems[name]

        for e in COMPUTE:
            cnt = 0
            for op in self.by_eng[e]:
                if op.is_dma or not op.signal:
                    continue
                op.sem = get_sem("s_%s_%d" % (e, cnt // EPOCH))
                op.val = cnt % EPOCH + 1
                cnt += 1
        slot_cnt = {}
        for op in self.ops:
            if op.is_dma:
                c = slot_cnt.get(op.slot, 0) + 1
                slot_cnt[op.slot] = c
                op.sem = get_sem("d_%s_%d" % op.slot)
                op.val = 16 * c
        finals = list(self.slot_last.values())
        block = stack.enter_context(nc.Block())

        def run(eng):
            def body(h):
                for op in self.by_eng[eng]:
                    if op.prev_slot_wait is not None:
                        p = op.prev_slot_wait
                        h.wait_ge(p.sem, p.val)
                    for d in op.waits:
                        h.wait_ge(d.sem, d.val)
                    ins = op.fn(h)
                    if op.signal:
                        ins.then_inc(op.sem, 16 if op.is_dma else 1)
                if eng == "sp":
                    for o in finals:
                        h.wait_ge(o.sem, o.val)
            return body

        block.tensor(run("pe"))
        block.scalar(run("act"))
        block.vector(run("dve"))
        block.gpsimd(run("pool"))
        block.sync(run("sp"))


GRAN = 256


class X:
    __slots__ = ("ap", "k")

    def __init__(self, ap, k):
        self.ap = ap
        self.k = k


class View:
    def __init__(self, space, base_ap, off, shape, esz, gran=GRAN):
        self.space = space
        self.ap = base_ap
        self.off = off
        self.shape = list(shape)
        self.esz = esz
        self.gran = gran
        st = []
        s = 1
        for n in reversed(self.shape):
            st.append(s)
            s *= n
        self.strides = list(reversed(st))
        self.nelem = s

    def __call__(self, pr=None, *idx):
        p0, p1 = (0, 128) if pr is None else pr
        sl = [slice(p0, p1)]
        lo = 0
        hi = 0
        for d, n in enumerate(self.shape):
            it = idx[d] if d < len(idx) else None
            if it is None:
                a, b = 0, n
                sl.append(slice(None))
            elif isinstance(it, int):
                a, b = it, it + 1
                sl.append(it)
            else:
                a, b = it
                sl.append(slice(a, b))
            lo += a * self.strides[d]
            hi += (b - 1) * self.strides[d]
        hi += 1
        b0 = self.off + lo * self.esz
        b1 = self.off + hi * self.esz
        halves = []
        if p0 < 64:
            halves.append(0)
        if p1 > 64:
            halves.append(1)
        g0 = b0 // self.gran
        g1 = (b1 - 1) // self.gran
        sp = self.space
        if sp == "ps":
            keys = [(sp, g) for g in range(g0, g1 + 1)]
        else:
            keys = [(sp, g, h) for g in range(g0, g1 + 1) for h in halves]
        return X(self.ap[tuple(sl)], keys)


class Arena:
    def __init__(self, nc, st, nbytes):
        self.nbytes = nbytes
        self.t = st.enter_context(nc.sbuf_tensor("arena", [128, nbytes // 2], BF16))
        self.top = 0

    def view(self, off, shape, dtype):
        esz = 4 if dtype == F32 else 2
        n = 1
        for s in shape:
            n *= s
        assert off % 4 == 0
        assert off + n * esz <= self.nbytes, ("arena overflow", off, n * esz, self.nbytes)
        ap = self.t[:, off // 2: off // 2 + n * esz // 2]
        if dtype == F32:
            ap = ap.bitcast(F32)
        if len(shape) > 1:
            names = " ".join("d%d" % i for i in range(len(shape)))
            kw = {"d%d" % i: shape[i] for i in range(1, len(shape))}
            ap = ap.rearrange("p (%s) -> p %s" % (names, names), **kw)
        return View("sb", ap, off, shape, esz)


class Bump:
    def __init__(self, arena, start, limit):
        self.a = arena
        self.top = start
        self.limit = limit
        self.peak = start

    def alloc(self, shape, dtype, align=GRAN):
        esz = 4 if dtype == F32 else 2
        n = esz
        for s in shape:
            n *= s
        off = (self.top + align - 1) // align * align
        self.top = off + n
        assert self.top <= self.limit, ("bump overflow", self.top, self.limit)
        self.peak = max(self.peak, self.top)
        return self.a.view(off, shape, dtype)


class Prog:
    def __init__(self, NB, layers, debug=None, stop=None):
        self.stop = stop
        self.NB = NB
        self.layers = layers
        self.debug = debug
        self.nc = bass.Bass("TRN2", target_bir_lowering=False)
        self.S = Sched()
        self.psrr = 0

    def mm(self, out, lhsT, rhs, start=True, stop=True):
        rk = lhsT.k + rhs.k
        if not start:
            rk = rk + out.k
        o, l, r = out.ap, lhsT.ap, rhs.ap
        self.S.op("pe", lambda h: h.matmul(o, lhsT=l, rhs=r, start=start, stop=stop),
                  reads=rk, writes=out.k)

    def tr(self, out, in_, ident):
        o, i, d = out.ap, in_.ap, ident.ap
        self.S.op("pe", lambda h: h.transpose(o, i, d), reads=in_.k + ident.k, writes=out.k)

    def act(self, out, in_, func, scale=1.0, bias=0.0, accum=None):
        rk = list(in_.k)
        sc = scale
        bi = bias
        if isinstance(scale, X):
            rk += scale.k
            sc = scale.ap
        if isinstance(bias, X):
            rk += bias.k
            bi = bias.ap
        wk = list(out.k)
        ac = None
        if accum is not None:
            wk += accum.k
            ac = accum.ap
        o, i = out.ap, in_.ap
        if ac is None:
            fn = lambda h: h.activation(out=o, in_=i, func=func, bias=bi, scale=sc)
        else:
            fn = lambda h: h.activation(out=o, in_=i, func=func, bias=bi, scale=sc, accum_out=ac)
        self.S.op("act", fn, reads=rk, writes=wk)

    def tt(self, eng, out, in0, in1, op):
        o, a, b = out.ap, in0.ap, in1.ap
        self.S.op(eng, lambda h: h.tensor_tensor(out=o, in0=a, in1=b, op=op),
                  reads=in0.k + in1.k, writes=out.k)

    def ts(self, eng, out, in0, s1, op0, s2=None, op1=None):
        rk = list(in0.k)
        a1 = s1
        a2 = s2
        if isinstance(s1, X):
            rk += s1.k
            a1 = s1.ap
        if isinstance(s2, X):
            rk += s2.k
            a2 = s2.ap
        o, i = out.ap, in0.ap
        if op1 is None:
            fn = lambda h: h.tensor_scalar(out=o, in0=i, scalar1=a1, scalar2=None, op0=op0)
        else:
            fn = lambda h: h.tensor_scalar(out=o, in0=i, scalar1=a1, scalar2=a2, op0=op0, op1=op1)
        self.S.op(eng, fn, reads=rk, writes=out.k)

    def stt(self, out, in0, scalar, in1, op0, op1):
        rk = in0.k + in1.k
        sc = scalar
        if isinstance(scalar, X):
            rk = rk + scalar.k
            sc = scalar.ap
        o, a, b = out.ap, in0.ap, in1.ap
        self.S.op("dve", lambda h: h.scalar_tensor_tensor(out=o, in0=a, scalar=sc, in1=b, op0=op0, op1=op1),
                  reads=rk, writes=out.k)

    def copy(self, eng, out, in_):
        o, i = out.ap, in_.ap
        if eng == "act":
            self.S.op("act", lambda h: h.activation(out=o, in_=i, func=AF.Copy), reads=in_.k, writes=out.k)
        else:
            self.S.op(eng, lambda h: h.tensor_copy(out=o, in_=i), reads=in_.k, writes=out.k)

    def recip(self, out, in_, fast=False):
        o, i = out.ap, in_.ap
        if fast:
            self.S.op("dve", lambda h: h.reciprocal_approx_fast(out=o, in_=i), reads=in_.k, writes=out.k)
        else:
            self.S.op("dve", lambda h: h.reciprocal(out=o, in_=i), reads=in_.k, writes=out.k)

    def memset(self, eng, out, val):
        o = out.ap
        self.S.op(eng, lambda h: h.memset(o, val), writes=out.k)

    def dma(self, eng, out, in_, out_keys=None, in_keys=None, **kw):
        ok = out.k if isinstance(out, X) else (out_keys or [])
        ik = in_.k if isinstance(in_, X) else (in_keys or [])
        o = out.ap if isinstance(out, X) else out
        i = in_.ap if isinstance(in_, X) else in_
        self.S.dma(eng, lambda h: h.dma_start(out=o, in_=i, **kw), reads=ik, writes=ok)

    def dump(self, name, view, nrow_chunks=1):
        if not (self.debug and name in self.debug):
            return
        tmpf = self.L.a.view(self.L.a.nbytes - TA * 4, [TA], F32)
        for k in range(nrow_chunks):
            src = view(None, k) if nrow_chunks > 1 else view()
            self.copy("dve", tmpf(), src)
            self.dma("sp", self.d[name][k * 128:(k + 1) * 128, :], tmpf())

    def barrier(self):
        S = self.S
        toks = []
        for e in ("pe", "act", "dve", "pool"):
            k = ("bar", e)
            toks.append(k)
            S.op(e, lambda h: h.nop(), reads=[], writes=[k])
        outstanding = [("bar_dma", o.idx) for o in S.slot_last.values()]
        for o in list(S.slot_last.values()):
            S.last_writer[("bar_dma", o.idx)] = o
        for e in ("pe", "act", "dve", "pool"):
            S.op(e, lambda h: h.nop(), reads=toks + outstanding, writes=[])
        S.op("sp", lambda h: h.nop(), reads=toks + outstanding, writes=[("bar", "sp")])

    def ps(self, n=1):
        if n == 2:
            if self.psrr % 2:
                self.psrr += 1
            i = (self.psrr // 2) % len(self.pspair)
            self.psrr += 2
            return self.pspair[i]
        i = self.psrr % len(self.psbank)
        self.psrr += 1
        return self.psbank[i]

    def build(self):
        nc = self.nc
        NB = self.NB
        dt = nc.dram_tensor
        I = "ExternalInput"
        self.d = d = {}

        def inp(name, shape, dtype=F32):
            d[name] = dt(name, list(shape), dtype, kind=I).ap()

        inp("x", [NB, T_LAT, D])
        inp("c", [NB, D])
        inp("ctx", [NB, T_CTX, D])
        inp("c_ctx", [D])
        inp("ada_w", [4, D, 3 * D])
        inp("ada_bc", [4, 128, 24])
        inp("npre_c", [4, 128, 8])
        inp("npost", [4, D])
        inp("mla_win", [2, D, 1920])

        inp("mla_qn_c", [2, 128, 2])
        inp("mla_kvn_c", [2, 128, 2])
        inp("mla_wuq", [2, 256, 2048])
        inp("mla_wukv", [2, 256, 2048])
        inp("mla_wout", [2, D, D])
        inp("ml_win", [2, D, 6144])
        inp("ml_cw_c", [2, 128, 16, 5])
        inp("ml_cb_c", [2, 128, 16])
        inp("ml_wq_c", [2, 128, 16, 4])
        inp("ml_wk_c", [2, 128, 16, 4])
        inp("ml_wv_c", [2, 128, 16, 4])
        inp("ml_wqT_c", [2, 128, 16, 4])
        inp("ml_wkT_c", [2, 128, 16, 4])
        inp("ml_wvT_c", [2, 128, 16, 4])
        inp("ml_wif", [2, 2, 6144, 16])
        inp("ml_bif_c", [2, 32, 1])
        inp("ml_hn_c", [2, 128, 16])
        inp("ml_sk_c", [2, 128, 16])
        inp("ml_wout", [2, 2 * D, D])
        inp("k_ident", [128, 128])
        inp("k_maskF", [128, 128])
        inp("k_maskB", [128, 128])
        inp("k_bdmask", [128, 128])
        inp("k_tq", [128, TA])
        inp("k_tkc", [128, TA])
        inp("k_tks", [128, TA])
        d["out"] = dt("out", [NB, T_LAT, D], F32, kind="ExternalOutput").ap()
        d["ctxs"] = dt("ctxs", [NB, T_CTX, D], F32, kind="Internal").ap()
        d["ptd"] = dt("ptd", [NT, 128, 16, 128], BF16, kind="Internal").ap()
        if self.debug:
            for name, shape in self.debug.items():
                d[name] = dt(name, list(shape), F32, kind="ExternalOutput").ap()

        with ExitStack() as st:
            self.st = st
            ARENA_BYTES = 200 * 1024
            self.arena = Arena(nc, st, ARENA_BYTES)
            pp = [st.enter_context(nc.psum_tensor("pp%d" % i, [128, 1024], F32)) for i in range(4)]
            self.pspair = []
            self.psbank = []
            for i in range(4):
                self.pspair.append(View("ps", pp[i][:, :], i * 4096, [1024], 4, gran=2048))
                for hh in range(2):
                    self.psbank.append(View("ps", pp[i][:, hh * 512:(hh + 1) * 512],
                                            i * 4096 + hh * 2048, [512], 4, gran=2048))
            self.psbf = []
            for i in range(4):
                for hh in range(2):
                    self.psbf.append(View("ps", pp[i][:, hh * 512:(hh + 1) * 512].bitcast(BF16),
                                          i * 4096 + hh * 2048, [1024], 2, gran=2048))
            self.psrr = 0
            self.psrr2 = 0
            self.setup()
            import os
            if os.environ.get("KBAR"):
                self.barrier()
            if self.stop != "setup":
                for b in range(NB):
                    for li in self.layers:
                        self.layer(b, li)
            self.S.emit(nc, st)
        return nc

    def setup(self):
        d = self.d
        A = self.arena
        P = Bump(A, 0, A.nbytes)
        self.persist = P
        al = P.alloc
        self.ident = al([128], F32)
        self.identb = al([128], BF16)
        self.maskF = al([128], F32)
        self.maskB = al([128], F32)
        self.bdmask = al([128], F32)
        self.ones = al([128], F32)
        self.onesb = al([128], BF16)
        self.modcol = al([4, 24, 3], F32)
        self.adab = al([4, 24], F32)
        self.npre = al([4, 8], F32)
        self.silc = al([8, 3], F32)
        self.hT = al([8, TA], BF16)
        self.junk = al([D], BF16)
        self.small = al([64], F32)
        self.acol = al([2, 8], F32)
        self.bcol = al([2, 8], F32)
        self.gcol = al([2, 8], F32)
        self.ssq_ = al([NT + 2, 8], F32)
        self.rr_ = al([NT + 2, 8], F32)
        self.ssq = lambda pr, cr: self.ssq_(pr, cr[0], (0, 1))
        self.rr = lambda pr, cr: self.rr_(pr, cr[0], (0, 1))
        self.cst = al([4], F32)
        craw = al([8, 3], F32)
        th = al([8, 3], F32)
        wst = [al([8, 128], F32) for _ in range(2)]
        self.layer_base = P.top
        for i_, v_ in enumerate((EPS, 4 * EPS, LN16, 1.0)):
            self.memset("dve", self.cst(None, (i_, i_ + 1)), v_)
        self.c_eps = self.cst(None, (0, 1))
        self.c_eps4 = self.cst(None, (1, 2))
        self.c_ln16 = self.cst(None, (2, 3))
        self.c_one = self.cst(None, (3, 4))
        for v, name in ((self.ident, "k_ident"), (self.maskF, "k_maskF"), (self.maskB, "k_maskB"),
                        (self.bdmask, "k_bdmask")):
            self.dma("sp", v(), d[name])
        self.copy("dve", self.identb(), self.ident())
        self.memset("dve", self.ones(), 1.0)
        self.memset("dve", self.onesb(), 1.0)
        self.dma("sp", self.adab(), d["ada_bc"].rearrange("l p c -> p l c"))
        self.dma("sp", self.npre(), d["npre_c"].rearrange("l p c -> p l c"))
        cc = self.small
        for v in range(3):
            if v < self.NB:
                src = d["c"][v].rearrange("(k p) -> p k", p=128)
            elif v == 2:
                src = d["c_ctx"].rearrange("(k p) -> p k", p=128)
            else:
                src = d["c_ctx"].rearrange("(k p) -> p k", p=128)
            self.S.dma("sp", (lambda o, i: (lambda h: h.dma_start(out=o, in_=i, allow_slow_non_contiguous=True)))(
                craw(None, None, v).ap, src), writes=craw(None, None, v).k)
        self.act(th(), craw(), AF.Tanh, scale=0.5)
        self.stt(self.silc(), th(), 1.0, craw(), ALU.add, ALU.mult)
        self.ts("dve", self.silc(), self.silc(), 0.5, ALU.mult)
        cnt = 0
        for l in range(4):
            if l not in self.layers:
                continue
            for n in range(24):
                w = wst[cnt % 2]
                cnt += 1
                self.dma("sp", w(), d["ada_w"][l].rearrange("(k p) n -> p k n", p=128)[:, :, n * 128:(n + 1) * 128])
                pst = self.ps()
                for k in range(8):
                    self.mm(pst(None, (0, 3)), w(None, k), self.silc(None, k), start=(k == 0), stop=(k == 7))
                self.ts("dve", self.modcol(None, l, n), pst(None, (0, 3)), self.adab(None, l, (n, n + 1)), ALU.add)

    def layer(self, b, li):
        d = self.d
        A = self.arena
        L = Bump(A, self.layer_base, A.nbytes)
        self.L = L
        is_mla = (li % 2 == 0)
        j = li // 2
        need_ctx = li < 3
        first = (li == self.layers[0])
        def xsrc(t):
            if t < 2:
                src = d["ctx"][b] if first else d["ctxs"][b]
                return src[t * 128:(t + 1) * 128, :], ("dram_ctx", b, t)
            src = d["x"][b] if first else d["out"][b]
            return src[(t - 2) * 128:(t - 1) * 128, :], ("dram_x", b, t)

        def xdst(t):
            if t < 2:
                return d["ctxs"][b][t * 128:(t + 1) * 128, :], ("dram_ctx", b, t)
            return d["out"][b][(t - 2) * 128:(t - 1) * 128, :], ("dram_x", b, t)

        for kind, v in ((0, 2), (1, b)):
            sc = self.modcol(None, li, (8, 16), v)
            sh = self.modcol(None, li, (0, 8), v)
            gt = self.modcol(None, li, (16, 24), v)
            self.stt(self.acol(None, kind), sc, 1.0, self.npre(None, li), ALU.add, ALU.mult)
            self.copy("dve", self.bcol(None, kind), sh)
            self.copy("dve", self.gcol(None, kind), gt)
        import os
        if os.environ.get("KSKIP"):
            L.top += int(os.environ["KSKIP"]) * 1024
        self.xg = L.alloc([4, D], F32)
        if self.stop == "A_mod":
            return
        ntl = NT
        if self.stop in ("A_t0", "A_t0b", "A_t0c"):
            ntl = 1
        elif self.stop == "A_n2p":
            ntl = 2
        elif self.stop and self.stop.startswith("A_n"):
            ntl = int(self.stop[3:])
        tl0 = 0
        if self.stop == "A_only1":
            tl0, ntl = 1, 2
        if self.stop == "A_only2":
            tl0, ntl = 2, 3
        for t in range(tl0, ntl):
            if self.stop == "A_n2p":
                self.psrr = 0
            xs = self.xg(None, t % 4)
            src, dk = xsrc(t)
            self.dma("sp", xs, src, in_keys=[dk])
            self.act(self.junk(), xs, AF.Square, accum=self.ssq(None, (t, t + 1)))
            self.act(self.rr(None, (t, t + 1)), self.ssq(None, (t, t + 1)), AF.Ln, scale=1.0 / D, bias=self.c_eps)
            self.act(self.rr(None, (t, t + 1)), self.rr(None, (t, t + 1)), AF.Exp, scale=-0.5)
            import os
            step = int(os.environ.get("KSTEP", "9"))
            if self.stop == "A_t0" or step <= 0:
                continue
            self.ts("dve", xs, xs, self.rr(None, (t, t + 1)), ALU.mult)
            if self.stop == "A_t0b" or step <= 1:
                continue
            kind = 0 if t < 2 else 1
            for kq in range(2):
                pst = self.ps()
                for kk in range(4):
                    k = kq * 4 + kk
                    self.tr(pst(None, (kk * 128, kk * 128 + 128)), self.xg(None, t % 4, (k * 128, k * 128 + 128)),
                            self.ident())
                if step <= 2:
                    continue
                for kk in range(4):
                    k = kq * 4 + kk
                    o = self.hT(None, k, (t * 128, t * 128 + 128))
                    i = pst(None, (kk * 128, kk * 128 + 128))
                    if kq == 0 and kk < 3:
                        self.act(o, i, AF.Identity, scale=self.acol(None, kind, (k, k + 1)),
                                 bias=self.bcol(None, kind, (k, k + 1)))
                    else:
                        self.ts("dve", o, i, self.acol(None, kind, (k, k + 1)), ALU.mult,
                                self.bcol(None, kind, (k, k + 1)), ALU.add)
        if self.stop in ("A_t0", "A_t0b", "A_t0c") or (self.stop and (self.stop.startswith("A_n") or self.stop.startswith("A_only"))):
            return
        if self.debug and "dbg_hT" in self.debug:
            tmpf = L.alloc([TA], F32)
            for k in range(8):
                self.copy("dve", tmpf(), self.hT(None, k))
                self.dma("sp", d["dbg_hT"][k * 128:(k + 1) * 128, :], tmpf())
            L.top = self.layer_base

        if self.stop in ("A", "A_nodbg"):
            return
        L.top = self.layer_base
        if is_mla:
            KC = 8
            self.mla(b, li, j, need_ctx)
        else:
            KC = 16
            self.mlstm(b, li, j, need_ctx)

        if self.stop in ("proj", "attn1"):
            return
        if is_mla:
            L.top = self.pt_end
            wout = L.alloc([8, D], BF16)
            for k4 in range(2):
                self.dma("pool", wout(None, (k4 * 4, k4 * 4 + 4)),
                         d["mla_wout"][j].rearrange("(k p) n -> p k n", p=128)[:, k4 * 4:(k4 + 1) * 4, :])
        else:
            L.top = self.layer_base
            wout = L.alloc([16, D], BF16)
            for k4 in range(4):
                self.dma("pool", wout(None, (k4 * 4, k4 * 4 + 4)),
                         d["ml_wout"][j].rearrange("(k p) n -> p k n", p=128)[:, k4 * 4:(k4 + 1) * 4, :])
            ptl = [L.alloc([16, 128], BF16) for _ in range(3)]
        xt = [L.alloc([D], F32) for _ in range(3)]
        t1 = [L.alloc([D], F32) for _ in range(3)]
        self.Gt = L.alloc([2, D], F32)
        self.npost_b = L.alloc([D], F32)
        self.dma("sp", self.npost_b(), d["npost"][li:li + 1, :].broadcast_to([128, D]))
        dg = L.alloc([128], F32)
        for kind in range(2):
            for hh in range(2):
                pst = self.ps()
                for kk in range(4):
                    k = hh * 4 + kk
                    self.ts("dve", dg(), self.ident(), self.gcol(None, kind, (k, k + 1)), ALU.mult)
                    self.mm(pst(None, (kk * 128, kk * 128 + 128)), self.ones(), dg())
                self.tt("dve", self.Gt(None, kind, (hh * 512, hh * 512 + 512)), pst(),
                        self.npost_b(None, (hh * 512, hh * 512 + 512)), ALU.mult)
        t_start = 0 if need_ctx else 2
        for t in range(t_start, NT):
            kind = 0 if t < 2 else 1
            if is_mla:
                lhs = lambda k: self.PT(None, k, (t * 128, t * 128 + 128))
            else:
                pl = ptl[t % 3]
                self.dma("sp", pl(), d["ptd"][t], in_keys=[("ptd", t)])
                lhs = lambda k: pl(None, k)
            y = self.ps(2)
            for hh in range(2):
                for k in range(KC):
                    self.mm(y(None, (hh * 512, hh * 512 + 512)), lhs(k), wout(None, k, (hh * 512, hh * 512 + 512)),
                            start=(k == 0), stop=(k == KC - 1))
            sq = self.ssq(None, (NT, NT + 1))
            r = self.rr(None, (NT, NT + 1))
            sq2 = self.ssq(None, (NT + 1, NT + 2))
            self.act(self.junk(None, (0, 512)), y(None, (0, 512)), AF.Square, accum=sq)
            self.act(self.junk(None, (512, 1024)), y(None, (512, 1024)), AF.Square, accum=sq2)
            self.tt("dve", sq, sq, sq2, ALU.add)
            self.act(r, sq, AF.Ln, scale=1.0 / D, bias=self.c_eps)
            self.act(r, r, AF.Exp, scale=-0.5)
            x_ = xt[t % 3]
            src, dk = xsrc(t)
            self.dma("sp", x_(), src, in_keys=[dk])
            tt_ = t1[t % 3]
            for hh in range(2):
                cs = (hh * 512, hh * 512 + 512)
                self.stt(tt_(None, cs), y(None, cs), r, self.Gt(None, kind, cs), ALU.mult, ALU.mult)
            self.tt("pool", x_(), x_(), tt_(), ALU.add)
            dst, dk = xdst(t)
            self.dma("pool", dst, x_(), out_keys=[dk])

    def mla(self, b, li, j, need_ctx):
        d = self.d
        L = self.L
        al = L.alloc
        self.PT = al([8, TA], BF16)
        self.pt_end = L.top
        cqn = al([2, TA], BF16)
        ckvn = al([2, TA], BF16)
        Qh = [al([TA], BF16) for _ in range(2)]
        Kh = [al([TA], BF16) for _ in range(2)]
        Vh = [al([NT, 128], BF16) for _ in range(2)]
        wuq = al([2, 2048], BF16)
        wukv = al([2, 2048], BF16)
        ws = [al([8, 256], BF16) for _ in range(3)]
        tq = al([TA], BF16)
        tks = [al([512], F32) for _ in range(4)]
        pexp = [al([1024], BF16) for _ in range(4)]
        rd = [al([512], F32) for _ in range(2)]
        thb = [al([512], F32) for _ in range(2)]
        sqb = [al([512], BF16) for _ in range(2)]
        rb = [al([512], F32) for _ in range(2)]
        qn = al([2], F32)
        kvn = al([2], F32)
        tmp_top = L.top

        win = d["mla_win"][j].rearrange("(k p) n -> p k n", p=128)
        self.dma("sp", qn(), d["mla_qn_c"][j])
        self.dma("sp", kvn(), d["mla_kvn_c"][j])
        self.dma("pool", tq(), d["k_tq"])
        self.dma("pool", wuq(), d["mla_wuq"][j].rearrange("(k p) n -> p k n", p=128))
        self.dma("pool", wukv(), d["mla_wukv"][j].rearrange("(k p) n -> p k n", p=128))
        for s in range(2):
            self.memset("dve", Vh[s](None, None, (64, 128)), 2.0)
        ntiles = [(0, 256)] + [(256 + 512 * i, 256 + 512 * (i + 1)) for i in range(4)]
        wsi = 0
        for gi, (dst, gn) in enumerate(((cqn, qn), (ckvn, kvn))):
            w = ws[wsi % 3]
            wsi += 1
            self.dma("pool", w(), win[:, :, gi * 256:(gi + 1) * 256])
            for ni, (n0, n1) in enumerate(ntiles):
                n = n1 - n0
                pc = [self.ps(), self.ps()]
                for cch in range(2):
                    for k in range(8):
                        self.mm(pc[cch](None, (0, n)), w(None, k, (cch * 128, cch * 128 + 128)),
                                self.hT(None, k, (n0, n1)), start=(k == 0), stop=(k == 7))
                pss = self.ps()
                for cch in range(2):
                    sq = sqb[cch]
                    self.act(sq(None, (0, n)), pc[cch](None, (0, n)), AF.Square)
                    self.mm(pss(None, (0, n)), self.onesb(), sq(None, (0, n)), start=(cch == 0), stop=(cch == 1))
                r = rb[ni % 2]
                self.act(r(None, (0, n)), pss(None, (0, n)), AF.Ln, scale=1.0 / 256, bias=self.c_eps)
                self.act(r(None, (0, n)), r(None, (0, n)), AF.Exp, scale=-0.5)
                for cch in range(2):
                    self.stt(dst(None, cch, (n0, n1)), pc[cch](None, (0, n)), gn(None, (cch, cch + 1)),
                             r(None, (0, n)), ALU.mult, ALU.mult)
        w = ws[wsi % 3]
        wsi += 1
        self.dma("pool", w(), win[:, :, 512:768])
        H1 = (64, 128)
        H0 = (0, 64)
        for ni, (n0, n1) in enumerate(ntiles):
            n = n1 - n0
            pa = self.ps()
            pb = self.ps()
            wa = w
            for k in range(8):
                self.mm(pa(None, (0, n)), wa(None, k, (0, 128)), self.hT(None, k, (n0, n1)), start=(k == 0), stop=(k == 7))
            for k in range(8):
                self.mm(pb(None, (0, n)), w(None, k, (128, 256)), self.hT(None, k, (n0, n1)), start=(k == 0), stop=(k == 7))
            tc_ = tks[(2 * ni) % 4]
            ts_ = tks[(2 * ni + 1) % 4]
            self.dma("sp", tc_(None, (0, n)), d["k_tkc"][:, n0:n1])
            self.dma("sp", ts_(None, (0, n)), d["k_tks"][:, n0:n1])
            u = thb[0]
            v = thb[1]
            self.tt("dve", u(H1, (0, n)), pa(H1, (0, n)), tc_(H1, (0, n)), ALU.mult)
            self.tt("dve", v(H1, (0, n)), pb(H1, (0, n)), ts_(H1, (0, n)), ALU.mult)
            self.tt("pool", Kh[0](H1, (n0, n1)), u(H1, (0, n)), v(H1, (0, n)), ALU.add)
            self.copy("pool", Kh[1](H1, (n0, n1)), Kh[0](H1, (n0, n1)))
        for gc in range(8):
            if gc % 2 == 0:
                w = ws[wsi % 3]
                wsi += 1
                self.dma("pool", w(), win[:, :, 896 + gc * 128: 896 + gc * 128 + 256])
            co = (gc % 2) * 128
            for ni, (n0, n1) in enumerate(ntiles):
                n = n1 - n0
                pg = self.ps()
                for k in range(8):
                    self.mm(pg(None, (0, n)), w(None, k, (co, co + 128)), self.hT(None, k, (n0, n1)),
                            start=(k == 0), stop=(k == 7))
                th = thb[ni % 2]
                self.act(th(None, (0, n)), pg(None, (0, n)), AF.Tanh, scale=0.5)
                self.stt(self.PT(None, gc, (n0, n1)), th(None, (0, n)), 1.0, pg(None, (0, n)), ALU.add, ALU.mult)
        self.dump("dbg_cqn", cqn, 2)
        self.dump("dbg_ckvn", ckvn, 2)
        self.dump("dbg_SG", self.PT, 8)
        if self.stop == "proj":
            return
        qchunks = ([(0, 256, 2)] if need_ctx else []) + [(256 + 512 * i, 256 + 512 * (i + 1), NT) for i in range(4)]
        acnt = [0]
        gcnt = [0]
        nheads = 1 if self.stop == "attn1" else NH

        def proj(h):
                s = h % 2
                Q = Qh[s]
                K = Kh[s]
                V = Vh[s]
                pj = [self.psbank[i_] for i_ in range(6)]
                pjc = 0
                for ni, (n0, n1) in enumerate(ntiles):
                    n = n1 - n0
                    pq = pj[pjc % 6]
                    pjc += 1
                    for k in range(2):
                        lq = wuq(None, k, (h * 128, h * 128 + 128))
                        self.mm(pq(None, (0, n)), lq, cqn(None, k, (n0, n1)),
                                start=(k == 0), stop=(k == 1))
                    self.copy("dve", Q(H0, (n0, n1)), pq(H0, (0, n)))
                    self.tt("dve", Q(H1, (n0, n1)), pq(H1, (0, n)), tq(H1, (n0, n1)), ALU.mult)
                    pk = pj[pjc % 6]
                    pjc += 1
                    for k in range(2):
                        self.mm(pk(H0, (0, n)), wukv(None, k, (h * 128, h * 128 + 64)), ckvn(None, k, (n0, n1)),
                                start=(k == 0), stop=(k == 1))
                    self.copy("dve", K(H0, (n0, n1)), pk(H0, (0, n)))
                for g0 in range(0, NT, 8):
                    g1 = min(NT, g0 + 8)
                    pv = pj[pjc % 6]
                    pjc += 1
                    for t in range(g0, g1):
                        for k in range(2):
                            self.mm(pv(None, ((t - g0) * 64, (t - g0) * 64 + 64)), ckvn(None, k, (t * 128, t * 128 + 128)),
                                    wukv(None, k, (h * 128 + 64, h * 128 + 128)), start=(k == 0), stop=(k == 1))
                    ng = g1 - g0
                    src = X(pv.ap[:, 0:ng * 64].rearrange("p (t c) -> p t c", c=64), pv(None, (0, ng * 64)).k)
                    self.copy("dve", V(None, (g0, g1), (0, 64)), src)

        proj(0)
        for h in range(nheads):
            s = h % 2
            Q = Qh[s]
            K = Kh[s]
            V = Vh[s]
            if h == 0:
                self.dump("dbg_Q0", Q)
                self.dump("dbg_K0", K)
            hp = (h % 2) * 64
            HP = (hp, hp + 64)
            steps = []
            accs_of = {}
            for qi, (q0, q1, nk) in enumerate(qchunks):
                accs_of[qi] = self.psbank[6 + (acnt[0] % 2)]
                acnt[0] += 1
                for kt in range(0, nk, 2):
                    steps.append((qi, q0, q1, nk, kt))

            def scores_exp(st, g):
                qi, q0, q1, nk, kt = st
                nq = q1 - q0
                sp_ = self.pspair[g % 3]
                for u2 in range(2):
                    self.mm(sp_(None, (u2 * 512, u2 * 512 + nq)), K(None, ((kt + u2) * 128, (kt + u2) * 128 + 128)),
                            Q(None, (q0, q1)))
                pe_ = pexp[g % 4]
                if nq == 512:
                    self.act(pe_(), sp_(), AF.Exp, scale=ATT_SCALE)
                else:
                    for u2 in range(2):
                        self.act(pe_(None, (u2 * 512, u2 * 512 + nq)), sp_(None, (u2 * 512, u2 * 512 + nq)),
                                 AF.Exp, scale=ATT_SCALE)

            def pv(st, g):
                qi, q0, q1, nk, kt = st
                nq = q1 - q0
                acc = accs_of[qi]
                pe_ = pexp[g % 4]
                for u2 in range(2):
                    self.mm(acc(None, (0, nq)), V(None, kt + u2), pe_(None, (u2 * 512, u2 * 512 + nq)),
                            start=(kt + u2 == 0), stop=(kt + u2 == nk - 1))

            def evac(st):
                qi, q0, q1, nk, kt = st
                nq = q1 - q0
                acc = accs_of[qi]
                r = rd[qi % 2]
                accs = thb[qi % 2]
                ptv = self.PT(HP, h // 2, (q0, q1))
                if hp == 0:
                    self.copy("dve", accs(None, (0, nq)), acc(None, (0, nq)))
                    self.recip(r(H0, (0, nq)), accs(H1, (0, nq)))
                    self.tt("pool", r(H0, (0, nq)), r(H0, (0, nq)), ptv, ALU.mult)
                    self.tt("pool", ptv, accs(H0, (0, nq)), r(H0, (0, nq)), ALU.mult)
                else:
                    self.copy("dve", accs(H1, (0, nq)), acc(H0, (0, nq)))
                    self.copy("dve", accs(H0, (0, nq)), acc(H1, (0, nq)))
                    self.recip(r(H1, (0, nq)), accs(H0, (0, nq)))
                    self.tt("pool", r(H1, (0, nq)), r(H1, (0, nq)), ptv, ALU.mult)
                    self.tt("pool", ptv, accs(H1, (0, nq)), r(H1, (0, nq)), ALU.mult)

            g0_ = gcnt[0]
            ns = len(steps)
            scores_exp(steps[0], g0_)
            if ns > 1:
                scores_exp(steps[1], g0_ + 1)
            for i_, st in enumerate(steps):
                if i_ + 2 < ns:
                    scores_exp(steps[i_ + 2], g0_ + i_ + 2)
                pv(st, g0_ + i_)
                if st[4] + 2 >= st[3]:
                    evac(st)
                if i_ == ns // 2 and h + 1 < nheads:
                    proj(h + 1)
            gcnt[0] += ns
        self.dump("dbg_PT", self.PT, 8)

    def mlstm(self, b, li, j, need_ctx):
        d = self.d
        L = self.L
        al = L.alloc
        XW = 2312
        ntiles = [(0, 256)] + [(256 + 512 * i, 256 + 512 * (i + 1)) for i in range(4)]

        def PO(n0):
            return 2 + n0 if n0 < 256 else 260 + (n0 - 256)

        cw = al([16, 5], F32)
        cb = al([16], F32)
        hbc = al([16], F32)
        hgc = al([16], F32)
        skc = al([16], F32)
        wsm = [al([16, 4], F32) for _ in range(6)]
        BD = [al([16, 128], BF16) for _ in range(3)]
        Wc = al([16, 32], BF16)
        Wm = al([16, 32], BF16)
        bif = al([1], F32)
        gtok = al([NT, 32], F32)
        spc = al([NT * 8], F32)
        tmpg = al([NT * 8], F32)
        alpha = [al([NT, 8], F32) for _ in range(2)]
        ib = [al([NT, 8], F32) for _ in range(2)]
        decay = [al([NT, 8], F32) for _ in range(2)]
        aend = [al([NT, 8], F32) for _ in range(2)]
        C32 = [al([2, 260], F32) for _ in range(2)]
        Cbf = [al([2, 260], BF16) for _ in range(2)]
        AT = [al([128], BF16) for _ in range(4)]
        khat = [al([256], BF16) for _ in range(4)]
        mcol = [al([8], F32) for _ in range(4)]
        dgc = [al([5, 128], BF16) for _ in range(1)]
        tA = [al([512], F32) for _ in range(2)]
        tB = [al([512], F32) for _ in range(2)]
        ssqh = al([NT], F32)
        rrh = al([NT], F32)
        hnb = [L.a.view(tB[i_].off, [256], BF16) for i_ in range(2)]
        preb = [al([2, 128], BF16) for _ in range(2)]
        utmp = [L.a.view(tA[i_].off, [128], F32) for i_ in range(2)]
        wxm = [al([8, 256], BF16) for _ in range(2)]
        wzo = [al([8, 512], BF16) for _ in range(2)]
        B1 = al([2, XW], BF16)
        B2 = al([2, TA], BF16)
        B3 = al([2, TA], BF16)
        B4 = al([2, TA], BF16)
        B5 = al([NT, 256], BF16)
        B6 = al([NT, 258], BF16)
        HF = al([NT, 256], BF16)
        HBv = al([NT, 256], BF16)
        gT = L.a.view(HF.off, [TA], F32)
        setup_top = L.top

        self.dma("sp", cw(), d["ml_cw_c"][j])
        self.dma("sp", cb(), d["ml_cb_c"][j])
        self.dma("sp", hgc(), d["ml_hn_c"][j])
        self.dma("sp", skc(), d["ml_sk_c"][j])
        self.dma("sp", bif((0, 32)), d["ml_bif_c"][j])
        for i, nm in enumerate(("ml_wq_c", "ml_wk_c", "ml_wv_c", "ml_wqT_c", "ml_wkT_c", "ml_wvT_c")):
            self.dma("sp", wsm[i](), d[nm][j])
        self.ts("dve", hbc(), cb(), 0.5, ALU.mult)
        self.ts("dve", hgc(), hgc(), 0.5, ALU.mult)
        self.ts("dve", skc(), skc(), 0.5, ALU.mult)

        def bd_build(out_x, wv, jj):
            w_ap = wv(None, jj).ap.unsqueeze(1).broadcast_to([128, 32, 4])
            m_ap = self.bdmask().ap.rearrange("p (m d) -> p m d", d=4)
            o_ap = out_x.ap.rearrange("p (m d) -> p m d", d=4)
            self.tt("dve", X(o_ap, out_x.k), X(w_ap, wv(None, jj).k), X(m_ap, self.bdmask().k), ALU.mult)

        for jj in range(16):
            for i in range(3):
                bd_build(BD[i](None, jj), wsm[i], jj)
        L.top = B3.off
        wif = L.alloc([48, 32], F32)
        for dd in range(2):
            for q4 in range(4):
                self.dma("sp", wif(None, (q4 * 12, q4 * 12 + 12), (dd * 16, dd * 16 + 16)),
                         d["ml_wif"][j][dd].rearrange("(k p) g -> p k g", p=128)[:, q4 * 12:(q4 + 1) * 12, :])
        bdt = [L.alloc([128], F32) for _ in range(3)]
        for jj in range(16):
            for i in range(3):
                bd_build(bdt[i](), wsm[3 + i], jj)
            p1 = self.ps()
            self.mm(p1(None, (0, 32)), bdt[0](), wif(None, jj), start=True, stop=False)
            self.mm(p1(None, (0, 32)), bdt[1](), wif(None, 16 + jj), start=False, stop=True)
            self.copy("dve", Wc(None, jj), p1(None, (0, 32)))
            p2 = self.ps()
            self.mm(p2(None, (0, 32)), bdt[2](), wif(None, 32 + jj))
            self.copy("dve", Wm(None, jj), p2(None, (0, 32)))
        L.top = setup_top
        for cc in range(2):
            self.memset("dve", B1(None, cc, (0, 2)), 0.0)
            self.memset("dve", B1(None, cc, (258, 260)), 0.0)
            self.memset("dve", B1(None, cc, (2308, XW)), 0.0)
        self.memset("dve", B6(None, None, (256, 258)), 1.0)

        win = d["ml_win"][j].rearrange("(k p) n -> p k n", p=128)
        dcnt = [0]
        pending = []

        def features(w, cc, jj, gates_first):
            for ni, (n0, n1) in enumerate(ntiles):
                n = n1 - n0
                pm = self.ps()
                for k in range(8):
                    self.mm(pm(None, (0, n)), w(None, k, (cc * 128, cc * 128 + 128)), self.hT(None, k, (n0, n1)),
                            start=(k == 0), stop=(k == 7))
                self.copy("act", B1(None, cc, (PO(n0), PO(n0) + n)), pm(None, (0, n)))
            while pending:
                pending.pop(0)()
            dg = dgc[0]
            dcnt[0] += 1
            for tap in range(5):
                self.ts("dve", dg(None, tap), self.identb(), cw(None, jj, (tap, tap + 1)), ALU.mult)
            for ni, (n0, n1) in enumerate(ntiles):
                n = n1 - n0
                pc = self.ps()
                for tap in range(5):
                    c0 = PO(n0) + tap - 2
                    self.mm(pc(None, (0, n)), dg(None, tap), B1(None, cc, (c0, c0 + n)),
                            start=(tap == 0), stop=(tap == 4))
                ta = tA[ni % 2]
                tb = tB[ni % 2]
                self.ts("dve", ta(None, (0, n)), pc(None, (0, n)), 0.5, ALU.mult, hbc(None, (jj, jj + 1)), ALU.add)
                self.act(tb(None, (0, n)), pc(None, (0, n)), AF.Tanh, scale=0.5, bias=hbc(None, (jj, jj + 1)))
                self.stt(B2(None, cc, (n0, n1)), tb(None, (0, n)), 1.0, ta(None, (0, n)), ALU.add, ALU.mult)
            if gates_first is not None:
                def gates(cc=cc, jj=jj, gates_first=gates_first):
                    for ni, (n0, n1) in enumerate(ntiles):
                        n = n1 - n0
                        pg = self.ps()
                        R32 = (0, 32)
                        self.mm(pg(R32, (0, n)), Wc(None, jj), B2(None, cc, (n0, n1)), start=True, stop=False)
                        self.mm(pg(R32, (0, n)), Wm(None, jj), B1(None, cc, (PO(n0), PO(n0) + n)), start=False, stop=True)
                        if gates_first:
                            self.ts("dve", gT(R32, (n0, n1)), pg(R32, (0, n)), bif(R32), ALU.add)
                        else:
                            self.tt("dve", gT(R32, (n0, n1)), pg(R32, (0, n)), gT(R32, (n0, n1)), ALU.add)
                pending.append(gates)

        wcnt = 0
        for hh in range(8):
            w = wxm[wcnt % 2]
            wcnt += 1
            self.dma("pool", w(), win[:, :, hh * 256:(hh + 1) * 256])
            for cc in range(2):
                features(w, cc, hh * 2 + cc, gates_first=(hh == 0 and cc == 0))
        while pending:
            pending.pop(0)()
        R32 = (0, 32)
        pgA = self.ps()
        pgB = self.ps()
        for c in range(NT):
            dst = pgA(None, (c * 32, c * 32 + 32)) if c < 16 else pgB(None, ((c - 16) * 32, (c - 16) * 32 + 32))
            idn = X(self.ident.ap[0:32, 0:32], self.ident().k)
            self.tr(dst, gT(R32, (c * 128, c * 128 + 128)), idn)
        self.copy("dve", X(gtok.ap[:, 0:16, :], gtok(None, (0, 16)).k),
                  X(pgA.ap.rearrange("p (c g) -> p c g", g=32), pgA().k))
        self.copy("dve", X(gtok.ap[:, 16:18, :], gtok(None, (16, 18)).k),
                  X(pgB.ap[:, 0:64].rearrange("p (c g) -> p c g", g=32), pgB(None, (0, 64)).k))
        for dd in range(2):
            li_x = X(gtok.ap[:, :, dd * 16:dd * 16 + 8], gtok().k)
            fp_x = X(gtok.ap[:, :, dd * 16 + 8:dd * 16 + 16], gtok().k)
            sp3 = X(spc.ap.rearrange("p (c g) -> p c g", g=8), spc().k)
            tm3 = X(tmpg.ap.rearrange("p (c g) -> p c g", g=8), tmpg().k)
            self.act(sp3, fp_x, AF.Exp, scale=-1.0)
            self.act(spc(), spc(), AF.Ln, bias=self.c_one)
            tri = self.maskF if dd == 0 else self.maskB
            pcum = self.ps()
            ptot = self.ps()
            NG = NT * 8
            self.mm(pcum(None, (0, NG)), tri(), spc())
            self.mm(ptot(None, (0, NG)), self.ones(), spc())
            cum3 = X(pcum.ap[:, 0:NG].rearrange("p (c g) -> p c g", g=8), pcum(None, (0, NG)).k)
            self.tt("dve", tm3, cum3, li_x, ALU.add)
            self.act(alpha[dd](), tm3, AF.Exp)
            self.act(ib[dd](), cum3, AF.Exp, bias=self.c_ln16)
            tot3 = X(ptot.ap[:, 0:NG].rearrange("p (c g) -> p c g", g=8), ptot(None, (0, NG)).k)
            self.act(decay[dd](), tot3, AF.Exp, scale=-1.0)
            self.tt("pool", aend[dd](), decay[dd](), alpha[dd](), ALU.mult)

        order = [list(range(NT)), [1, 0] + list(range(NT - 1, 1, -1))]
        c_lo = 0 if need_ctx else 2
        slot = [0]
        def load_head_w(h2):
            w_ = wxm[(wcnt0 + h2) % 2]
            self.dma("pool", w_(), win[:, :, h2 * 256:(h2 + 1) * 256])
            wz_ = wzo[h2 % 2]
            self.dma("pool", wz_(None, None, (0, 256)), win[:, :, 2048 + h2 * 256: 2048 + (h2 + 1) * 256])
            self.dma("pool", wz_(None, None, (256, 512)), win[:, :, 4096 + h2 * 256: 4096 + (h2 + 1) * 256])

        wcnt0 = wcnt
        load_head_w(0)
        for hh in range(8):
            w = wxm[(wcnt0 + hh) % 2]
            wz = wzo[hh % 2]
            for cc in range(2):
                features(w, cc, hh * 2 + cc, gates_first=None)
            for cc in range(2):
                jj = hh * 2 + cc
                for (dstB, bd) in ((B3, BD[0]), (B4, BD[1])):
                    for ni, (n0, n1) in enumerate(ntiles):
                        n = n1 - n0
                        pq = self.ps()
                        self.mm(pq(None, (0, n)), bd(None, jj), B2(None, cc, (n0, n1)))
                        if ni % 2 == 0:
                            self.copy("act", dstB(None, cc, (n0, n1)), pq(None, (0, n)))
                        else:
                            self.copy("dve", dstB(None, cc, (n0, n1)), pq(None, (0, n)))
            for c in range(0, NT, 2):
                pk = self.ps()
                pv = self.ps()
                for u in range(2):
                    for cc in range(2):
                        jj = hh * 2 + cc
                        t0 = (c + u) * 128
                        self.mm(pk(None, (u * 256 + cc * 128, u * 256 + cc * 128 + 128)),
                                B2(None, cc, (t0, t0 + 128)), BD[1](None, jj))
                        self.mm(pv(None, (u * 256 + cc * 128, u * 256 + cc * 128 + 128)),
                                B1(None, cc, (PO(t0), PO(t0) + 128)), BD[2](None, jj))
                self.copy("act", B5(None, (c, c + 2)), X(pk.ap.rearrange("p (u f) -> p u f", f=256), pk().k))
                self.copy("dve", B6(None, (c, c + 2), (0, 256)), X(pv.ap.rearrange("p (u f) -> p u f", f=256), pv().k))
            for cc in range(2):
                self.ts("dve", B2(None, cc), B2(None, cc), skc(None, (hh * 2 + cc, hh * 2 + cc + 1)), ALU.mult)
            def front(step, dd):
                c = order[dd][step]
                sl = (step % 2) * 2 + dd
                t0 = c * 128
                pS = self.psbank[dd]
                for e in range(2):
                    self.mm(pS(None, (0, 128)), B4(None, e, (t0, t0 + 128)), B3(None, e, (t0, t0 + 128)),
                            start=(e == 0), stop=(e == 1))
                msk = self.maskF if dd == 0 else self.maskB
                self.stt(AT[sl](), pS(None, (0, 128)), alpha[dd](None, c, (hh, hh + 1)), msk(), ALU.mult, ALU.mult)
                self.act(khat[sl](), B5(None, c), AF.Copy, scale=aend[dd](None, c, (hh, hh + 1)))

            def back(step, dd):
                c = order[dd][step]
                first = (step == 0)
                sl = (step % 2) * 2 + dd
                t0 = c * 128
                po = self.psbank[2 + dd]
                self.mm(po(None, (0, 257)), AT[sl](), B6(None, c, (0, 257)), start=True, stop=first)
                if not first:
                    for e in range(2):
                        self.mm(po(None, (0, 257)), B3(None, e, (t0, t0 + 128)), Cbf[dd](None, e, (0, 257)),
                                start=False, stop=(e == 1))
                if step < NT - 1:
                    pU = self.pspair[2 + dd]
                    for e in range(2):
                        self.mm(pU(None, (e * 512, e * 512 + 257)), khat[sl](None, (e * 128, e * 128 + 128)),
                                B6(None, c, (0, 257)))
                    u3 = X(pU.ap.rearrange("p (e f) -> p e f", f=512)[:, :, 0:257], pU().k)
                    c3 = X(C32[dd].ap[:, :, 0:257], C32[dd]().k)
                    if first:
                        self.copy("dve", c3, u3)
                    else:
                        self.stt(c3, c3, decay[dd](None, c, (hh, hh + 1)), u3, ALU.mult, ALU.add)
                    self.copy("pool", Cbf[dd](), C32[dd]())
                if c >= c_lo:
                    m = mcol[sl]
                    self.act(m(None, (0, 1)), po(None, (256, 257)), AF.Abs)
                    self.ts("dve", m(None, (0, 1)), m(None, (0, 1)), ib[dd](None, c, (hh, hh + 1)), ALU.max)
                    self.recip(m(None, (1, 2)), m(None, (0, 1)))
                    hdst = HF if dd == 0 else HBv
                    self.act(hdst(None, c), po(None, (0, 256)), AF.Copy, scale=m(None, (1, 2)))

            if hh + 1 < 8:
                load_head_w(hh + 1)
            front(0, 0)
            front(0, 1)
            for step in range(NT):
                for dd in range(2):
                    if step + 1 < NT:
                        front(step + 1, dd)
                    back(step, dd)
            for c in range(c_lo, NT):
                t0 = c * 128
                pog = self.ps()
                for k in range(8):
                    self.mm(pog(None, (0, 256)), self.hT(None, k, (t0, t0 + 128)), wz(None, k, (256, 512)),
                            start=(k == 0), stop=(k == 7))
                ta = tA[c % 2]
                tb = tB[c % 2]
                self.act(ta(None, (0, 256)), pog(None, (0, 256)), AF.Tanh, scale=0.5)
                self.tt("pool", tb(None, (0, 256)), HF(None, c), HBv(None, c), ALU.add)
                self.stt(HF(None, c), ta(None, (0, 256)), 1.0, tb(None, (0, 256)), ALU.add, ALU.mult)
                self.act(self.junk(None, (0, 256)), HF(None, c), AF.Square, accum=ssqh(None, (c, c + 1)))
            for cc in range(2):
                for ni, (n0, n1) in enumerate(ntiles):
                    n = n1 - n0
                    if n1 <= 256 and not need_ctx:
                        continue
                    pz = self.ps()
                    for k in range(8):
                        self.mm(pz(None, (0, n)), wz(None, k, (cc * 128, cc * 128 + 128)), self.hT(None, k, (n0, n1)),
                                start=(k == 0), stop=(k == 7))
                    tb = tB[ni % 2]
                    self.act(tb(None, (0, n)), pz(None, (0, n)), AF.Tanh, scale=0.5)
                    self.stt(B4(None, cc, (n0, n1)), tb(None, (0, n)), 1.0, pz(None, (0, n)), ALU.add, ALU.mult)
            self.act(rrh(None, (c_lo, NT)), ssqh(None, (c_lo, NT)), AF.Ln, scale=1.0 / 256, bias=self.c_eps4)
            self.act(rrh(None, (c_lo, NT)), rrh(None, (c_lo, NT)), AF.Exp, scale=-0.5)
            for c in range(c_lo, NT):
                t0 = c * 128
                hn = hnb[c % 2]
                self.act(hn(), HF(None, c), AF.Copy, scale=rrh(None, (c, c + 1)))
                pT = self.psbf[(self.psrr) % 8]
                self.psrr += 1
                for cc in range(2):
                    self.tr(pT(None, (cc * 128, cc * 128 + 128)), hn(None, (cc * 128, cc * 128 + 128)), self.identb())
                pre = preb[c % 2]
                for cc in range(2):
                    jj = hh * 2 + cc
                    u = utmp[cc]
                    self.stt(u(), pT(None, (cc * 128, cc * 128 + 128)), hgc(None, (jj, jj + 1)),
                             B2(None, cc, (t0, t0 + 128)), ALU.mult, ALU.add)
                    self.tt("pool", pre(None, cc), u(), B4(None, cc, (t0, t0 + 128)), ALU.mult)
                self.dma("sp", d["ptd"][c][:, hh * 2:hh * 2 + 2, :], pre(), out_keys=[("ptd", c)])


def _rope_tables():
    rows = T_LAT // 64
    row = np.repeat(np.arange(rows), 64).astype(np.float32)
    col = np.tile(np.arange(64), rows).astype(np.float32)
    inv = (10000.0 ** (-np.arange(8, dtype=np.float32) / 8)).astype(np.float32)
    cr = np.cos(row[:, None] * inv)
    sr = np.sin(row[:, None] * inv)
    cc = np.cos(col[:, None] * inv)
    sc = np.sin(col[:, None] * inv)
    C = np.concatenate([cr, cr, cc, cc], axis=1).T
    Sg = np.concatenate([-sr, sr, -sc, sc], axis=1).T
    Cf = np.concatenate([np.ones((32, T_CTX), np.float32), C], axis=1).astype(np.float32)
    Sf = np.concatenate([np.zeros((32, T_CTX), np.float32), Sg], axis=1).astype(np.float32)
    tq = np.concatenate([Cf, Sf, Cf, Sf], axis=0)
    tkc = np.concatenate([Cf] * 4, axis=0)
    tks = np.concatenate([Sf] * 4, axis=0)
    return np.ascontiguousarray(tq), np.ascontiguousarray(tkc), np.ascontiguousarray(tks)


PERM_A = np.arange(32)
PERM_B = np.concatenate([np.arange(8, 16), np.arange(0, 8), np.arange(24, 32), np.arange(16, 24)])


def _col(v, nch):
    s = v.shape[:-1]
    return np.ascontiguousarray(np.swapaxes(v.reshape(s + (nch, 128)), -1, -2))


def prep_shared(inp):
    f = lambda a: np.ascontiguousarray(np.asarray(a, dtype=np.float32))
    o = {}
    o["c_ctx"] = f(inp["c_ctx"])
    o["ada_w"] = f(inp["ada_w"])
    o["ada_bc"] = _col(f(inp["ada_b"]), 24)
    o["npre_c"] = _col(f(inp["norm_pre"]), 8)
    o["npost"] = f(inp["norm_post"])
    w = f(inp["mla_w_in"])
    kr = w[:, :, 512:544]
    krA = np.tile(kr[:, :, PERM_A], (1, 1, 4))
    krB = np.tile(kr[:, :, PERM_B], (1, 1, 4))
    krO = np.tile(kr, (1, 1, 4))
    o["mla_win"] = np.ascontiguousarray(np.concatenate([w[:, :, 0:512], krA, krB, krO, w[:, :, 544:]], axis=2))
    o["mla_qn_c"] = _col(f(inp["mla_q_norm"]), 2)
    o["mla_kvn_c"] = _col(f(inp["mla_kv_norm"]), 2)
    wq = f(inp["mla_w_uq"]).reshape(2, 256, NH, 96)
    rope = wq[..., 64:]
    o["mla_wuq"] = np.ascontiguousarray(
        np.concatenate([wq[..., :64], rope[..., PERM_A], rope[..., PERM_B]], axis=-1).reshape(2, 256, 2048))
    o["mla_wukv"] = f(inp["mla_w_ukv"])
    o["mla_wout"] = f(inp["mla_w_out"])
    o["ml_win"] = f(inp["ml_w_in"])
    o["ml_cw_c"] = np.ascontiguousarray(np.transpose(f(inp["ml_conv_w"]).reshape(2, 5, 16, 128), (0, 3, 2, 1)))
    o["ml_cb_c"] = _col(f(inp["ml_conv_b"]), 16)
    for nm, key in (("ml_wq", "ml_w_q"), ("ml_wk", "ml_w_k"), ("ml_wv", "ml_w_v")):
        wqk = f(inp[key])
        a = wqk.reshape(2, 16, 32, 4, 4)
        o[nm + "_c"] = np.ascontiguousarray(np.transpose(a, (0, 2, 3, 1, 4)).reshape(2, 128, 16, 4))
        o[nm + "T_c"] = np.ascontiguousarray(np.transpose(a, (0, 2, 4, 1, 3)).reshape(2, 128, 16, 4))
    o["ml_wif"] = f(inp["ml_w_if"])
    o["ml_bif_c"] = np.ascontiguousarray(f(inp["ml_b_if"]).reshape(2, 32, 1))
    o["ml_hn_c"] = _col(f(inp["ml_head_norm"]), 16)
    o["ml_sk_c"] = _col(f(inp["ml_skip"]), 16)
    o["ml_wout"] = f(inp["ml_w_out"])
    o["k_ident"] = np.eye(128, dtype=np.float32)
    o["k_maskF"] = np.triu(np.ones((128, 128), np.float32))
    o["k_maskB"] = np.tril(np.ones((128, 128), np.float32))
    o["k_bdmask"] = np.kron(np.eye(32, dtype=np.float32), np.ones((4, 4), np.float32))
    o["k_tq"], o["k_tkc"], o["k_tks"] = _rope_tables()
    return o


_CACHE = {}


def kernel(**inputs):
    n_cores = 8
    NB = 2
    key = ("full",)
    if key not in _CACHE:
        _CACHE[key] = Prog(NB, [0, 1, 2, 3]).build()
    nc = _CACHE[key]
    shared = prep_shared(inputs)
    x = np.asarray(inputs["x"], dtype=np.float32)
    c = np.asarray(inputs["c"], dtype=np.float32)
    ctx = np.asarray(inputs["ctx"], dtype=np.float32)
    in_maps = []
    for i in range(n_cores):
        m = dict(shared)
        m["x"] = np.ascontiguousarray(x[i * NB:(i + 1) * NB])
        m["c"] = np.ascontiguousarray(c[i * NB:(i + 1) * NB])
        m["ctx"] = np.ascontiguousarray(ctx[i * NB:(i + 1) * NB])
        in_maps.append(m)
    res = run_bass_kernel_spmd(nc, in_maps, core_ids=list(range(n_cores)))
    return np.concatenate([np.asarray(r["out"]) for r in res.results], axis=0).astype(np.float32)
```
